# Optimizing a Trainium2 kernel written in Bass

```python
import functools
import jax, jax.numpy as jnp
from jax import lax
import numpy as np

D_MODEL = 1024
BATCH = 4
SEQ = 8192
DEPTH = 1

GRID_W = 64
CTX_LEN = 256
D_CONV = 1024
CONV_W = 3
D_LRU = 1024
LRU_BLOCKS = 16
LRU_BLOCK_W = D_LRU // LRU_BLOCKS
LRU_CONV_W = 4
LRU_C = 8.0
N_BRANCH = 2
N_EXPERTS = 32
TOP_K = 4
D_EXPERT = 1024
SWIGLU_LIMIT = 7.0
SWIGLU_ALPHA = 1.702
MOE_BLOCK = 512
N_MOD = 6
EPS = 1e-6
IN_COLS = 3 * D_CONV + 2 * D_LRU + N_BRANCH * D_MODEL
SPLIT_AT = (D_CONV, 2 * D_CONV, 3 * D_CONV, 3 * D_CONV + D_LRU, 3 * D_CONV + 2 * D_LRU,
            3 * D_CONV + 2 * D_LRU + D_MODEL)

kernel_name = "hybrid_conv_rglru_moe_diffusion_block"


def rmsnorm(x, g):
    xf = x.astype(jnp.float32)
    y = xf * lax.rsqrt(jnp.mean(xf * xf, axis=-1, keepdims=True) + EPS)
    return (y * g.astype(jnp.float32)).astype(x.dtype)


def modulate(h, shift, scale):
    return h * (1 + scale) + shift


def adaln_params(silu_cond, w, b):
    m = silu_cond @ w + b
    m = m.reshape(m.shape[0], 1, N_MOD, D_MODEL)
    return tuple(m[:, :, i] for i in range(N_MOD))


def seq_conv3(u, w):
    L = u.shape[1]
    up = jnp.pad(u, ((0, 0), (1, 1), (0, 0)))
    return sum(w[k] * up[:, k:k + L] for k in range(CONV_W))


def grid_conv3(u, w, rows):
    B_, L, C = u.shape
    half = C // 2
    g = u.reshape(B_, rows, GRID_W, C)
    gh = jnp.pad(g[..., :half], ((0, 0), (0, 0), (1, 1), (0, 0)))
    gv = jnp.pad(g[..., half:], ((0, 0), (1, 1), (0, 0), (0, 0)))
    yh = sum(w[k, :half] * gh[:, :, k:k + GRID_W] for k in range(CONV_W))
    yv = sum(w[k, half:] * gv[:, k:k + rows] for k in range(CONV_W))
    return jnp.concatenate([yh, yv], axis=-1).reshape(B_, L, C)


def directional_conv4(u, w, b, reverse):
    L = u.shape[1]
    k = LRU_CONV_W
    if reverse:
        up = jnp.pad(u, ((0, 0), (0, k - 1), (0, 0)))
        return b + sum(w[j] * up[:, k - 1 - j:k - 1 - j + L] for j in range(k))
    up = jnp.pad(u, ((0, 0), (k - 1, 0), (0, 0)))
    return b + sum(w[j] * up[:, j:j + L] for j in range(k))


def rglru_coeffs(v, wa, ba, wx, bx, lam):
    B_, L, C = v.shape
    vb = v.reshape(B_, L, LRU_BLOCKS, LRU_BLOCK_W)
    r = jax.nn.sigmoid(jnp.einsum('blnk,nkj->blnj', vb, wa).reshape(B_, L, C) + ba)
    i = jax.nn.sigmoid(jnp.einsum('blnk,nkj->blnj', vb, wx).reshape(B_, L, C) + bx)
    log_a = (-LRU_C * jax.nn.softplus(-lam.astype(jnp.float32))) * r.astype(jnp.float32)
    a = jnp.exp(log_a)
    b = jnp.sqrt(-jnp.expm1(2.0 * log_a)) * (i * v).astype(jnp.float32)
    return a, b


def linear_scan(a, b, h0, reverse):
    edge = -1 if reverse else 0
    b = b.at[:, edge].add(a[:, edge] * h0)

    def comb(lhs, rhs):
        return (lhs[0] * rhs[0], rhs[0] * lhs[1] + rhs[1])

    _, h = lax.associative_scan(comb, (a, b), reverse=reverse, axis=1)
    return h


def lru_direction(u, conv_w, conv_b, wa, ba, wx, bx, lam, h0, reverse):
    v = directional_conv4(u, conv_w, conv_b, reverse)
    a, b = rglru_coeffs(v, wa, ba, wx, bx, lam)
    return linear_scan(a, b, h0, reverse)


def merge_branches(cols, conv_fn, h_lru, conv_w, w_out_a, w_out_b, b_merge, w_o):
    gate_b, gate_c, xc, _, lru_gate, m_a, m_b = cols
    y_a = (gate_b * conv_fn(gate_c * xc, conv_w)) @ w_out_a
    y_b = (jax.nn.gelu(lru_gate) * h_lru.astype(lru_gate.dtype)) @ w_out_b
    merged = jax.nn.sigmoid(m_a + b_merge[0]) * y_a + jax.nn.sigmoid(m_b + b_merge[1]) * y_b
    return merged @ w_o


def moe(xt, router_w, router_b, w1, b1, w2, b2):
    T, D = xt.shape
    logits = xt.astype(jnp.float32) @ router_w.astype(jnp.float32) + router_b.astype(jnp.float32)
    top_val, top_idx = lax.top_k(logits, TOP_K)
    gates = jax.nn.softmax(top_val, axis=-1)
    flat_e = top_idx.reshape(-1)
    flat_tok = jnp.arange(T * TOP_K, dtype=jnp.int32) // TOP_K
    flat_g = gates.reshape(-1)
    order = jnp.argsort(flat_e)
    se, stok, sg = flat_e[order], flat_tok[order], flat_g[order]
    counts = jnp.bincount(flat_e, length=N_EXPERTS)
    starts = jnp.cumsum(counts) - counts
    pcounts = ((counts + MOE_BLOCK - 1) // MOE_BLOCK) * MOE_BLOCK
    pends = jnp.cumsum(pcounts)
    pstarts = pends - pcounts
    dest = pstarts[se] + (jnp.arange(T * TOP_K, dtype=jnp.int32) - starts[se])
    n_blocks = (T * TOP_K + MOE_BLOCK - 1) // MOE_BLOCK + N_EXPERTS
    n_rows = n_blocks * MOE_BLOCK
    row_tok = jnp.zeros((n_rows,), jnp.int32).at[dest].set(stok)
    row_g = jnp.zeros((n_rows,), xt.dtype).at[dest].set(sg.astype(xt.dtype))
    block_e = jnp.clip(jnp.searchsorted(pends, jnp.arange(n_blocks) * MOE_BLOCK, side='right'),
                       0, N_EXPERTS - 1)

    def run_block(args):
        tok, e = args
        h = xt[tok] @ w1[e] + b1[e]
        glu = jnp.minimum(h[:, ::2], SWIGLU_LIMIT)
        lin = jnp.clip(h[:, 1::2], -SWIGLU_LIMIT, SWIGLU_LIMIT)
        act = glu * jax.nn.sigmoid(SWIGLU_ALPHA * glu) * (lin + 1)
        return act @ w2[e] + b2[e]

    y = lax.map(run_block, (row_tok.reshape(n_blocks, MOE_BLOCK), block_e))
    y = y.reshape(n_rows, D) * row_g[:, None]
    return jax.ops.segment_sum(y, row_tok, num_segments=T)


def setup_inputs(seed: int = 0) -> dict:
    key = jax.random.key(seed)
    ks = jax.random.split(key, 32)
    f32 = jnp.float32

    def nrm(k, shape, scale):
        return jax.random.normal(k, shape, f32) * scale

    u = jax.random.uniform(ks[16], (DEPTH, 2, D_LRU), f32, 0.9, 0.999)
    a0 = u ** (1.0 / LRU_C)
    lam = jnp.log(a0) - jnp.log1p(-a0)
    return {
        "x": nrm(ks[0], (BATCH, SEQ, D_MODEL), 1.0),
        "c": nrm(ks[1], (BATCH, D_MODEL), 1.0),
        "ctx": nrm(ks[2], (BATCH, CTX_LEN, D_MODEL), 1.0),
        "c_ctx": nrm(ks[3], (D_MODEL,), 1.0),
        "w_ada": nrm(ks[4], (DEPTH, D_MODEL, N_MOD * D_MODEL), D_MODEL ** -0.5),
        "b_ada": nrm(ks[5], (DEPTH, N_MOD * D_MODEL), 0.02),
        "norm_mix": 1.0 + nrm(ks[6], (DEPTH, D_MODEL), 0.02),
        "w_in": nrm(ks[7], (DEPTH, D_MODEL, IN_COLS), D_MODEL ** -0.5),
        "conv_a_w": nrm(ks[8], (DEPTH, CONV_W, D_CONV), CONV_W ** -0.5),
        "w_out_a": nrm(ks[9], (DEPTH, D_CONV, D_MODEL), D_CONV ** -0.5),
        "lru_conv_w": nrm(ks[10], (DEPTH, 2, LRU_CONV_W, D_LRU), LRU_CONV_W ** -0.5),
        "lru_conv_b": nrm(ks[11], (DEPTH, 2, D_LRU), 0.02),
        "lru_wa": nrm(ks[12], (DEPTH, 2, LRU_BLOCKS, LRU_BLOCK_W, LRU_BLOCK_W), LRU_BLOCK_W ** -0.5),
        "lru_ba": nrm(ks[13], (DEPTH, 2, D_LRU), 0.02),
        "lru_wx": nrm(ks[14], (DEPTH, 2, LRU_BLOCKS, LRU_BLOCK_W, LRU_BLOCK_W), LRU_BLOCK_W ** -0.5),
        "lru_bx": nrm(ks[15], (DEPTH, 2, D_LRU), 0.02),
        "lru_lambda": lam,
        "w_out_b": nrm(ks[17], (DEPTH, D_LRU, D_MODEL), D_LRU ** -0.5),
        "b_merge": nrm(ks[18], (DEPTH, N_BRANCH, D_MODEL), 0.02),
        "w_o": nrm(ks[19], (DEPTH, D_MODEL, D_MODEL), D_MODEL ** -0.5),
        "norm_ffn": 1.0 + nrm(ks[20], (DEPTH, D_MODEL), 0.02),
        "router_w": nrm(ks[21], (DEPTH, D_MODEL, N_EXPERTS), D_MODEL ** -0.5),
        "router_b": nrm(ks[22], (DEPTH, N_EXPERTS), 0.01),
        "w1": nrm(ks[23], (DEPTH, N_EXPERTS, D_MODEL, 2 * D_EXPERT), D_MODEL ** -0.5),
        "b1": nrm(ks[24], (DEPTH, N_EXPERTS, 2 * D_EXPERT), 0.02),
        "w2": nrm(ks[25], (DEPTH, N_EXPERTS, D_EXPERT, D_MODEL), D_EXPERT ** -0.5),
        "b2": nrm(ks[26], (DEPTH, N_EXPERTS, D_MODEL), 0.02),
        "norm_final": 1.0 + nrm(ks[27], (D_MODEL,), 0.02),
    }


def reference(x, c, ctx, c_ctx, w_ada, b_ada, norm_mix, w_in, conv_a_w, w_out_a, lru_conv_w,
              lru_conv_b, lru_wa, lru_ba, lru_wx, lru_bx, lru_lambda, w_out_b, b_merge, w_o,
              norm_ffn, router_w, router_b, w1, b1, w2, b2, norm_final):
    B_, S, D = x.shape
    rows = S // GRID_W
    latent_conv = functools.partial(grid_conv3, rows=rows)
    silu_c = jax.nn.silu(c)
    silu_cc = jax.nn.silu(c_ctx)[None]
    split_cols = lambda p: jnp.split(p, SPLIT_AT, axis=-1)
    for l in range(DEPTH):
        last = l == DEPTH - 1
        mx = adaln_params(silu_c, w_ada[l], b_ada[l])
        mc = adaln_params(silu_cc, w_ada[l], b_ada[l])

        hx = modulate(rmsnorm(x, norm_mix[l]), mx[0], mx[1])
        hc = modulate(rmsnorm(ctx, norm_mix[l]), mc[0], mc[1])
        xs = split_cols(jnp.einsum('bld,de->ble', hx, w_in[l]))
        cs = split_cols(jnp.einsum('bld,de->ble', hc, w_in[l]))
        fwd = (lru_conv_w[l, 0], lru_conv_b[l, 0], lru_wa[l, 0], lru_ba[l, 0],
               lru_wx[l, 0], lru_bx[l, 0], lru_lambda[l, 0])
        bwd = (lru_conv_w[l, 1], lru_conv_b[l, 1], lru_wa[l, 1], lru_ba[l, 1],
               lru_wx[l, 1], lru_bx[l, 1], lru_lambda[l, 1])
        h_zero = jnp.zeros((B_, D_LRU), jnp.float32)
        hcf = lru_direction(cs[3], *fwd, h0=h_zero, reverse=False)
        hcb = lru_direction(cs[3], *bwd, h0=h_zero, reverse=True)
        hxf = lru_direction(xs[3], *fwd, h0=hcf[:, -1], reverse=False)
        hxb = lru_direction(xs[3], *bwd, h0=hcb[:, 0], reverse=True)
        mix_x = merge_branches(xs, latent_conv, hxf + hxb, conv_a_w[l], w_out_a[l], w_out_b[l],
                               b_merge[l], w_o[l])
        x = x + mx[2] * mix_x
        if not last:
            mix_c = merge_branches(cs, seq_conv3, hcf + hcb, conv_a_w[l], w_out_a[l], w_out_b[l],
                                   b_merge[l], w_o[l])
            ctx = ctx + mc[2] * mix_c

        fx = modulate(rmsnorm(x, norm_ffn[l]), mx[3], mx[4]).reshape(-1, D)
        if last:
            y = moe(fx, router_w[l], router_b[l], w1[l], b1[l], w2[l], b2[l])
            x = x + mx[5] * y.reshape(x.shape)
        else:
            fc = modulate(rmsnorm(ctx, norm_ffn[l]), mc[3], mc[4]).reshape(-1, D)
            y = moe(jnp.concatenate([fx, fc], axis=0), router_w[l], router_b[l], w1[l], b1[l],
                    w2[l], b2[l])
            n_lat = fx.shape[0]
            x = x + mx[5] * y[:n_lat].reshape(x.shape)
            ctx = ctx + mc[5] * y[n_lat:].reshape(ctx.shape)
    return rmsnorm(x, norm_final)
```

```python
import numpy as np
from contextlib import ExitStack
import concourse.bass as bass
import concourse.mybir as mybir
from concourse.bass_utils import run_bass_kernel_spmd

F32 = mybir.dt.float32
BF16 = mybir.dt.bfloat16
AF = mybir.ActivationFunctionType
ALU = mybir.AluOpType

D = 1024
NTOK = 8192
OWN = 4096
TB = 512
NEXP = 32
EPS = 1e-6
GC0 = 0.7978845608028654
GC1 = 0.044715
DEBUG = False
STAGE = "full"
CORES = list(range(8))


class T:
    def __init__(self, t, name):
        self.t = t; self.name = name
        self.w = None; self.r = {}
        self.dsem = None; self.dcnt = 0

    def __getitem__(self, k):
        return self.t[k]


class Sem:
    uid = 0

    def __init__(self, h):
        self.h = h
        Sem.uid += 1
        self.id = Sem.uid


class E:
    def __init__(self, fw, name, eng):
        self.fw = fw; self.name = name; self.eng = eng
        self.sem = fw.newsem("e_" + name, fw.es0); self.count = 0
        self.seen = {}

    def wait(self, deps):
        best = {}
        for (s, v) in deps:
            if s.id not in best or best[s.id][1] < v:
                best[s.id] = (s, v)
        for k, (s, v) in best.items():
            if s is self.sem and self.name == "pe":
                continue
            if self.seen.get(k, 0) >= v:
                continue
            self.eng.wait_ge(s.h, v)
            self.seen[k] = v


class FW:
    def __init__(self, nc, es0):
        self.nc = nc; self.es0 = es0; self.es = es0
        self.nname = 0
        self.pe = E(self, "pe", nc.tensor)
        self.act = E(self, "act", nc.scalar)
        self.dve = E(self, "dve", nc.vector)
        self.pool = E(self, "pool", nc.gpsimd)
        self.sp = E(self, "sp", nc.sync)
        self.engs = [self.pe, self.act, self.dve, self.pool, self.sp]
        self.dtiles = []
        self.nname = 0

    def newsem(self, name, es=None):
        self.nname += 1
        return Sem((es or self.es).enter_context(self.nc.semaphore(f"{name}_{self.nname}")))

    def sb(self, name, shape, dt):
        self.nname += 1
        return T(self.es.enter_context(self.nc.sbuf_tensor(f"{name}_{self.nname}", shape, dt)), name)

    def ps(self, name, shape, dt):
        self.nname += 1
        return T(self.es.enter_context(self.nc.psum_tensor(f"{name}_{self.nname}", shape, dt)), name)

    def _deps(self, reads, writes):
        deps = []
        for t in reads:
            if t.w: deps.append(t.w)
        for t in writes:
            if t.w: deps.append(t.w)
            deps += list(t.r.values())
        return deps

    def op(self, e, fn, reads=(), writes=(), inc=True):
        e.wait(self._deps(reads, writes))
        inst = fn()
        if inc:
            e.count += 1
            inst.then_inc(e.sem.h, 1)
            d = (e.sem, e.count)
        else:
            d = (e.sem, e.count + 1)
        for t in reads:
            t.r[e.sem.id] = d
        for t in writes:
            t.w = d; t.r = {}
        return inst

    def dma(self, q, out, in_, semt, reads=(), writes=(), **kw):
        q.wait(self._deps(reads, writes))
        if semt.dsem is None:
            semt.dsem = self.newsem("d_" + semt.name)
            self.dtiles.append(semt)
        inst = q.eng.dma_start(out=out, in_=in_, **kw)
        semt.dcnt += 16
        inst.then_inc(semt.dsem.h, 16)
        d = (semt.dsem, semt.dcnt)
        for t in reads:
            t.r[semt.dsem.id] = d
        for t in writes:
            t.w = d; t.r = {}
        return inst

    def barrier(self):
        deps = [(e.sem, e.count) for e in self.engs if e.count > 0]
        deps += [(t.dsem, t.dcnt) for t in self.dtiles if t.dcnt > 0]
        for e in self.engs:
            e.wait(deps)
        self.dtiles = []


def build_nc():
    nc = bass.Bass("TRN2", target_bir_lowering=False)

    def din(name, shape):
        return nc.dram_tensor(name, shape, F32, kind="ExternalInput").ap()

    x_d = din("x", [NTOK, D]); ctx_d = din("ctx", [256, D]); cv_d = din("cvec", [2, D])
    wada_d = din("w_ada", [D, 6 * D]); bada_d = din("b_ada", [1, 6 * D])
    nmix_d = din("norm_mix", [1, D]); win_d = din("w_in", [D, 7 * D])
    caw_d = din("conv_a_w", [3, D]); wouta_d = din("w_out_a", [D, D])
    lcw_d = din("lru_conv_w", [2, 4, D]); lcb_d = din("lru_conv_b", [2, D])
    lwa_d = din("lru_wa", [2, 16, 64, 64]); lba_d = din("lru_ba", [2, D])
    lwx_d = din("lru_wx", [2, 16, 64, 64]); lbx_d = din("lru_bx", [2, D])
    lam_d = din("lru_lambda", [2, D]); woutb_d = din("w_out_b", [D, D])
    bm_d = din("b_merge", [2, D]); wo_d = din("w_o", [D, D]); nffn_d = din("norm_ffn", [1, D])
    rw_d = din("router_w", [D, NEXP]); rb_d = din("router_b", [1, NEXP])
    NE_ = NEXP if STAGE == "full" else 1
    w1_d = din("w1", [NE_, D, 2 * D]); b1_d = din("b1", [NEXP, 2 * D])
    w2_d = din("w2", [NE_, D, D]); b2_d = din("b2", [NEXP, D]); nfin_d = din("norm_final", [1, D])
    out_d = nc.dram_tensor("out", [OWN, D], F32, kind="ExternalOutput").ap()
    HB_d = nc.dram_tensor("HB", [128, 8, OWN], BF16).ap()
    YB_d = nc.dram_tensor("YB", [128, 8, OWN], BF16).ap()
    YA_d = nc.dram_tensor("YA", [128, 8, OWN], BF16).ap()
    YI_d = nc.dram_tensor("YI", [128, 8, OWN], BF16).ap()
    MB_d = nc.dram_tensor("MB", [3, 128, D], F32).ap()
    XS_d = nc.dram_tensor("XS", [64 * 512, D], BF16).ap()
    YS_d = nc.dram_tensor("YS", [64 * 512, D], F32).ap()
    FXS_d = nc.dram_tensor("FXS", [OWN, D], BF16).ap()
    WB1_d = nc.dram_tensor("WB1", [NEXP, 128, 8 * 2 * D], BF16).ap()
    WB2_d = nc.dram_tensor("WB2", [NEXP, 128, 8 * D], BF16).ap()
    B1T_d = nc.dram_tensor("B1T", [NEXP * 128, 16], F32).ap()
    if DEBUG:
        X1_d = nc.dram_tensor("X1", [OWN, D], F32, kind="ExternalOutput").ap()
    else:
        X1_d = nc.dram_tensor("X1", [OWN, D], F32).ap()

    with ExitStack() as es0:
        fw = FW(nc, es0)
        es0.enter_context(nc.Block())
        pe, act, dve, pool, sp = fw.pe, fw.act, fw.dve, fw.pool, fw.sp
        V, S, G, PE = nc.vector, nc.scalar, nc.gpsimd, nc.tensor
        HB_t = [T(None, f"HB{n}") for n in range(8)]
        YB_t = [T(None, f"YB{n}") for n in range(8)]
        YA_t = [T(None, f"YA{n}") for n in range(8)]
        YI_t = [T(None, f"YI{n}") for n in range(8)]
        MB_t = T(None, "MB")
        X1_t = [T(None, f"X1{n}") for n in range(32)]

        def wview(ap2d):
            return ap2d.rearrange("(kc p) n -> p kc n", p=128)

        identf = fw.sb("identf", [128, 128], F32)
        identb = fw.sb("identb", [128, 128], BF16)
        ones = fw.sb("ones", [128, 128], F32)
        mhalf = fw.sb("mhalf", [128, 1], F32)
        fw.op(pool, lambda: G.memset(ones[:], 1.0), writes=[ones])
        fw.op(pool, lambda: G.memset(mhalf[:], -0.5), writes=[mhalf])
        fw.op(pool, lambda: G.memset(identf[:], 1.0), writes=[identf])
        fw.op(pool, lambda: G.affine_select(identf[:], identf[:], [[-1, 128]], ALU.is_equal, 0.0, base=0,
                                            channel_multiplier=1), reads=[identf], writes=[identf])
        fw.op(dve, lambda: V.tensor_copy(identb[:], identf[:]), reads=[identf], writes=[identb])

        class NormBufs:
            def __init__(self, dt_out, tag):
                self.i = 0
                self.xt = [fw.sb(f"xt{tag}{i}", [128, D], F32) for i in range(2)]
                self.junk = fw.sb(f"junk{tag}", [128, D], BF16)
                self.ss = [fw.sb(f"ss{tag}{i}", [128, 1], F32) for i in range(2)]
                self.rs = [fw.sb(f"rs{tag}{i}", [128, 1], F32) for i in range(2)]
                self.t1 = [fw.sb(f"t1{tag}{i}", [128, D], F32) for i in range(2)]
                self.hx = [fw.sb(f"hx{tag}{i}", [128, D], dt_out) for i in range(2)]

        def norm_load(nb, src_ap, nr, src_dep=()):
            i = nb.i; nb.i ^= 1
            xt = nb.xt[i]
            fw.dma(sp, xt[0:nr, :], src_ap, xt, reads=list(src_dep), writes=[xt])
            return i

        def norm_tile(nb, src_ap, nr, Gt, SHt, src_dep=()):
            i = norm_load(nb, src_ap, nr, src_dep)
            return norm_compute(nb, i, nr, Gt, SHt)

        def norm_compute(nb, i, nr, Gt, SHt):
            xt, ss, rs, t1, hx = nb.xt[i], nb.ss[i], nb.rs[i], nb.t1[i], nb.hx[i]
            fw.op(act, lambda: S.activation(nb.junk[0:nr, :], xt[0:nr, :], AF.Square, scale=1.0 / 32.0,
                                            accum_out=ss[0:nr, :]), reads=[xt], writes=[nb.junk, ss])
            fw.op(pool, lambda: G.tensor_scalar(rs[0:nr, :], ss[0:nr, :], EPS, None, ALU.add), reads=[ss], writes=[rs])
            fw.op(pool, lambda: G.tensor_tensor(rs[0:nr, :], rs[0:nr, :], mhalf[0:nr, :], ALU.pow),
                  reads=[rs, mhalf], writes=[rs])
            fw.op(dve, lambda: V.scalar_tensor_tensor(t1[0:nr, :], xt[0:nr, :], rs[0:nr, 0:1], Gt[0:nr, :],
                                                      ALU.mult, ALU.mult), reads=[xt, rs, Gt], writes=[t1])
            fw.op(pool, lambda: G.tensor_tensor(hx[0:nr, :], t1[0:nr, :], SHt[0:nr, :], ALU.add),
                  reads=[t1, SHt], writes=[hx])
            return hx, xt

        class HxT:
            def __init__(self, ncols, tag):
                self.nb = NormBufs(BF16, tag)
                self.ptr = [fw.ps(f"ptr{tag}{i}", [128, D], BF16) for i in range(2)]
                self.out = [fw.sb(f"hxT{tag}{i}", [128, 8, ncols], BF16) for i in range(2)]
                self.i = 0; self.pi = 0

            def start(self, src_d, row0, ntok, Gt, SHt):
                o = self.out[self.i]; self.i ^= 1

                def gen():
                    nb = self.nb
                    tiles = []
                    c = 0
                    while c < ntok:
                        tiles.append((c, min(128, ntok - c)))
                        c += 128
                    nt = len(tiles)
                    st_ = {}

                    def stage(t, s_):
                        c, nr = tiles[t]
                        if s_ == -1:
                            st_[t] = norm_load(nb, src_d[row0 + c: row0 + c + nr, :], nr)
                            return
                        i = st_[t]
                        xt, ss, rs, t1, hx = nb.xt[i], nb.ss[i], nb.rs[i], nb.t1[i], nb.hx[i]
                        if s_ == 0:
                            fw.op(act, lambda: S.activation(nb.junk[0:nr, :], xt[0:nr, :], AF.Square, scale=1.0 / 32.0,
                                                            accum_out=ss[0:nr, :]), reads=[xt], writes=[nb.junk, ss])
                            fw.op(pool, lambda: G.tensor_scalar(rs[0:nr, :], ss[0:nr, :], EPS, None, ALU.add), reads=[ss], writes=[rs])
                            fw.op(pool, lambda: G.tensor_tensor(rs[0:nr, :], rs[0:nr, :], mhalf[0:nr, :], ALU.pow),
                                  reads=[rs, mhalf], writes=[rs])
                        elif s_ == 1:
                            fw.op(dve, lambda: V.scalar_tensor_tensor(t1[0:nr, :], xt[0:nr, :], rs[0:nr, 0:1], Gt[0:nr, :],
                                                                      ALU.mult, ALU.mult), reads=[xt, rs, Gt], writes=[t1])
                            fw.op(pool, lambda: G.tensor_tensor(hx[0:nr, :], t1[0:nr, :], SHt[0:nr, :], ALU.add),
                                  reads=[t1, SHt], writes=[hx])
                        elif s_ == 2:
                            p = self.ptr[t % 2]
                            for kc in range(8):
                                fw.op(pe, lambda: PE.transpose(p[:, kc * 128: kc * 128 + nr], hx[0:nr, kc * 128:(kc + 1) * 128],
                                                               identb[0:nr, 0:nr]), reads=[hx, identb], writes=[p], inc=(kc == 7))
                        else:
                            p = self.ptr[t % 2]
                            pv = p.t[:].rearrange("p (k n) -> p k n", k=8)
                            fw.op(act, lambda: S.copy(o[:, :, c:c + nr], pv[:, :, 0:nr]), reads=[p], writes=[o])

                    stage(0, -1)
                    ncalls = 2 * (nt - 1) + 4
                    for ci_ in range(ncalls):
                        for t in range(nt):
                            s_ = ci_ - 2 * t
                            if s_ == 0 and t + 1 < nt:
                                stage(t + 1, -1)
                            if 0 <= s_ <= 3:
                                stage(t, s_)
                        yield
                return o, gen()

            def make(self, src_d, row0, ntok, Gt, SHt):
                o, g = self.start(src_d, row0, ntok, Gt, SHt)
                for _ in g:
                    pass
                return o

        def drain(g):
            for _ in g:
                pass

        def pipelined(hb, order, ntok):
            hxT = hb.make(x_d, order[0] * TB, ntok, G1h[0], SH1h[0])
            for idx, n in enumerate(order):
                if idx + 1 < len(order):
                    nxt, g = hb.start(x_d, order[idx + 1] * TB, ntok, G1h[0], SH1h[0])
                else:
                    nxt, g = None, iter(())
                yield n, hxT, g
                drain(g)
                hxT = nxt

        G1h = [None]; SH1h = [None]

        def proj(ps_ap, pst, wt, col0, hxT, n0, n):
            for kc in range(8):
                fw.op(pe, lambda: PE.matmul(ps_ap, wt[:, kc, col0:col0 + 128], hxT[:, kc, n0:n0 + n],
                                            start=(kc == 0), stop=(kc == 7)), reads=[wt, hxT], writes=[pst], inc=(kc == 7))

        def load_w(tile, ap2d, n):
            for c0 in range(0, n, 512):
                fw.dma(pool, tile[:, :, c0:c0 + 512], wview(ap2d[:, c0:c0 + 512]), tile, writes=[tile])

        wbsem = fw.newsem("wbsem", es0)
        wbcnt = [0]

        def precast_gen():
            if STAGE != "full":
                return
            for e in range(NEXP):
                inst = G.dma_start(out=WB1_d[e].rearrange("p (k n) -> p k n", k=8), in_=w1_d[e].rearrange("(k p) n -> p k n", p=128))
                inst.then_inc(wbsem.h, 16); wbcnt[0] += 16
                yield
                inst = G.dma_start(out=WB2_d[e].rearrange("p (k n) -> p k n", k=8), in_=w2_d[e].rearrange("(k p) n -> p k n", p=128))
                inst.then_inc(wbsem.h, 16); wbcnt[0] += 16
                yield
        pcg = precast_gen()

        with ExitStack() as esM:
            fw.es = esM
            G1 = fw.sb("G1", [128, D], F32); SH1 = fw.sb("SH1", [128, D], F32)
            HG1 = fw.sb("HG1", [128, D], F32)
            G1h[0] = G1; SH1h[0] = SH1
            vT1 = fw.sb("vT1", [128, 128], F32); vT2 = fw.sb("vT2", [128, 64], F32)
            sc = fw.sb("sc", [128, 96], F32)
            stA = [fw.sb(f"stA{i}", [128, 1], F32) for i in range(8)]; stB = [fw.sb(f"stB{i}", [128, 1], F32) for i in range(8)]
            zst = fw.sb("zst", [128, 8], F32)
            esL = ExitStack()
            fw.es = esL
            Dg = fw.sb("Dg", [128, 64, 128], BF16)
            Wg = fw.sb("Wg", [128, 32, 128], BF16)
            esC = ExitStack()
            fw.es = esC
            G1c = fw.sb("G1c", [128, D], F32); SH1c = fw.sb("SH1c", [128, D], F32)
            C_C, C_CC, C_CB, C_BA, C_BX, C_LAM, C_BM, C_CAW = 0, 8, 16, 32, 48, 64, 80, 96
            S_HBA, S_HBX, S_CS, S_HC, S_HBM = 0, 16, 32, 48, 64

            with ExitStack() as esA:
                fw.es = esA
                vr1 = fw.sb("vr1", [128, 128], F32); vr2 = fw.sb("vr2", [64, 128], F32)
                G2 = fw.sb("G2", [128, D], F32); SH2 = fw.sb("SH2", [128, D], F32); GATE2 = fw.sb("GATE2", [128, D], F32)
                fw.op(pool, lambda: G.memset(vr1[:], 0.0), writes=[vr1])
                rows = [(cv_d, C_C, 16), (lcb_d, C_CB, 16), (lba_d, C_BA, 16), (lbx_d, C_BX, 16), (lam_d, C_LAM, 16),
                        (bm_d, C_BM, 16), (caw_d, C_CAW, 24)]
                for ap, r0, n in rows:
                    fw.dma(sp, vr1[r0:r0 + n, :], ap.rearrange("d (k p) -> (d k) p", p=128), vr1, writes=[vr1])
                fw.dma(sp, vr2[:, :], lcw_d.rearrange("d j (k p) -> (d j k) p", p=128), vr2, writes=[vr2])
                pt = fw.ps("pt0", [128, 128], F32)
                fw.op(pe, lambda: PE.transpose(pt[:, 0:120], vr1[0:120, :], identf[0:120, 0:120]), reads=[vr1, identf], writes=[pt])
                fw.op(dve, lambda: V.tensor_copy(vT1[:, 0:120], pt[:, 0:120]), reads=[pt], writes=[vT1])
                fw.op(pe, lambda: PE.transpose(pt[:, 0:64], vr2[0:64, :], identf[0:64, 0:64]), reads=[vr2, identf], writes=[pt])
                fw.op(dve, lambda: V.tensor_copy(vT2[:, :], pt[:, 0:64]), reads=[pt], writes=[vT2])
                fw.op(dve, lambda: V.tensor_scalar(sc[:, S_HBA:S_HBA + 32], vT1[:, C_BA:C_BA + 32], 0.5, None, ALU.mult),
                      reads=[vT1], writes=[sc])
                fw.op(dve, lambda: V.tensor_scalar(sc[:, S_HBM:S_HBM + 16], vT1[:, C_BM:C_BM + 16], 0.5, None, ALU.mult),
                      reads=[vT1], writes=[sc])
                tl = fw.sb("tl", [128, 16], F32)
                fw.op(act, lambda: S.activation(tl[:], vT1[:, C_LAM:C_LAM + 16], AF.Exp, scale=-1.0), reads=[vT1], writes=[tl])
                fw.op(act, lambda: S.activation(tl[:], tl[:], AF.Ln, bias=1.0, scale=1.0), reads=[tl], writes=[tl])
                fw.op(dve, lambda: V.tensor_scalar(sc[:, S_CS:S_CS + 16], tl[:], -8.0, None, ALU.mult), reads=[tl], writes=[sc])
                fw.op(dve, lambda: V.tensor_scalar(sc[:, S_HC:S_HC + 16], tl[:], -4.0, None, ALU.mult), reads=[tl], writes=[sc])
                fw.op(pool, lambda: G.memset(zst[:], 0.0), writes=[zst])
                for idx in range(64):
                    fw.op(dve, lambda: V.tensor_scalar(Dg[:, idx, :], identf[:], vT2[:, idx:idx + 1], None, ALU.mult),
                          reads=[identf, vT2], writes=[Dg])
                fw.op(pool, lambda: G.memset(Wg[:], 0.0), writes=[Wg])
                for g, wd in enumerate((lwa_d, lwx_d)):
                    for d in range(2):
                        base = (g * 2 + d) * 8
                        src = wd[d].rearrange("(kc two) k j -> two k kc j", two=2)
                        for h in range(2):
                            fw.dma(pool, Wg[64 * h:64 * h + 64, base:base + 8, 64 * h:64 * h + 64], src[h], Wg, writes=[Wg])
                sil = fw.sb("sil", [128, 16], F32); th0 = fw.sb("th0", [128, 16], F32)
                fw.op(act, lambda: S.activation(th0[:], vT1[:, 0:16], AF.Tanh, scale=0.5), reads=[vT1], writes=[th0])
                fw.op(dve, lambda: V.tensor_scalar(th0[:], th0[:], 0.5, 0.5, ALU.mult, ALU.add), reads=[th0], writes=[th0])
                fw.op(dve, lambda: V.tensor_tensor(sil[:], th0[:], vT1[:, 0:16], ALU.mult), reads=[th0, vT1], writes=[sil])
                Sx = fw.sb("Sx", [128, 16, 128], F32)
                for k in range(16):
                    fw.op(dve, lambda: V.tensor_scalar(Sx[:, k, :], ones[:], sil[:, k:k + 1], None, ALU.mult),
                          reads=[ones, sil], writes=[Sx])
                nmx = fw.sb("nmx", [128, D], F32); nfx = fw.sb("nfx", [128, D], F32)
                fw.dma(sp, nmx[:], nmix_d[0:1, :].partition_broadcast(128), nmx, writes=[nmx])
                fw.dma(sp, nfx[:], nffn_d[0:1, :].partition_broadcast(128), nfx, writes=[nfx])
                wad = [fw.sb(f"wad{i}", [128, 8, 512], F32) for i in range(2)]
                bad = [fw.sb(f"bad{i}", [128, 512], F32) for i in range(2)]
                pa = [fw.ps(f"pa{i}", [128, 512], F32) for i in range(2)]
                tmpg = fw.sb("tmpg", [128, 512], F32)
                dests = {0: (SH1, 0), 1: (G1, 1), 2: (HG1, 2), 3: (SH2, 0), 4: (G2, 1), 5: (GATE2, 0)}
                cdests = {0: (SH1c, 0), 1: (G1c, 1)}
                for g in range(12):
                    wt = wad[g % 2]; bt = bad[g % 2]
                    fw.dma(sp, wt[:], wview(wada_d[:, g * 512:(g + 1) * 512]), wt, writes=[wt])
                    fw.dma(sp, bt[:], bada_d[0:1, g * 512:(g + 1) * 512].partition_broadcast(128), bt, writes=[bt])
                    for which in range(2):
                        if which == 1 and g >= 4:
                            continue
                        p = pa[which]
                        for kc in range(8):
                            fw.op(pe, lambda: PE.matmul(p[:], Sx[:, which * 8 + kc, :], wt[:, kc, :], start=(kc == 0), stop=(kc == 7)),
                                  reads=[Sx, wt], writes=[p], inc=(kc == 7))
                        dt_, kind = (dests if which == 0 else cdests)[g // 2]
                        cs = slice((g % 2) * 512, (g % 2) * 512 + 512)
                        if kind == 0:
                            fw.op(dve, lambda: V.tensor_tensor(dt_[:, cs], p[:], bt[:], ALU.add), reads=[p, bt], writes=[dt_])
                        elif kind == 2:
                            fw.op(dve, lambda: V.tensor_tensor(tmpg[:], p[:], bt[:], ALU.add), reads=[p, bt], writes=[tmpg])
                            fw.op(dve, lambda: V.tensor_scalar(dt_[:, cs], tmpg[:], 0.5, None, ALU.mult), reads=[tmpg], writes=[dt_])
                        else:
                            nrm = nmx if (which == 1 or g // 2 == 1) else nfx
                            fw.op(dve, lambda: V.tensor_tensor(tmpg[:], p[:], bt[:], ALU.add), reads=[p, bt], writes=[tmpg])
                            fw.op(dve, lambda: V.scalar_tensor_tensor(dt_[:, cs], tmpg[:], 1.0, nrm[:, cs], ALU.add, ALU.mult),
                                  reads=[tmpg, nrm], writes=[dt_])
                for i_, t_ in enumerate((G2, SH2, GATE2)):
                    fw.dma(pool, MB_d[i_], t_[:], t_, reads=[t_], writes=[MB_t])
                fw.barrier()
            fw.es = esC

            class LruBufs:
                def __init__(self, nset=2):
                    self.i2 = 0; self.ig = 0; self.ih = 0; self.nset = nset
                    mk = lambda nm, dt, k=2: [fw.sb(f"{nm}{i}", [128, TB], dt) for i in range(k)]
                    self.v = mk("lv", BF16); self.thr = mk("lthr", F32); self.thi = mk("lthi", F32)
                    self.a = mk("la", F32, nset); self.a2 = mk("la2", F32, nset); self.bb = mk("lbb", F32, nset)
                    self.h = mk("lh", F32)
                    self.pv = [fw.ps(f"lpv{i}", [128, TB], F32) for i in range(2)]
                    self.pr = [fw.ps(f"lpr{i}", [128, TB], F32) for i in range(1)]
                    self.pi_ = [fw.ps(f"lpi{i}", [128, TB], F32) for i in range(1)]

            def lru_alpha(lb, d, kc, uext, n, reverse):
                i = lb.i2; lb.i2 ^= 1
                g_ = lb.ig; lb.ig = (lb.ig + 1) % lb.nset
                v, thr, thi = lb.v[i], lb.thr[i], lb.thi[i]
                a, a2, bb = lb.a[g_], lb.a2[g_], lb.bb[g_]
                pv, pr, pi_ = lb.pv[i], lb.pr[0], lb.pi_[0]
                ci = d * 8 + kc
                for j in range(4):
                    off = (6 - j) if reverse else j
                    fw.op(pe, lambda: PE.matmul(pv[:, 0:n], Dg[:, d * 32 + j * 8 + kc, :], uext[:, kc, off:off + n],
                                                start=(j == 0), stop=(j == 3)), reads=[Dg, uext], writes=[pv], inc=(j == 3))
                fw.op(dve, lambda: V.tensor_scalar(v[:, 0:n], pv[:, 0:n], vT1[:, C_CB + ci:C_CB + ci + 1], None, ALU.add),
                      reads=[pv, vT1], writes=[v])
                fw.op(pe, lambda: PE.matmul(pr[:, 0:n], Wg[:, (0 * 2 + d) * 8 + kc, :], v[:, 0:n], start=True, stop=True),
                      reads=[Wg, v], writes=[pr])
                fw.op(pe, lambda: PE.matmul(pi_[:, 0:n], Wg[:, (1 * 2 + d) * 8 + kc, :], v[:, 0:n], start=True, stop=True),
                      reads=[Wg, v], writes=[pi_])
                fw.op(act, lambda: S.activation(thr[:, 0:n], pr[:, 0:n], AF.Tanh, bias=sc[:, S_HBA + ci:S_HBA + ci + 1], scale=0.5),
                      reads=[pr, sc], writes=[thr])
                fw.op(act, lambda: S.activation(thi[:, 0:n], pi_[:, 0:n], AF.Tanh, bias=sc[:, S_HBX + ci:S_HBX + ci + 1], scale=0.5),
                      reads=[pi_, sc], writes=[thi])
                fw.op(act, lambda: S.activation(a[:, 0:n], thr[:, 0:n], AF.Exp, bias=sc[:, S_HC + ci:S_HC + ci + 1],
                                                scale=sc[:, S_HC + ci:S_HC + ci + 1]), reads=[thr, sc], writes=[a])
                fw.op(pool, lambda: G.tensor_tensor(a2[:, 0:n], a[:, 0:n], a[:, 0:n], ALU.mult), reads=[a], writes=[a2])
                fw.op(dve, lambda: V.scalar_tensor_tensor(bb[:, 0:n], thi[:, 0:n], 1.0, v[:, 0:n], ALU.add, ALU.mult),
                      reads=[thi, v], writes=[bb])
                return (a, a2, bb)

            def lru_sqrt(ctx_, n):
                a, a2, bb = ctx_
                fw.op(act, lambda: S.activation(a2[:, 0:n], a2[:, 0:n], AF.Sqrt, bias=1.0, scale=-1.0), reads=[a2], writes=[a2])

            def lru_beta(lb, ctx_, kc, n, st, reverse):
                a, a2, bb = ctx_
                h = lb.h[lb.ih]; lb.ih ^= 1
                fw.op(dve, lambda: V.scalar_tensor_tensor(bb[:, 0:n], bb[:, 0:n], 0.5, a2[:, 0:n], ALU.mult, ALU.mult),
                      reads=[bb, a2], writes=[bb])
                if reverse:
                    fw.op(dve, lambda: V.tensor_tensor_scan(h[:, 0:n][:, ::-1], a[:, 0:n][:, ::-1], bb[:, 0:n][:, ::-1],
                                                            st[kc][:, 0:1], ALU.mult, ALU.add), reads=[a, bb, st[kc]], writes=[h])
                    fw.op(pool, lambda: G.tensor_copy(st[kc][:, 0:1], h[:, 0:1]), reads=[h], writes=[st[kc]])
                else:
                    fw.op(dve, lambda: V.tensor_tensor_scan(h[:, 0:n], a[:, 0:n], bb[:, 0:n], st[kc][:, 0:1], ALU.mult, ALU.add),
                          reads=[a, bb, st[kc]], writes=[h])
                    fw.op(pool, lambda: G.tensor_copy(st[kc][:, 0:1], h[:, n - 1:n]), reads=[h], writes=[st[kc]])
                return h

            def lru_group(lb, d, kcs, uext, n, st, reverse, cb=None, cba=None):
                ctxs = []
                for kc in kcs:
                    ctxs.append(lru_alpha(lb, d, kc, uext, n, reverse))
                    if cba is not None:
                        cba(kc)
                for c_ in ctxs:
                    lru_sqrt(c_, n)
                for kc, c_ in zip(kcs, ctxs):
                    h = lru_beta(lb, c_, kc, n, st, reverse)
                    if cb is not None:
                        cb(kc, h)

            with ExitStack() as es1:
                fw.es = es1
                w_lx = fw.sb("w_lx", [128, 8, D], BF16)
                load_w(w_lx, win_d[:, 3 * D:4 * D], D)
                hb = HxT(TB, "a")
                lb = LruBufs(8)
                uext = [fw.sb(f"uext{i}", [128, 8, TB + 6], BF16) for i in range(2)]
                pu = [fw.ps(f"pu{i}", [128, TB], F32) for i in range(2)]
                for u in uext:
                    fw.op(pool, lambda: G.memset(u[:], 0.0), writes=[u])
                for k_ in range(8):
                    fw.op(pool, lambda: G.tensor_copy(stA[k_][:], zst[:, 0:1]), reads=[zst], writes=[stA[k_]])
                    fw.op(pool, lambda: G.tensor_copy(stB[k_][:], zst[:, 0:1]), reads=[zst], writes=[stB[k_]])
                hcT = hb.make(ctx_d, 0, 256, G1c, SH1c)
                ue = uext[0]
                for kc in range(8):
                    p = pu[kc % 2]
                    proj(p[:, 0:256], p, w_lx, kc * 128, hcT, 0, 256)
                    fw.op(dve, lambda: V.tensor_copy(ue[:, kc, 3:259], p[:, 0:256]), reads=[p], writes=[ue])
                lru_group(lb, 0, list(range(8)), ue, 256, stA, False)
                lru_group(lb, 1, list(range(8)), ue, 256, stB, True)
                fw.op(pool, lambda: G.memset(ue[:], 0.0), writes=[ue])
                def do_proj1(nb, hxT_, kc):
                    p = pu[kc % 2]
                    proj(p[:], p, w_lx, kc * 128, hxT_, 0, TB)
                    fw.op(dve, lambda: V.tensor_copy(uext[nb % 2][:, kc, 3:3 + TB], p[:]), reads=[p], writes=[uext[nb % 2]])
                order = list(range(15, -1, -1))
                hxT = hb.make(x_d, order[0] * TB, TB, G1, SH1)
                for kc in range(8):
                    do_proj1(order[0], hxT, kc)
                for idx, n in enumerate(order):
                    ue = uext[n % 2]
                    if idx + 1 < len(order):
                        nn = order[idx + 1]
                        nxt, gnx = hb.start(x_d, nn * TB, TB, G1, SH1)
                    else:
                        nn, nxt, gnx = None, None, iter(())

                    def cba1(kc, gnx=gnx):
                        next(gnx, None)
                        if kc == 7:
                            drain(gnx)

                    def cb1(kc, h, n=n, nn=nn, nxt=nxt):
                        if n < 8:
                            fw.dma(pool, HB_d[:, kc, n * TB:(n + 1) * TB], h[:], h, reads=[h], writes=[HB_t[n]])
                        if nn is not None:
                            do_proj1(nn, nxt, kc)
                    lru_group(lb, 1, list(range(8)), ue, TB, stB, True, cb1, cba1)
                    drain(gnx)
                    if nn is not None:
                        un = uext[nn % 2]
                        fw.op(pool, lambda: G.tensor_copy(un[:, :, 3 + TB:6 + TB], ue[:, :, 3:6]), reads=[ue], writes=[un])
                fw.barrier()
            esC.close()
            fw.es = esL

            with ExitStack() as es2:
                fw.es = es2
                w_lx = fw.sb("w_lx", [128, 8, D], BF16); w_lg = fw.sb("w_lg", [128, 8, D], BF16)
                load_w(w_lx, win_d[:, 3 * D:4 * D], D); load_w(w_lg, win_d[:, 4 * D:5 * D], D)
                hb = HxT(TB, "b")
                lb = LruBufs(4)
                uext = [fw.sb(f"uext{i}", [128, 8, TB + 6], BF16) for i in range(2)]
                pu = [fw.ps(f"pu{i}", [128, TB], F32) for i in range(2)]
                for u in uext:
                    fw.op(pool, lambda: G.memset(u[:], 0.0), writes=[u])
                hbl = [fw.sb(f"hbl{i}", [128, 8, TB], BF16) for i in range(1)]
                ybin = [fw.sb(f"ybin{i}", [128, 8, TB], BF16) for i in range(2)]
                mk = lambda nm, dt: [fw.sb(f"{nm}{i}", [128, TB], dt) for i in range(2)]
                xs_, x2_, th_, hs_ = mk("gxs", F32), mk("gx2", F32), mk("gth", F32), mk("ghs", F32)
                def do_proj2(nb, hxT_, kc):
                    p = pu[kc % 2]
                    proj(p[:], p, w_lx, kc * 128, hxT_, 0, TB)
                    fw.op(dve, lambda: V.tensor_copy(uext[nb % 2][:, kc, 3:3 + TB], p[:]), reads=[p], writes=[uext[nb % 2]])
                hxT = hb.make(x_d, 0, TB, G1, SH1)
                for kc in range(8):
                    do_proj2(0, hxT, kc)
                nxt = None
                for n in range(8):
                    if n > 0:
                        hxT = nxt
                    next(pcg, None); next(pcg, None)
                    ue = uext[n % 2]
                    if n + 1 < 8:
                        nn = n + 1
                        nxt, gnx = hb.start(x_d, nn * TB, TB, G1, SH1)
                    else:
                        nn, nxt, gnx = None, None, iter(())
                    hbt = hbl[0]
                    fw.dma(sp, hbt[:], HB_d[:, :, n * TB:(n + 1) * TB], hbt, reads=[HB_t[n]], writes=[hbt])
                    yb = ybin[n % 2]
                    def cb2(kc, h, hxT=hxT, hbt=hbt, yb=yb, gnx=gnx, nn=nn, nxt=nxt):
                        i = kc % 2
                        hs = hs_[i]
                        fw.op(pool, lambda: G.tensor_tensor(hs[:], h[:], hbt[:, kc, :], ALU.add), reads=[h, hbt], writes=[hs])
                        p = pu[kc % 2]
                        proj(p[:], p, w_lg, kc * 128, hxT, 0, TB)
                        xs, x2, th = xs_[i], x2_[i], th_[i]
                        fw.op(act, lambda: S.copy(xs[:], p[:]), reads=[p], writes=[xs])
                        fw.op(act, lambda: S.activation(x2[:], p[:], AF.Square), reads=[p], writes=[x2])
                        fw.op(dve, lambda: V.tensor_scalar(x2[:], x2[:], GC0 * GC1, GC0, ALU.mult, ALU.add), reads=[x2], writes=[x2])
                        fw.op(dve, lambda: V.tensor_tensor(x2[:], x2[:], xs[:], ALU.mult), reads=[x2, xs], writes=[x2])
                        fw.op(act, lambda: S.activation(th[:], x2[:], AF.Tanh), reads=[x2], writes=[th])
                        fw.op(dve, lambda: V.scalar_tensor_tensor(th[:], th[:], 1.0, xs[:], ALU.add, ALU.mult), reads=[th, xs], writes=[th])
                        fw.op(dve, lambda: V.scalar_tensor_tensor(yb[:, kc, :], th[:], 0.5, hs[:], ALU.mult, ALU.mult),
                              reads=[th, hs], writes=[yb])
                        if nn is not None:
                            do_proj2(nn, nxt, kc)

                    def cba2(kc, gnx=gnx):
                        next(gnx, None); next(gnx, None)
                        if kc >= 3:
                            drain(gnx)
                    lru_group(lb, 0, [0, 1, 2, 3], ue, TB, stA, False, cb2, cba2)
                    lru_group(lb, 0, [4, 5, 6, 7], ue, TB, stA, False, cb2, cba2)
                    drain(gnx)
                    if nn is not None:
                        un = uext[nn % 2]
                        fw.op(pool, lambda: G.tensor_copy(un[:, :, 0:3], ue[:, :, TB:TB + 3]), reads=[ue], writes=[un])
                    fw.dma(pool, YI_d[:, :, n * TB:(n + 1) * TB], yb[:], yb, reads=[yb], writes=[YI_t[n]])
                fw.barrier()
            esL.close()
            fw.es = esM

            with ExitStack() as es2:
                fw.es = es2
                w_mb = fw.sb("w_mb", [128, 8, D], BF16); w_ob = fw.sb("w_ob", [128, 8, D], BF16)
                load_w(w_mb, win_d[:, 6 * D:7 * D], D); load_w(w_ob, woutb_d, D)
                hb = HxT(TB, "b2")
                pu = [fw.ps(f"pu{i}", [128, TB], F32) for i in range(2)]
                pyb = [fw.ps(f"pyb{i}", [128, TB], F32) for i in range(2)]
                ybin = [fw.sb(f"ybin{i}", [128, 8, TB], BF16) for i in range(2)]
                gyb = [fw.sb(f"gyb{i}", [128, 8, TB], BF16) for i in range(2)]
                sg_ = [fw.sb(f"gsg{i}", [128, TB], F32) for i in range(2)]
                for n, hxT, gnx in pipelined(hb, list(range(8)), TB):
                    next(pcg, None); next(pcg, None)
                    yb = ybin[n % 2]
                    fw.dma(sp, yb[:], YI_d[:, :, n * TB:(n + 1) * TB], yb, reads=[YI_t[n]], writes=[yb])
                    gy = gyb[n % 2]
                    for mo in range(8):
                        i = mo % 2
                        py = pyb[i]
                        for kc in range(8):
                            fw.op(pe, lambda: PE.matmul(py[:], w_ob[:, kc, mo * 128:(mo + 1) * 128], yb[:, kc, :],
                                                        start=(kc == 0), stop=(kc == 7)), reads=[w_ob, yb], writes=[py], inc=(kc == 7))
                        p = pu[i]
                        proj(p[:], p, w_mb, mo * 128, hxT, 0, TB)
                        sg = sg_[i]
                        fw.op(act, lambda: S.activation(sg[:], p[:], AF.Tanh, bias=sc[:, S_HBM + 8 + mo:S_HBM + 9 + mo], scale=0.5),
                              reads=[p, sc], writes=[sg])
                        fw.op(dve, lambda: V.scalar_tensor_tensor(gy[:, mo, :], sg[:], 1.0, py[:], ALU.add, ALU.mult),
                              reads=[sg, py], writes=[gy])
                        next(gnx, None)
                    fw.dma(pool, YB_d[:, :, n * TB:(n + 1) * TB], gy[:], gy, reads=[gy], writes=[YB_t[n]])
                fw.barrier()
            fw.es = esM


            with ExitStack() as es3:
                fw.es = es3
                w_gb = fw.sb("w_gb", [128, 8, D], BF16); w_gc = fw.sb("w_gc", [128, 8, D], BF16)
                w_xc = fw.sb("w_xc", [128, 8, D], BF16); w_oa = fw.sb("w_oa", [128, 8, D], BF16)
                load_w(w_gc, win_d[:, 1 * D:2 * D], D); load_w(w_xc, win_d[:, 2 * D:3 * D], D)
                load_w(w_gb, win_d[:, 0:D], D); load_w(w_oa, wouta_d, D)
                hb = HxT(TB + 64, "c")
                pext = [fw.sb(f"pext{i}", [128, 8, TB + 128], BF16) for i in range(2)]
                for u in pext:
                    fw.op(pool, lambda: G.memset(u[:], 0.0), writes=[u])
                pg = [fw.ps(f"pg{i}", [128, TB], F32) for i in range(2)]
                px = [fw.ps(f"px{i}", [128, TB], F32) for i in range(2)]
                ph = [fw.ps(f"ph{i}", [128, 128], F32) for i in range(2)]
                mk = lambda nm, dt, w=TB: [fw.sb(f"{nm}{i}", [128, w], dt) for i in range(2)]
                gcs, gch, q_ = mk("gcs", F32), mk("gch", F32, 64), mk("cq", F32)
                yain = [fw.sb(f"yain{i}", [128, 8, TB], BF16) for i in range(2)]
                yat = [fw.sb(f"yat{i}", [128, 8, TB], BF16) for i in range(2)]
                for n, hxT, gnx in pipelined(hb, list(range(8)), TB + 64):
                    next(pcg, None); next(pcg, None)
                    pe_t = pext[n % 2]; pprev = pext[(n + 1) % 2]
                    for kc in range(8):
                        i = kc % 2
                        proj(pg[i][:], pg[i], w_gc, kc * 128, hxT, 0, TB)
                        proj(px[i][:], px[i], w_xc, kc * 128, hxT, 0, TB)
                        fw.op(act, lambda: S.copy(gcs[i][:], pg[i][:]), reads=[pg[i]], writes=[gcs[i]])
                        fw.op(dve, lambda: V.tensor_tensor(pe_t[:, kc, 64:64 + TB], gcs[i][:], px[i][:], ALU.mult),
                              reads=[gcs[i], px[i]], writes=[pe_t])
                        if kc >= 4:
                            proj(ph[i][:, 0:64], ph[i], w_gc, kc * 128, hxT, TB, 64)
                            proj(ph[i][:, 64:128], ph[i], w_xc, kc * 128, hxT, TB, 64)
                            fw.op(act, lambda: S.copy(gch[i][:], ph[i][:, 0:64]), reads=[ph[i]], writes=[gch[i]])
                            fw.op(dve, lambda: V.tensor_tensor(pe_t[:, kc, 64 + TB:128 + TB], gch[i][:], ph[i][:, 64:128], ALU.mult),
                                  reads=[gch[i], ph[i]], writes=[pe_t])
                    if n > 0:
                        fw.op(pool, lambda: G.tensor_copy(pe_t[:, 4:8, 0:64], pprev[:, 4:8, TB:TB + 64]), reads=[pprev], writes=[pe_t])
                    else:
                        fw.op(pool, lambda: G.memset(pe_t[:, 4:8, 0:64], 0.0), writes=[pe_t])
                    ya = yain[n % 2]
                    for kc in range(8):
                        i = kc % 2
                        q = q_[i]
                        w0 = vT1[:, C_CAW + kc:C_CAW + kc + 1]; w1_ = vT1[:, C_CAW + 8 + kc:C_CAW + 9 + kc]
                        w2_ = vT1[:, C_CAW + 16 + kc:C_CAW + 17 + kc]
                        fw.op(dve, lambda: V.tensor_scalar(q[:], pe_t[:, kc, 64:64 + TB], w1_, None, ALU.mult), reads=[pe_t, vT1], writes=[q])
                        if kc < 4:
                            pb = pe_t.t[:, kc, 64:64 + TB].rearrange("p (r c) -> p r c", c=64)
                            qv = q.t[:].rearrange("p (r c) -> p r c", c=64)
                            fw.op(dve, lambda: V.scalar_tensor_tensor(qv[:, :, 1:64], pb[:, :, 0:63], w0, qv[:, :, 1:64], ALU.mult, ALU.add),
                                  reads=[pe_t, q, vT1], writes=[q])
                            fw.op(dve, lambda: V.scalar_tensor_tensor(qv[:, :, 0:63], pb[:, :, 1:64], w2_, qv[:, :, 0:63], ALU.mult, ALU.add),
                                  reads=[pe_t, q, vT1], writes=[q])
                        else:
                            fw.op(dve, lambda: V.scalar_tensor_tensor(q[:], pe_t[:, kc, 0:TB], w0, q[:], ALU.mult, ALU.add),
                                  reads=[pe_t, q, vT1], writes=[q])
                            fw.op(dve, lambda: V.scalar_tensor_tensor(q[:], pe_t[:, kc, 128:128 + TB], w2_, q[:], ALU.mult, ALU.add),
                                  reads=[pe_t, q, vT1], writes=[q])
                        proj(pg[i][:], pg[i], w_gb, kc * 128, hxT, 0, TB)
                        fw.op(dve, lambda: V.tensor_tensor(ya[:, kc, :], q[:], pg[i][:], ALU.mult), reads=[q, pg[i]], writes=[ya])
                        next(gnx, None)
                    yt = yat[n % 2]
                    for mo in range(8):
                        i = mo % 2
                        for kc in range(8):
                            fw.op(pe, lambda: PE.matmul(px[i][:], w_oa[:, kc, mo * 128:(mo + 1) * 128], ya[:, kc, :],
                                                        start=(kc == 0), stop=(kc == 7)), reads=[w_oa, ya], writes=[px[i]], inc=(kc == 7))
                        fw.op(act, lambda: S.copy(yt[:, mo, :], px[i][:]), reads=[px[i]], writes=[yt])
                    fw.dma(pool, YA_d[:, :, n * TB:(n + 1) * TB], yt[:], yt, reads=[yt], writes=[YA_t[n]])
                fw.barrier()
            fw.es = esM

            with ExitStack() as es4:
                fw.es = es4
                w_ma = fw.sb("w_ma", [128, 8, D], BF16); w_oo = fw.sb("w_oo", [128, 8, D], BF16)
                load_w(w_ma, win_d[:, 5 * D:6 * D], D); load_w(w_oo, wo_d, D)
                hb = HxT(TB, "d")
                pm = [fw.ps(f"pm{i}", [128, TB], F32) for i in range(2)]
                pmx = [fw.ps(f"pmx{i}", [128, TB], F32) for i in range(2)]
                yab = [fw.sb(f"yab{i}", [128, 8, TB], BF16) for i in range(2)]
                ybb = [fw.sb(f"ybb{i}", [128, 8, TB], BF16) for i in range(2)]
                mg = [fw.sb(f"mg{i}", [128, 8, TB], BF16) for i in range(2)]
                mk = lambda nm, dt, w=TB: [fw.sb(f"{nm}{i}", [128, w], dt) for i in range(2)]
                sga, tt_ = mk("sga", F32), mk("mtt", F32)
                xr = [fw.sb(f"xr{i}", [128, D], F32) for i in range(2)]
                x1o = [fw.sb(f"x1o{i}", [128, D], F32) for i in range(2)]
                for n, hxT, gnx in pipelined(hb, list(range(8)), TB):
                    next(pcg, None); next(pcg, None)
                    ya = yab[n % 2]; yb = ybb[n % 2]; m = mg[n % 2]
                    fw.dma(sp, ya[:], YA_d[:, :, n * TB:(n + 1) * TB], ya, reads=[YA_t[n]], writes=[ya])
                    fw.dma(sp, yb[:], YB_d[:, :, n * TB:(n + 1) * TB], yb, reads=[YB_t[n]], writes=[yb])
                    for mo in range(8):
                        i = mo % 2
                        proj(pm[i][:], pm[i], w_ma, mo * 128, hxT, 0, TB)
                        fw.op(act, lambda: S.activation(sga[i][:], pm[i][:], AF.Tanh, bias=sc[:, S_HBM + mo:S_HBM + mo + 1], scale=0.5),
                              reads=[pm[i], sc], writes=[sga[i]])
                        fw.op(dve, lambda: V.scalar_tensor_tensor(tt_[i][:], sga[i][:], 1.0, ya[:, mo, :], ALU.add, ALU.mult),
                              reads=[sga[i], ya], writes=[tt_[i]])
                        fw.op(dve, lambda: V.tensor_tensor(m[:, mo, :], tt_[i][:], yb[:, mo, :], ALU.add), reads=[tt_[i], yb], writes=[m])
                        next(gnx, None)
                    for tt in range(4):
                        tile_i = n * 4 + tt
                        xt = xr[tt % 2]; xo = x1o[tt % 2]
                        fw.dma(sp, xt[:], x_d[tile_i * 128:(tile_i + 1) * 128, :], xt, writes=[xt])
                        for hh in range(2):
                            p = pmx[hh]
                            for kc in range(8):
                                fw.op(pe, lambda: PE.matmul(p[:], m[:, kc, tt * 128:(tt + 1) * 128], w_oo[:, kc, hh * 512:(hh + 1) * 512],
                                                            start=(kc == 0), stop=(kc == 7)), reads=[m, w_oo], writes=[p], inc=(kc == 7))
                            cs = slice(hh * 512, hh * 512 + 512)
                            fw.op(dve, lambda: V.tensor_tensor(xo[:, cs], p[:], HG1[:, cs], ALU.mult), reads=[p, HG1], writes=[xo])
                            fw.op(pool, lambda: G.tensor_tensor(xo[:, cs], xo[:, cs], xt[:, cs], ALU.add), reads=[xo, xt], writes=[xo])
                        fw.dma(pool, X1_d[tile_i * 128:(tile_i + 1) * 128, :], xo[:], xo, reads=[xo], writes=[X1_t[tile_i]])
                fw.barrier()
            fw.es = esM
        fw.es = es0

        with ExitStack() as esE:
          if STAGE == "full":
            fw.es = esE
            NS = 64
            I32 = mybir.dt.int32
            for _ in pcg:
                pass
            XS_t = T(None, "XSall"); FXS_t = [T(None, f"FXS{n}") for n in range(32)]
            YS_t = [T(None, f"YS{n}") for n in range(NS)]
            rw = fw.sb("rw", [128, 8, NEXP], F32)
            rbb = fw.sb("rbb", [128, NEXP], F32)
            b2s = fw.sb("b2s", [NEXP, D], F32)
            LG = fw.sb("LG", [128, 32, NEXP], F32); RANK = fw.sb("RANK", [128, 32, NEXP], F32)
            GD = fw.sb("GD", [128, 32, NEXP], F32); MX8 = fw.sb("MX8", [128, 32, 8], F32)
            G4h = fw.sb("G4h", [128, 32, 4], F32); POS4f = fw.sb("POS4f", [128, 32, 4], F32)
            POS4 = fw.sb("POS4", [128, 32, 4], I32)
            cnt = fw.sb("cnt", [128, NEXP], F32); pcn = fw.sb("pcn", [128, NEXP], F32)
            pend = fw.sb("pend", [128, NEXP], F32); pstart = fw.sb("pstart", [128, NEXP], F32)
            esl = fw.sb("esl", [128, NS], F32); wfl = fw.sb("wfl", [128, NS], F32)
            widx = fw.sb("widx", [128, NS], I32); widx2 = fw.sb("widx2", [128, NS], I32); eidx = fw.sb("eidx", [128, NS], I32)
            Ustr = fw.sb("Ustr", [128, 128], F32); iop = fw.sb("iop", [128, 1], F32)
            onesb = fw.sb("onesb", [1, TB], BF16)
            j32 = fw.sb("j32", [128, NEXP], F32)
            fw.dma(sp, rw[:], rw_d.rearrange("(kc p) e -> p kc e", p=128), rw, writes=[rw])
            fw.dma(sp, rbb[:], rb_d[0:1, :].partition_broadcast(128), rbb, writes=[rbb])
            fw.dma(sp, b2s[:], b2_d, b2s, writes=[b2s])
            B1T_t = T(None, "B1Tt")
            with ExitStack() as esb:
                fw.es = esb
                b1r = fw.sb("b1r", [NEXP, 2 * D], F32)
                pb1 = fw.ps("pb1", [128, 16 * NEXP], F32)
                b1Te = fw.sb("b1Te", [128, NEXP, 16], F32)
                fw.dma(sp, b1r[:], b1_d, b1r, writes=[b1r])
                b1v_ = b1r.t[:].rearrange("e (j p two) -> e j two p", p=128, two=2)
                for j in range(8):
                    for two in range(2):
                        s_ = j * 2 + two
                        fw.op(pe, lambda: PE.transpose(pb1[:, s_ * NEXP:(s_ + 1) * NEXP], b1v_[:, j, two, :], identf[0:NEXP, 0:NEXP]),
                              reads=[b1r, identf], writes=[pb1], inc=(s_ == 15))
                fw.op(dve, lambda: V.tensor_copy(b1Te.t[:].rearrange("p e s -> p s e"), pb1.t[:].rearrange("p (s e) -> p s e", e=NEXP)),
                      reads=[pb1], writes=[b1Te])
                fw.dma(sp, B1T_d.rearrange("(e p) s -> p e s", p=128), b1Te[:], b1Te, reads=[b1Te], writes=[B1T_t])
                fw.barrier()
            fw.es = esE
            fw.op(pool, lambda: G.memset(Ustr[:], 1.0), writes=[Ustr])
            fw.op(pool, lambda: G.affine_select(Ustr[:], Ustr[:], [[1, 128]], ALU.is_gt, 0.0, base=0, channel_multiplier=-1),
                  reads=[Ustr], writes=[Ustr])
            fw.op(pool, lambda: G.iota(iop[:], [[0, 1]], base=0, channel_multiplier=1, allow_small_or_imprecise_dtypes=True), writes=[iop])
            fw.op(pool, lambda: G.memset(onesb[:], 1.0), writes=[onesb])
            fw.op(pool, lambda: G.memset(cnt[:], 0.0), writes=[cnt])

            def idma(out, in_, semt, in_off=None, out_off=None, eoff=0, reads=(), writes=()):
                pool.wait(fw._deps(reads, writes))
                if semt.dsem is None:
                    semt.dsem = fw.newsem("d_" + semt.name)
                    fw.dtiles.append(semt)
                inst = G.indirect_dma_start(out=out, out_offset=out_off, in_=in_, in_offset=in_off, element_offset=eoff)
                semt.dcnt += 16
                inst.then_inc(semt.dsem.h, 16)
                d = (semt.dsem, semt.dcnt)
                for t in reads:
                    t.r[semt.dsem.id] = d
                for t in writes:
                    t.w = d; t.r = {}

            with ExitStack() as esR:
                fw.es = esR
                G2 = fw.sb("G2", [128, D], F32); SH2 = fw.sb("SH2", [128, D], F32)
                for i_, t_ in enumerate((G2, SH2)):
                    fw.dma(sp, t_[:], MB_d[i_], t_, reads=[MB_t], writes=[t_])
                zt = fw.sb("zt", [128, 8192], BF16)
                fw.op(pool, lambda: G.memset(zt[:], 0.0), writes=[zt])
                XSv = XS_d.rearrange("(a p r) n -> a p (r n)", p=128, r=8)
                for a_ in range(NS * TB // 1024):
                    fw.dma(act, XSv[a_], zt[:], zt, reads=[zt])
                XS_t.w = (zt.dsem, zt.dcnt)
                nbf = NormBufs(F32, "f")
                ptf = fw.ps("ptf", [128, D], F32)
                fxf = [fw.sb(f"fxf{i}", [128, 8, 128], F32) for i in range(2)]
                fxb = [fw.sb(f"fxb{i}", [128, D], BF16) for i in range(2)]
                plg = fw.ps("plg", [128, 128], F32); prk = fw.ps("prk", [128, 128], F32); pcn_ = fw.ps("pcnp", [128, 128], F32)
                nmx_ = fw.sb("nmx_", [128, 1], F32); msk = fw.sb("msk", [128, NEXP], F32); ex = fw.sb("ex", [128, NEXP], F32)
                den = fw.sb("den", [128, 1], F32); e4 = fw.sb("e4", [128, 4], F32); den4 = fw.sb("den4", [128, 1], F32)
                fx_next, _ = norm_tile(nbf, X1_d[0:128, :], 128, G2, SH2, src_dep=[X1_t[0]])
                for ti in range(32):
                    fx = fx_next
                    if ti + 1 < 32:
                        fx_next, _ = norm_tile(nbf, X1_d[(ti + 1) * 128:(ti + 2) * 128, :], 128, G2, SH2, src_dep=[X1_t[ti + 1]])
                    fb = fxb[ti % 2]
                    fw.op(pool, lambda: G.tensor_copy(fb[:], fx[:]), reads=[fx], writes=[fb])
                    fw.dma(sp, FXS_d[ti * 128:(ti + 1) * 128, :], fb[:], fb, reads=[fb], writes=[FXS_t[ti]])
                    for kc in range(8):
                        fw.op(pe, lambda: PE.transpose(ptf[:, kc * 128:(kc + 1) * 128], fx[:, kc * 128:(kc + 1) * 128], identf[:]),
                              reads=[fx, identf], writes=[ptf], inc=(kc == 7))
                    ff = fxf[ti % 2]
                    fw.op(act, lambda: S.copy(ff.t[:].rearrange("p k n -> p (k n)"), ptf[:]), reads=[ptf], writes=[ff])
                    for kc in range(8):
                        fw.op(pe, lambda: PE.matmul(plg[:, 0:NEXP], ff[:, kc, :], rw[:, kc, :], start=(kc == 0), stop=(kc == 7)),
                              reads=[ff, rw], writes=[plg], inc=(kc == 7))
                    lg = LG[:, ti, :]; mx8 = MX8[:, ti, :]
                    fw.op(dve, lambda: V.tensor_tensor(lg, plg[:, 0:NEXP], rbb[:], ALU.add), reads=[plg, rbb], writes=[LG])
                    fw.op(dve, lambda: V.max(mx8, lg), reads=[LG], writes=[MX8])
                    fw.op(dve, lambda: V.tensor_scalar(msk[:], lg, MX8[:, ti, 3:4], None, ALU.is_ge), reads=[LG, MX8], writes=[msk])
                    fw.op(dve, lambda: V.tensor_scalar(nmx_[:], MX8[:, ti, 0:1], -1.0, None, ALU.mult), reads=[MX8], writes=[nmx_])
                    fw.op(act, lambda: S.activation(ex[:], lg, AF.Exp, bias=nmx_[:, 0:1], scale=1.0), reads=[LG, nmx_], writes=[ex])
                    fw.op(act, lambda: S.activation(e4[:], MX8[:, ti, 0:4], AF.Exp, bias=nmx_[:, 0:1], scale=1.0, accum_out=den4[:]),
                          reads=[MX8, nmx_], writes=[e4, den4])
                    fw.op(dve, lambda: V.reciprocal(den[:], den4[:]), reads=[den4], writes=[den])
                    fw.op(dve, lambda: V.tensor_scalar(G4h[:, ti, :], e4[:], den[:, 0:1], 0.5, ALU.mult, ALU.mult), reads=[e4, den], writes=[G4h])
                    fw.op(dve, lambda: V.scalar_tensor_tensor(GD[:, ti, :], ex[:], den[:, 0:1], msk[:], ALU.mult, ALU.mult),
                          reads=[ex, den, msk], writes=[GD])
                    fw.op(pe, lambda: PE.matmul(prk[:, 0:NEXP], Ustr[:], msk[:], start=True, stop=True), reads=[Ustr, msk], writes=[prk])
                    fw.op(pe, lambda: PE.matmul(pcn_[:, 0:NEXP], ones[:], msk[:], start=True, stop=True), reads=[ones, msk], writes=[pcn_])
                    fw.op(dve, lambda: V.tensor_tensor(RANK[:, ti, :], prk[:, 0:NEXP], cnt[:], ALU.add), reads=[prk, cnt], writes=[RANK])
                    fw.op(dve, lambda: V.tensor_tensor(cnt[:], pcn_[:, 0:NEXP], cnt[:], ALU.add), reads=[pcn_, cnt], writes=[cnt])
                fw.op(dve, lambda: V.tensor_scalar(pcn[:], cnt[:], 0.0, None, ALU.is_gt), reads=[cnt], writes=[pcn])
                for j in range(1, 8):
                    fw.op(dve, lambda: V.scalar_tensor_tensor(pcn[:], cnt[:], 512.0 * j, pcn[:], ALU.is_gt, ALU.add), reads=[cnt, pcn], writes=[pcn])
                fw.op(dve, lambda: V.tensor_scalar(pcn[:], pcn[:], 512.0, None, ALU.mult), reads=[pcn], writes=[pcn])
                fw.op(dve, lambda: V.tensor_tensor_scan(pend[:], ones[:, 0:NEXP], pcn[:], 0.0, ALU.mult, ALU.add), reads=[ones, pcn], writes=[pend])
                fw.op(dve, lambda: V.tensor_tensor(pstart[:], pend[:], pcn[:], ALU.subtract), reads=[pend, pcn], writes=[pstart])
                for s_ in range(NS):
                    fw.op(dve, lambda: V.tensor_scalar(j32[:], pend[:], 512.0 * s_, 0.0, ALU.is_le, ALU.add, accum_out=esl[:, s_:s_ + 1]),
                          reads=[pend], writes=[j32, esl])
                fw.op(dve, lambda: V.tensor_scalar(esl[:], esl[:], float(NEXP - 1), None, ALU.min), reads=[esl], writes=[esl])
                fw.op(dve, lambda: V.tensor_copy(eidx[:], esl[:]), reads=[esl], writes=[eidx])
                fw.op(dve, lambda: V.tensor_scalar(wfl[:], esl[:], 128.0, iop[:, 0:1], ALU.mult, ALU.add), reads=[esl, iop], writes=[wfl])
                fw.op(dve, lambda: V.tensor_copy(widx[:], wfl[:]), reads=[wfl], writes=[widx])
                posf = fw.sb("posf", [128, NEXP], F32)
                fbsc = [T(None, f"fbsc{i}") for i in range(2)]
                for ti in range(32):
                    fw.op(dve, lambda: V.tensor_tensor(posf[:], RANK[:, ti, :], pstart[:], ALU.add), reads=[RANK, pstart], writes=[posf])
                    for k in range(4):
                        fw.op(dve, lambda: V.scalar_tensor_tensor(j32[:], LG[:, ti, :], MX8[:, ti, k:k + 1], posf[:], ALU.is_equal, ALU.mult,
                                                                  accum_out=POS4f[:, ti, k:k + 1]), reads=[LG, MX8, posf], writes=[j32, POS4f])
                fw.op(dve, lambda: V.tensor_copy(POS4[:], POS4f[:]), reads=[POS4f], writes=[POS4])
                for ti in range(32):
                    fb = fxb[ti % 2]
                    fw.dma(sp, fb[:], FXS_d[ti * 128:(ti + 1) * 128, :], fb, reads=[FXS_t[ti]], writes=[fb])
                    for k in range(4):
                        idma(XS_d, fb[:], fbsc[ti % 2], out_off=bass.IndirectOffsetOnAxis(ap=POS4[:, ti, k:k + 1], axis=0), reads=[fb, POS4, XS_t])
                fw.barrier()
            fw.es = esE

            with ExitStack() as esS:
                fw.es = esS
                w1s = [fw.sb(f"w1s{i}", [128, 8, 2 * D], BF16) for i in range(2)]
                w2s = [fw.sb(f"w2s{i}", [128, 8, D], BF16) for i in range(2)]
                b1s = [fw.sb(f"b1s{i}", [128, 16], F32) for i in range(2)]
                xr = [fw.sb(f"xr{i}", [128, 4, D], BF16) for i in range(2)]
                xT = [fw.sb(f"xT{i}", [128, 8, TB], BF16) for i in range(2)]
                ptr = [fw.ps(f"ptrE{i}", [128, D], BF16) for i in range(2)]
                pgl = [fw.ps(f"pgl{i}", [128, TB], F32) for i in range(2)]
                pyy = [fw.ps(f"pyy{i}", [128, TB], F32) for i in range(2)]
                mk = lambda nm, dt: [fw.sb(f"{nm}{i}", [128, TB], dt) for i in range(2)]
                gl_, th_, l1_ = mk("egl", F32), mk("eth", F32), mk("el1", F32)
                actT = [fw.sb(f"actT{i}", [128, 8, TB], BF16) for i in range(2)]
                ysb = [fw.sb(f"ysb{i}", [128, D], F32) for i in range(2)]
                w1f = WB1_d.rearrange("e p n -> (e p) n"); w2f = WB2_d.rearrange("e p n -> (e p) n")
                pool.wait([(wbsem, wbcnt[0])])

                def load_slot(s_):
                    ws = s_ % 2
                    off = bass.IndirectOffsetOnAxis(ap=widx[:, s_:s_ + 1], axis=0)
                    idma(b1s[ws][:], B1T_d, b1s[ws], in_off=off, reads=[widx, B1T_t], writes=[b1s[ws]])
                    idma(w1s[ws].t[:].rearrange("p k n -> p (k n)"), w1f, w1s[ws], in_off=off, reads=[widx], writes=[w1s[ws]])
                    idma(w2s[ws].t[:].rearrange("p k n -> p (k n)"), w2f, w2s[ws], in_off=off, reads=[widx], writes=[w2s[ws]])
                    fw.dma(sp, xr[ws][:], XS_d[s_ * TB:(s_ + 1) * TB, :].rearrange("(t p) n -> p t n", p=128), xr[ws], writes=[xr[ws]])

                def slot_transposes(sx):
                    xtx = xT[sx % 2]; xrx = xr[sx % 2]
                    for t in range(4):
                        p = ptr[t % 2]
                        for kc in range(8):
                            fw.op(pe, lambda: PE.transpose(p[:, kc * 128:(kc + 1) * 128], xrx[:, t, kc * 128:(kc + 1) * 128], identb[:]),
                                  reads=[xrx, identb], writes=[p], inc=(kc == 7))
                        fw.op(act, lambda: S.copy(xtx[:, :, t * 128:(t + 1) * 128], p.t[:].rearrange("p (k n) -> p k n", k=8)),
                              reads=[p], writes=[xtx])

                load_slot(0)
                slot_transposes(0)
                for s_ in range(NS):
                    ws = s_ % 2
                    if s_ + 1 < NS:
                        load_slot(s_ + 1)
                    xt_ = xT[ws]; xr_ = xr[ws]
                    at = actT[ws]
                    w1v = w1s[ws].t[:].rearrange("p k (m two) -> p k m two", two=2)
                    for j in range(8):
                        i = j % 2
                        for two in range(2):
                            p = pgl[two]
                            for kc in range(8):
                                fw.op(pe, lambda: PE.matmul(p[:], w1v[:, kc, j * 128:(j + 1) * 128, two], xt_[:, kc, :],
                                                            start=(kc == 0), stop=(kc == 7)), reads=[w1s[ws], xt_], writes=[p], inc=(kc == 7))
                        gl, th, l1 = gl_[i], th_[i], l1_[i]
                        fw.op(dve, lambda: V.tensor_scalar(gl[:], pgl[0][:], b1s[ws][:, 2 * j:2 * j + 1], 7.0, ALU.add, ALU.min),
                              reads=[pgl[0], b1s[ws]], writes=[gl])
                        fw.op(dve, lambda: V.tensor_scalar(l1[:], pgl[1][:], b1s[ws][:, 2 * j + 1:2 * j + 2], 7.0, ALU.add, ALU.min),
                              reads=[pgl[1], b1s[ws]], writes=[l1])
                        fw.op(act, lambda: S.activation(th[:], gl[:], AF.Tanh, scale=0.851), reads=[gl], writes=[th])
                        fw.op(act, lambda: S.activation(l1[:], l1[:], AF.Relu, bias=7.0, scale=1.0), reads=[l1], writes=[l1])
                        fw.op(dve, lambda: V.scalar_tensor_tensor(th[:], th[:], 1.0, gl[:], ALU.add, ALU.mult), reads=[th, gl], writes=[th])
                        fw.op(dve, lambda: V.scalar_tensor_tensor(at[:, j, :], l1[:], -6.0, th[:], ALU.add, ALU.mult),
                              reads=[l1, th], writes=[at])
                    if s_ + 1 < NS:
                        slot_transposes(s_ + 1)
                    for tt in range(4):
                        yb_ = ysb[tt % 2]
                        for hh in range(2):
                            py = pyy[hh]
                            for kc in range(8):
                                fw.op(pe, lambda: PE.matmul(py[:], at[:, kc, tt * 128:(tt + 1) * 128], w2s[ws][:, kc, hh * 512:(hh + 1) * 512],
                                                            start=(kc == 0), stop=(kc == 7)), reads=[at, w2s[ws]], writes=[py], inc=(kc == 7))
                            fw.op(act, lambda: S.copy(yb_[:, hh * 512:(hh + 1) * 512], py[:]), reads=[py], writes=[yb_])
                        r0 = s_ * TB + tt * 128
                        fw.dma(sp, YS_d[r0:r0 + 128, :], yb_[:], yb_, reads=[yb_], writes=[YS_t[s_]])
                fw.barrier()
            fw.es = esE

            with ExitStack() as esC2:
                fw.es = esC2
                GATE2 = fw.sb("GATE2", [128, D], F32); NF = fw.sb("NF", [128, D], F32)
                fw.dma(sp, GATE2[:], MB_d[2], GATE2, reads=[MB_t], writes=[GATE2])
                fw.dma(sp, NF[:], nfin_d[0:1, :].partition_broadcast(128), NF, writes=[NF])
                plg = fw.ps("plgC", [128, 128], F32)
                pyy = [fw.ps(f"pyyC{i}", [128, TB], F32) for i in range(2)]
                ghT = [fw.sb(f"ghT{i}", [NEXP, 128], F32) for i in range(2)]
                yk = [fw.sb(f"yk{i}", [128, D], F32) for i in range(8)]
                accs = [fw.sb(f"accs{i}", [128, D], F32) for i in range(2)]
                x1r = [fw.sb(f"x1r{i}", [128, D], F32) for i in range(2)]
                junk = fw.sb("junkC", [128, D], BF16)
                ssC = [fw.sb(f"ssC{i}", [128, 1], F32) for i in range(2)]
                rsC = [fw.sb(f"rsC{i}", [128, 1], F32) for i in range(2)]
                def cgather(ti):
                    for k in range(4):
                        y_ = yk[(ti % 2) * 4 + k]
                        idma(y_[:], YS_d, y_, in_off=bass.IndirectOffsetOnAxis(ap=POS4[:, ti, k:k + 1], axis=0), reads=[POS4] + YS_t, writes=[y_])
                cgather(0)
                for ti in range(32):
                    i = ti % 2
                    if ti + 1 < 32:
                        cgather(ti + 1)
                    ac = accs[i]; xo = x1r[i]; ss = ssC[i]; rs = rsC[i]; gT = ghT[i]
                    fw.op(pe, lambda: PE.transpose(plg[0:NEXP, :], GD[:, ti, :], identf[:]), reads=[GD, identf], writes=[plg])
                    fw.op(act, lambda: S.copy(gT[:], plg[0:NEXP, :]), reads=[plg], writes=[gT])
                    for hh in range(2):
                        py = pyy[hh]
                        fw.op(pe, lambda: PE.matmul(py[:], gT[:], b2s[:, hh * 512:(hh + 1) * 512], start=True, stop=True),
                              reads=[gT, b2s], writes=[py])
                        fw.op(act, lambda: S.copy(ac[:, hh * 512:(hh + 1) * 512], py[:]), reads=[py], writes=[ac])
                    fw.dma(sp, xo[:], X1_d[ti * 128:(ti + 1) * 128, :], xo, reads=[X1_t[ti]], writes=[xo])
                    for k in range(4):
                        y_ = yk[(ti % 2) * 4 + k]
                        fw.op(dve, lambda: V.scalar_tensor_tensor(ac[:], y_[:], G4h[:, ti, k:k + 1], ac[:], ALU.mult, ALU.add),
                              reads=[y_, G4h, ac], writes=[ac])
                    fw.op(dve, lambda: V.tensor_tensor(ac[:], ac[:], GATE2[:], ALU.mult), reads=[ac, GATE2], writes=[ac])
                    fw.op(pool, lambda: G.tensor_tensor(ac[:], ac[:], xo[:], ALU.add), reads=[ac, xo], writes=[ac])
                    fw.op(act, lambda: S.activation(junk[:], ac[:], AF.Square, scale=1.0 / 32.0, accum_out=ss[:]), reads=[ac], writes=[junk, ss])
                    fw.op(pool, lambda: G.tensor_scalar(rs[:], ss[:], EPS, None, ALU.add), reads=[ss], writes=[rs])
                    fw.op(pool, lambda: G.tensor_tensor(rs[:], rs[:], mhalf[:], ALU.pow), reads=[rs, mhalf], writes=[rs])
                    fw.op(dve, lambda: V.scalar_tensor_tensor(xo[:], ac[:], rs[:, 0:1], NF[:], ALU.mult, ALU.mult), reads=[ac, rs, NF], writes=[xo])
                    fw.dma(sp, out_d[ti * 128:(ti + 1) * 128, :], xo[:], xo, reads=[xo])
                fw.barrier()
            fw.es = esE
            fw.barrier()
        fw.es = es0

    return nc


_NC = None


def kernel(**inp):
    global _NC
    f = lambda a: np.ascontiguousarray(np.asarray(a, dtype=np.float32))
    def wl2(a):
        a = np.asarray(a, dtype=np.float32)
        E_ = a.shape[0]
        return np.ascontiguousarray(a.reshape(E_, 4, 2, 128, D).transpose(0, 1, 3, 2, 4)).reshape(E_, 512, 2 * D)
    x = f(inp["x"]); ctx = f(inp["ctx"]); c = f(inp["c"]); c_ctx = f(inp["c_ctx"])
    if _NC is None:
        _NC = build_nc()
    nc = _NC
    shared = {
        "w_ada": f(inp["w_ada"][0]), "b_ada": f(inp["b_ada"][0]).reshape(1, -1),
        "norm_mix": f(inp["norm_mix"][0]).reshape(1, -1), "w_in": f(inp["w_in"][0]),
        "w_out_a": f(inp["w_out_a"][0]), "w_out_b": f(inp["w_out_b"][0]), "b_merge": f(inp["b_merge"][0]),
        "w_o": f(inp["w_o"][0]), "norm_ffn": f(inp["norm_ffn"][0]).reshape(1, -1),
        "router_w": f(inp["router_w"][0]), "router_b": f(inp["router_b"][0]).reshape(1, -1),
        "w1": f(inp["w1"][0] if STAGE == "full" else inp["w1"][0][:1]), "b1": f(inp["b1"][0]),
        "w2": f(inp["w2"][0] if STAGE == "full" else inp["w2"][0][:1]), "b2": f(inp["b2"][0]),
        "norm_final": f(inp["norm_final"]).reshape(1, -1),
    }
    lru_keys = ["lru_conv_w", "lru_conv_b", "lru_wa", "lru_ba", "lru_wx", "lru_bx", "lru_lambda"]
    in_maps = []
    for k in CORES:
        b, half = k // 2, k % 2
        m = dict(shared)
        if half == 0:
            m["x"] = x[b]; m["ctx"] = ctx[b]
            m["conv_a_w"] = f(inp["conv_a_w"][0])
            for key in lru_keys:
                m[key] = f(inp[key][0])
        else:
            m["x"] = np.ascontiguousarray(x[b][::-1]); m["ctx"] = np.ascontiguousarray(ctx[b][::-1])
            m["conv_a_w"] = np.ascontiguousarray(f(inp["conv_a_w"][0])[::-1])
            for key in lru_keys:
                m[key] = np.ascontiguousarray(f(inp[key][0])[::-1])
        m["cvec"] = np.ascontiguousarray(np.stack([c[b], c_ctx], axis=0))
        in_maps.append(m)
    res = run_bass_kernel_spmd(nc, in_maps, core_ids=list(range(len(CORES))))
    out = np.zeros((4, NTOK, D), np.float32)
    for i_, k in enumerate(CORES):
        b, half = k // 2, k % 2
        o = np.asarray(res.results[i_]["out"], dtype=np.float32)
        if half == 0:
            out[b, :OWN] = o
        else:
            out[b, OWN:] = o[::-1]
    if DEBUG:
        kernel.debug = res.results
    return out
```

```python
import numpy as np
from contextlib import ExitStack
import concourse.bass as bass
import concourse.mybir as mybir
from concourse.bass_utils import run_bass_kernel_spmd

F32 = mybir.dt.float32
BF16 = mybir.dt.bfloat16
AF = mybir.ActivationFunctionType
ALU = mybir.AluOpType

D = 1024
NTOK = 8192
OWN = 4096
TB = 512
NEXP = 32
EPS = 1e-6
GC0 = 0.7978845608028654
GC1 = 0.044715
DEBUG = False
STAGE = "full"
CORES = list(range(8))


class T:
    def __init__(self, t, name):
        self.t = t; self.name = name
        self.w = None; self.r = {}
        self.dsem = None; self.dcnt = 0

    def __getitem__(self, k):
        return self.t[k]


class Sem:
    uid = 0

    def __init__(self, h):
        self.h = h
        Sem.uid += 1
        self.id = Sem.uid


class E:
    def __init__(self, fw, name, eng):
        self.fw = fw; self.name = name; self.eng = eng
        self.sem = fw.newsem("e_" + name, fw.es0); self.count = 0
        self.seen = {}

    def wait(self, deps):
        best = {}
        for (s, v) in deps:
            if s.id not in best or best[s.id][1] < v:
                best[s.id] = (s, v)
        for k, (s, v) in best.items():
            if s is self.sem and self.name == "pe":
                continue
            if self.seen.get(k, 0) >= v:
                continue
            self.eng.wait_ge(s.h, v)
            self.seen[k] = v


class FW:
    def __init__(self, nc, es0):
        self.nc = nc; self.es0 = es0; self.es = es0
        self.nname = 0
        self.pe = E(self, "pe", nc.tensor)
        self.act = E(self, "act", nc.scalar)
        self.dve = E(self, "dve", nc.vector)
        self.pool = E(self, "pool", nc.gpsimd)
        self.sp = E(self, "sp", nc.sync)
        self.engs = [self.pe, self.act, self.dve, self.pool, self.sp]
        self.dtiles = []
        self.nname = 0

    def newsem(self, name, es=None):
        self.nname += 1
        return Sem((es or self.es).enter_context(self.nc.semaphore(f"{name}_{self.nname}")))

    def sb(self, name, shape, dt):
        self.nname += 1
        return T(self.es.enter_context(self.nc.sbuf_tensor(f"{name}_{self.nname}", shape, dt)), name)

    def ps(self, name, shape, dt):
        self.nname += 1
        return T(self.es.enter_context(self.nc.psum_tensor(f"{name}_{self.nname}", shape, dt)), name)

    def _deps(self, reads, writes):
        deps = []
        for t in reads:
            if t.w: deps.append(t.w)
        for t in writes:
            if t.w: deps.append(t.w)
            deps += list(t.r.values())
        return deps

    def op(self, e, fn, reads=(), writes=(), inc=True):
        e.wait(self._deps(reads, writes))
        inst = fn()
        if inc:
            e.count += 1
            inst.then_inc(e.sem.h, 1)
            d = (e.sem, e.count)
        else:
            d = (e.sem, e.count + 1)
        for t in reads:
            t.r[e.sem.id] = d
        for t in writes:
            t.w = d; t.r = {}
        return inst

    def dma(self, q, out, in_, semt, reads=(), writes=(), **kw):
        q.wait(self._deps(reads, writes))
        if semt.dsem is None:
            semt.dsem = self.newsem("d_" + semt.name)
            self.dtiles.append(semt)
        inst = q.eng.dma_start(out=out, in_=in_, **kw)
        semt.dcnt += 16
        inst.then_inc(semt.dsem.h, 16)
        d = (semt.dsem, semt.dcnt)
        for t in reads:
            t.r[semt.dsem.id] = d
        for t in writes:
            t.w = d; t.r = {}
        return inst

    def barrier(self):
        deps = [(e.sem, e.count) for e in self.engs if e.count > 0]
        deps += [(t.dsem, t.dcnt) for t in self.dtiles if t.dcnt > 0]
        for e in self.engs:
            e.wait(deps)
        self.dtiles = []


def build_nc():
    nc = bass.Bass("TRN2", target_bir_lowering=False)

    def din(name, shape):
        return nc.dram_tensor(name, shape, F32, kind="ExternalInput").ap()

    x_d = din("x", [NTOK, D]); ctx_d = din("ctx", [256, D]); cv_d = din("cvec", [2, D])
    wada_d = din("w_ada", [D, 6 * D]); bada_d = din("b_ada", [1, 6 * D])
    nmix_d = din("norm_mix", [1, D]); win_d = din("w_in", [D, 7 * D])
    caw_d = din("conv_a_w", [3, D]); wouta_d = din("w_out_a", [D, D])
    lcw_d = din("lru_conv_w", [2, 4, D]); lcb_d = din("lru_conv_b", [2, D])
    lwa_d = din("lru_wa", [2, 16, 64, 64]); lba_d = din("lru_ba", [2, D])
    lwx_d = din("lru_wx", [2, 16, 64, 64]); lbx_d = din("lru_bx", [2, D])
    lam_d = din("lru_lambda", [2, D]); woutb_d = din("w_out_b", [D, D])
    bm_d = din("b_merge", [2, D]); wo_d = din("w_o", [D, D]); nffn_d = din("norm_ffn", [1, D])
    rw_d = din("router_w", [D, NEXP]); rb_d = din("router_b", [1, NEXP])
    NE_ = NEXP if STAGE == "full" else 1
    w1_d = din("w1", [NE_, D, 2 * D]); b1_d = din("b1", [NEXP, 2 * D])
    w2_d = din("w2", [NE_, D, D]); b2_d = din("b2", [NEXP, D]); nfin_d = din("norm_final", [1, D])
    out_d = nc.dram_tensor("out", [OWN, D], F32, kind="ExternalOutput").ap()
    HB_d = nc.dram_tensor("HB", [128, 8, OWN], BF16).ap()
    YB_d = nc.dram_tensor("YB", [128, 8, OWN], BF16).ap()
    YA_d = nc.dram_tensor("YA", [128, 8, OWN], BF16).ap()
    YI_d = nc.dram_tensor("YI", [128, 8, OWN], BF16).ap()
    MB_d = nc.dram_tensor("MB", [3, 128, D], F32).ap()
    XS_d = nc.dram_tensor("XS", [64 * 512, D], BF16).ap()
    YS_d = nc.dram_tensor("YS", [64 * 512, D], F32).ap()
    FXS_d = nc.dram_tensor("FXS", [OWN, D], BF16).ap()
    WB1_d = nc.dram_tensor("WB1", [NEXP, 128, 8 * 2 * D], BF16).ap()
    WB2_d = nc.dram_tensor("WB2", [NEXP, 128, 8 * D], BF16).ap()
    B1T_d = nc.dram_tensor("B1T", [NEXP * 128, 16], F32).ap()
    if DEBUG:
        X1_d = nc.dram_tensor("X1", [OWN, D], F32, kind="ExternalOutput").ap()
    else:
        X1_d = nc.dram_tensor("X1", [OWN, D], F32).ap()

    with ExitStack() as es0:
        fw = FW(nc, es0)
        es0.enter_context(nc.Block())
        pe, act, dve, pool, sp = fw.pe, fw.act, fw.dve, fw.pool, fw.sp
        V, S, G, PE = nc.vector, nc.scalar, nc.gpsimd, nc.tensor
        HB_t = [T(None, f"HB{n}") for n in range(8)]
        YB_t = [T(None, f"YB{n}") for n in range(8)]
        YA_t = [T(None, f"YA{n}") for n in range(8)]
        YI_t = [T(None, f"YI{n}") for n in range(8)]
        MB_t = T(None, "MB")
        X1_t = [T(None, f"X1{n}") for n in range(32)]

        def wview(ap2d):
            return ap2d.rearrange("(kc p) n -> p kc n", p=128)

        identf = fw.sb("identf", [128, 128], F32)
        identb = fw.sb("identb", [128, 128], BF16)
        ones = fw.sb("ones", [128, 128], F32)
        mhalf = fw.sb("mhalf", [128, 1], F32)
        fw.op(pool, lambda: G.memset(ones[:], 1.0), writes=[ones])
        fw.op(pool, lambda: G.memset(mhalf[:], -0.5), writes=[mhalf])
        fw.op(pool, lambda: G.memset(identf[:], 1.0), writes=[identf])
        fw.op(pool, lambda: G.affine_select(identf[:], identf[:], [[-1, 128]], ALU.is_equal, 0.0, base=0,
                                            channel_multiplier=1), reads=[identf], writes=[identf])
        fw.op(dve, lambda: V.tensor_copy(identb[:], identf[:]), reads=[identf], writes=[identb])

        class NormBufs:
            def __init__(self, dt_out, tag):
                self.i = 0
                self.xt = [fw.sb(f"xt{tag}{i}", [128, D], F32) for i in range(2)]
                self.junk = fw.sb(f"junk{tag}", [128, D], BF16)
                self.ss = [fw.sb(f"ss{tag}{i}", [128, 1], F32) for i in range(2)]
                self.rs = [fw.sb(f"rs{tag}{i}", [128, 1], F32) for i in range(2)]
                self.t1 = [fw.sb(f"t1{tag}{i}", [128, D], F32) for i in range(2)]
                self.hx = [fw.sb(f"hx{tag}{i}", [128, D], dt_out) for i in range(2)]

        def norm_load(nb, src_ap, nr, src_dep=()):
            i = nb.i; nb.i ^= 1
            xt = nb.xt[i]
            fw.dma(sp, xt[0:nr, :], src_ap, xt, reads=list(src_dep), writes=[xt])
            return i

        def norm_tile(nb, src_ap, nr, Gt, SHt, src_dep=()):
            i = norm_load(nb, src_ap, nr, src_dep)
            return norm_compute(nb, i, nr, Gt, SHt)

        def norm_compute(nb, i, nr, Gt, SHt):
            xt, ss, rs, t1, hx = nb.xt[i], nb.ss[i], nb.rs[i], nb.t1[i], nb.hx[i]
            fw.op(act, lambda: S.activation(nb.junk[0:nr, :], xt[0:nr, :], AF.Square, scale=1.0 / 32.0,
                                            accum_out=ss[0:nr, :]), reads=[xt], writes=[nb.junk, ss])
            fw.op(pool, lambda: G.tensor_scalar(rs[0:nr, :], ss[0:nr, :], EPS, None, ALU.add), reads=[ss], writes=[rs])
            fw.op(pool, lambda: G.tensor_tensor(rs[0:nr, :], rs[0:nr, :], mhalf[0:nr, :], ALU.pow),
                  reads=[rs, mhalf], writes=[rs])
            fw.op(dve, lambda: V.scalar_tensor_tensor(t1[0:nr, :], xt[0:nr, :], rs[0:nr, 0:1], Gt[0:nr, :],
                                                      ALU.mult, ALU.mult), reads=[xt, rs, Gt], writes=[t1])
            fw.op(pool, lambda: G.tensor_tensor(hx[0:nr, :], t1[0:nr, :], SHt[0:nr, :], ALU.add),
                  reads=[t1, SHt], writes=[hx])
            return hx, xt

        class HxT:
            def __init__(self, ncols, tag):
                self.nb = NormBufs(BF16, tag)
                self.ptr = [fw.ps(f"ptr{tag}{i}", [128, D], BF16) for i in range(2)]
                self.out = [fw.sb(f"hxT{tag}{i}", [128, 8, ncols], BF16) for i in range(2)]
                self.i = 0; self.pi = 0

            def start(self, src_d, row0, ntok, Gt, SHt):
                o = self.out[self.i]; self.i ^= 1

                def gen():
                    nb = self.nb
                    tiles = []
                    c = 0
                    while c < ntok:
                        tiles.append((c, min(128, ntok - c)))
                        c += 128
                    nt = len(tiles)
                    st_ = {}

                    def stage(t, s_):
                        c, nr = tiles[t]
                        if s_ == -1:
                            st_[t] = norm_load(nb, src_d[row0 + c: row0 + c + nr, :], nr)
                            return
                        i = st_[t]
                        xt, ss, rs, t1, hx = nb.xt[i], nb.ss[i], nb.rs[i], nb.t1[i], nb.hx[i]
                        if s_ == 0:
                            fw.op(act, lambda: S.activation(nb.junk[0:nr, :], xt[0:nr, :], AF.Square, scale=1.0 / 32.0,
                                                            accum_out=ss[0:nr, :]), reads=[xt], writes=[nb.junk, ss])
                            fw.op(pool, lambda: G.tensor_scalar(rs[0:nr, :], ss[0:nr, :], EPS, None, ALU.add), reads=[ss], writes=[rs])
                            fw.op(pool, lambda: G.tensor_tensor(rs[0:nr, :], rs[0:nr, :], mhalf[0:nr, :], ALU.pow),
                                  reads=[rs, mhalf], writes=[rs])
                        elif s_ == 1:
                            fw.op(dve, lambda: V.scalar_tensor_tensor(t1[0:nr, :], xt[0:nr, :], rs[0:nr, 0:1], Gt[0:nr, :],
                                                                      ALU.mult, ALU.mult), reads=[xt, rs, Gt], writes=[t1])
                            fw.op(pool, lambda: G.tensor_tensor(hx[0:nr, :], t1[0:nr, :], SHt[0:nr, :], ALU.add),
                                  reads=[t1, SHt], writes=[hx])
                        elif s_ == 2:
                            p = self.ptr[t % 2]
                            for kc in range(8):
                                fw.op(pe, lambda: PE.transpose(p[:, kc * 128: kc * 128 + nr], hx[0:nr, kc * 128:(kc + 1) * 128],
                                                               identb[0:nr, 0:nr]), reads=[hx, identb], writes=[p], inc=(kc == 7))
                        else:
                            p = self.ptr[t % 2]
                            pv = p.t[:].rearrange("p (k n) -> p k n", k=8)
                            fw.op(act, lambda: S.copy(o[:, :, c:c + nr], pv[:, :, 0:nr]), reads=[p], writes=[o])

                    stage(0, -1)
                    ncalls = 2 * (nt - 1) + 4
                    for ci_ in range(ncalls):
                        for t in range(nt):
                            s_ = ci_ - 2 * t
                            if s_ == 0 and t + 1 < nt:
                                stage(t + 1, -1)
                            if 0 <= s_ <= 3:
                                stage(t, s_)
                        yield
                return o, gen()

            def make(self, src_d, row0, ntok, Gt, SHt):
                o, g = self.start(src_d, row0, ntok, Gt, SHt)
                for _ in g:
                    pass
                return o

        def drain(g):
            for _ in g:
                pass

        def pipelined(hb, order, ntok):
            hxT = hb.make(x_d, order[0] * TB, ntok, G1h[0], SH1h[0])
            for idx, n in enumerate(order):
                if idx + 1 < len(order):
                    nxt, g = hb.start(x_d, order[idx + 1] * TB, ntok, G1h[0], SH1h[0])
                else:
                    nxt, g = None, iter(())
                yield n, hxT, g
                drain(g)
                hxT = nxt

        G1h = [None]; SH1h = [None]

        def proj(ps_ap, pst, wt, col0, hxT, n0, n):
            for kc in range(8):
                fw.op(pe, lambda: PE.matmul(ps_ap, wt[:, kc, col0:col0 + 128], hxT[:, kc, n0:n0 + n],
                                            start=(kc == 0), stop=(kc == 7)), reads=[wt, hxT], writes=[pst], inc=(kc == 7))

        def load_w(tile, ap2d, n):
            for c0 in range(0, n, 512):
                fw.dma(pool, tile[:, :, c0:c0 + 512], wview(ap2d[:, c0:c0 + 512]), tile, writes=[tile])

        wbsem = fw.newsem("wbsem", es0)
        wbcnt = [0]

        def precast_gen():
            if STAGE != "full":
                return
            for e in range(NEXP):
                inst = G.dma_start(out=WB1_d[e].rearrange("p (k n) -> p k n", k=8), in_=w1_d[e].rearrange("(k p) n -> p k n", p=128))
                inst.then_inc(wbsem.h, 16); wbcnt[0] += 16
                yield
                inst = G.dma_start(out=WB2_d[e].rearrange("p (k n) -> p k n", k=8), in_=w2_d[e].rearrange("(k p) n -> p k n", p=128))
                inst.then_inc(wbsem.h, 16); wbcnt[0] += 16
                yield
        pcg = precast_gen()

        with ExitStack() as esM:
            fw.es = esM
            G1 = fw.sb("G1", [128, D], F32); SH1 = fw.sb("SH1", [128, D], F32)
            HG1 = fw.sb("HG1", [128, D], F32)
            G1h[0] = G1; SH1h[0] = SH1
            vT1 = fw.sb("vT1", [128, 128], F32); vT2 = fw.sb("vT2", [128, 64], F32)
            sc = fw.sb("sc", [128, 96], F32)
            stA = [fw.sb(f"stA{i}", [128, 1], F32) for i in range(8)]; stB = [fw.sb(f"stB{i}", [128, 1], F32) for i in range(8)]
            zst = fw.sb("zst", [128, 8], F32)
            esL = ExitStack()
            fw.es = esL
            Dg = fw.sb("Dg", [128, 64, 128], BF16)
            Wg = fw.sb("Wg", [128, 32, 128], BF16)
            esC = ExitStack()
            fw.es = esC
            G1c = fw.sb("G1c", [128, D], F32); SH1c = fw.sb("SH1c", [128, D], F32)
            C_C, C_CC, C_CB, C_BA, C_BX, C_LAM, C_BM, C_CAW = 0, 8, 16, 32, 48, 64, 80, 96
            S_HBA, S_HBX, S_CS, S_HC, S_HBM = 0, 16, 32, 48, 64

            with ExitStack() as esA:
                fw.es = esA
                vr1 = fw.sb("vr1", [128, 128], F32); vr2 = fw.sb("vr2", [64, 128], F32)
                G2 = fw.sb("G2", [128, D], F32); SH2 = fw.sb("SH2", [128, D], F32); GATE2 = fw.sb("GATE2", [128, D], F32)
                fw.op(pool, lambda: G.memset(vr1[:], 0.0), writes=[vr1])
                rows = [(cv_d, C_C, 16), (lcb_d, C_CB, 16), (lba_d, C_BA, 16), (lbx_d, C_BX, 16), (lam_d, C_LAM, 16),
                        (bm_d, C_BM, 16), (caw_d, C_CAW, 24)]
                for ap, r0, n in rows:
                    fw.dma(sp, vr1[r0:r0 + n, :], ap.rearrange("d (k p) -> (d k) p", p=128), vr1, writes=[vr1])
                fw.dma(sp, vr2[:, :], lcw_d.rearrange("d j (k p) -> (d j k) p", p=128), vr2, writes=[vr2])
                pt = fw.ps("pt0", [128, 128], F32)
                fw.op(pe, lambda: PE.transpose(pt[:, 0:120], vr1[0:120, :], identf[0:120, 0:120]), reads=[vr1, identf], writes=[pt])
                fw.op(dve, lambda: V.tensor_copy(vT1[:, 0:120], pt[:, 0:120]), reads=[pt], writes=[vT1])
                fw.op(pe, lambda: PE.transpose(pt[:, 0:64], vr2[0:64, :], identf[0:64, 0:64]), reads=[vr2, identf], writes=[pt])
                fw.op(dve, lambda: V.tensor_copy(vT2[:, :], pt[:, 0:64]), reads=[pt], writes=[vT2])
                fw.op(dve, lambda: V.tensor_scalar(sc[:, S_HBA:S_HBA + 32], vT1[:, C_BA:C_BA + 32], 0.5, None, ALU.mult),
                      reads=[vT1], writes=[sc])
                fw.op(dve, lambda: V.tensor_scalar(sc[:, S_HBM:S_HBM + 16], vT1[:, C_BM:C_BM + 16], 0.5, None, ALU.mult),
                      reads=[vT1], writes=[sc])
                tl = fw.sb("tl", [128, 16], F32)
                fw.op(act, lambda: S.activation(tl[:], vT1[:, C_LAM:C_LAM + 16], AF.Exp, scale=-1.0), reads=[vT1], writes=[tl])
                fw.op(act, lambda: S.activation(tl[:], tl[:], AF.Ln, bias=1.0, scale=1.0), reads=[tl], writes=[tl])
                fw.op(dve, lambda: V.tensor_scalar(sc[:, S_CS:S_CS + 16], tl[:], -8.0, None, ALU.mult), reads=[tl], writes=[sc])
                fw.op(dve, lambda: V.tensor_scalar(sc[:, S_HC:S_HC + 16], tl[:], -4.0, None, ALU.mult), reads=[tl], writes=[sc])
                fw.op(pool, lambda: G.memset(zst[:], 0.0), writes=[zst])
                for idx in range(64):
                    fw.op(dve, lambda: V.tensor_scalar(Dg[:, idx, :], identf[:], vT2[:, idx:idx + 1], None, ALU.mult),
                          reads=[identf, vT2], writes=[Dg])
                fw.op(pool, lambda: G.memset(Wg[:], 0.0), writes=[Wg])
                for g, wd in enumerate((lwa_d, lwx_d)):
                    for d in range(2):
                        base = (g * 2 + d) * 8
                        src = wd[d].rearrange("(kc two) k j -> two k kc j", two=2)
                        for h in range(2):
                            fw.dma(pool, Wg[64 * h:64 * h + 64, base:base + 8, 64 * h:64 * h + 64], src[h], Wg, writes=[Wg])
                sil = fw.sb("sil", [128, 16], F32); th0 = fw.sb("th0", [128, 16], F32)
                fw.op(act, lambda: S.activation(th0[:], vT1[:, 0:16], AF.Tanh, scale=0.5), reads=[vT1], writes=[th0])
                fw.op(dve, lambda: V.tensor_scalar(th0[:], th0[:], 0.5, 0.5, ALU.mult, ALU.add), reads=[th0], writes=[th0])
                fw.op(dve, lambda: V.tensor_tensor(sil[:], th0[:], vT1[:, 0:16], ALU.mult), reads=[th0, vT1], writes=[sil])
                Sx = fw.sb("Sx", [128, 16, 128], F32)
                for k in range(16):
                    fw.op(dve, lambda: V.tensor_scalar(Sx[:, k, :], ones[:], sil[:, k:k + 1], None, ALU.mult),
                          reads=[ones, sil], writes=[Sx])
                nmx = fw.sb("nmx", [128, D], F32); nfx = fw.sb("nfx", [128, D], F32)
                fw.dma(sp, nmx[:], nmix_d[0:1, :].partition_broadcast(128), nmx, writes=[nmx])
                fw.dma(sp, nfx[:], nffn_d[0:1, :].partition_broadcast(128), nfx, writes=[nfx])
                wad = [fw.sb(f"wad{i}", [128, 8, 512], F32) for i in range(2)]
                bad = [fw.sb(f"bad{i}", [128, 512], F32) for i in range(2)]
                pa = [fw.ps(f"pa{i}", [128, 512], F32) for i in range(2)]
                tmpg = fw.sb("tmpg", [128, 512], F32)
                dests = {0: (SH1, 0), 1: (G1, 1), 2: (HG1, 2), 3: (SH2, 0), 4: (G2, 1), 5: (GATE2, 0)}
                cdests = {0: (SH1c, 0), 1: (G1c, 1)}
                for g in range(12):
                    wt = wad[g % 2]; bt = bad[g % 2]
                    fw.dma(sp, wt[:], wview(wada_d[:, g * 512:(g + 1) * 512]), wt, writes=[wt])
                    fw.dma(sp, bt[:], bada_d[0:1, g * 512:(g + 1) * 512].partition_broadcast(128), bt, writes=[bt])
                    for which in range(2):
                        if which == 1 and g >= 4:
                            continue
                        p = pa[which]
                        for kc in range(8):
                            fw.op(pe, lambda: PE.matmul(p[:], Sx[:, which * 8 + kc, :], wt[:, kc, :], start=(kc == 0), stop=(kc == 7)),
                                  reads=[Sx, wt], writes=[p], inc=(kc == 7))
                        dt_, kind = (dests if which == 0 else cdests)[g // 2]
                        cs = slice((g % 2) * 512, (g % 2) * 512 + 512)
                        if kind == 0:
                            fw.op(dve, lambda: V.tensor_tensor(dt_[:, cs], p[:], bt[:], ALU.add), reads=[p, bt], writes=[dt_])
                        elif kind == 2:
                            fw.op(dve, lambda: V.tensor_tensor(tmpg[:], p[:], bt[:], ALU.add), reads=[p, bt], writes=[tmpg])
                            fw.op(dve, lambda: V.tensor_scalar(dt_[:, cs], tmpg[:], 0.5, None, ALU.mult), reads=[tmpg], writes=[dt_])
                        else:
                            nrm = nmx if (which == 1 or g // 2 == 1) else nfx
                            fw.op(dve, lambda: V.tensor_tensor(tmpg[:], p[:], bt[:], ALU.add), reads=[p, bt], writes=[tmpg])
                            fw.op(dve, lambda: V.scalar_tensor_tensor(dt_[:, cs], tmpg[:], 1.0, nrm[:, cs], ALU.add, ALU.mult),
                                  reads=[tmpg, nrm], writes=[dt_])
                for i_, t_ in enumerate((G2, SH2, GATE2)):
                    fw.dma(pool, MB_d[i_], t_[:], t_, reads=[t_], writes=[MB_t])
                fw.barrier()
            fw.es = esC

            class LruBufs:
                def __init__(self, nset=2, extra=None):
                    self.i2 = 0; self.ig = 0; self.ih = 0; self.nset = nset
                    mk = lambda nm, dt, k=2: [fw.sb(f"{nm}{i}", [128, TB], dt) for i in range(k)]
                    self.v = mk("lv", BF16); self.thr = mk("lthr", F32); self.thi = mk("lthi", F32)
                    self.a = mk("la", F32, nset); self.a2 = mk("la2", F32, nset); self.bb = mk("lbb", F32, nset)
                    self.h = mk("lh", F32)
                    self.pv = [fw.ps(f"lpv{i}", [128, TB], F32) for i in range(2)]
                    self.pr = [fw.ps(f"lpr{i}", [128, TB], F32) for i in range(1)]
                    self.pi_ = [fw.ps(f"lpi{i}", [128, TB], F32) for i in range(1)]
                    if extra is not None:
                        self.pr.append(extra[0]); self.pi_.append(extra[1])

            def lru_alpha(lb, d, kc, uext, n, reverse):
                i = lb.i2; lb.i2 ^= 1
                g_ = lb.ig; lb.ig = (lb.ig + 1) % lb.nset
                v, thr, thi = lb.v[i], lb.thr[i], lb.thi[i]
                a, a2, bb = lb.a[g_], lb.a2[g_], lb.bb[g_]
                pv, pr, pi_ = lb.pv[i], lb.pr[i % len(lb.pr)], lb.pi_[i % len(lb.pi_)]
                ci = d * 8 + kc
                for j in range(4):
                    off = (6 - j) if reverse else j
                    fw.op(pe, lambda: PE.matmul(pv[:, 0:n], Dg[:, d * 32 + j * 8 + kc, :], uext[:, kc, off:off + n],
                                                start=(j == 0), stop=(j == 3)), reads=[Dg, uext], writes=[pv], inc=(j == 3))
                fw.op(dve, lambda: V.tensor_scalar(v[:, 0:n], pv[:, 0:n], vT1[:, C_CB + ci:C_CB + ci + 1], None, ALU.add),
                      reads=[pv, vT1], writes=[v])
                fw.op(pe, lambda: PE.matmul(pr[:, 0:n], Wg[:, (0 * 2 + d) * 8 + kc, :], v[:, 0:n], start=True, stop=True),
                      reads=[Wg, v], writes=[pr])
                fw.op(pe, lambda: PE.matmul(pi_[:, 0:n], Wg[:, (1 * 2 + d) * 8 + kc, :], v[:, 0:n], start=True, stop=True),
                      reads=[Wg, v], writes=[pi_])
                fw.op(act, lambda: S.activation(thr[:, 0:n], pr[:, 0:n], AF.Tanh, bias=sc[:, S_HBA + ci:S_HBA + ci + 1], scale=0.5),
                      reads=[pr, sc], writes=[thr])
                fw.op(act, lambda: S.activation(thi[:, 0:n], pi_[:, 0:n], AF.Tanh, bias=sc[:, S_HBX + ci:S_HBX + ci + 1], scale=0.5),
                      reads=[pi_, sc], writes=[thi])
                fw.op(act, lambda: S.activation(a[:, 0:n], thr[:, 0:n], AF.Exp, bias=sc[:, S_HC + ci:S_HC + ci + 1],
                                                scale=sc[:, S_HC + ci:S_HC + ci + 1]), reads=[thr, sc], writes=[a])
                fw.op(pool, lambda: G.tensor_tensor(a2[:, 0:n], a[:, 0:n], a[:, 0:n], ALU.mult), reads=[a], writes=[a2])
                fw.op(dve, lambda: V.scalar_tensor_tensor(bb[:, 0:n], thi[:, 0:n], 1.0, v[:, 0:n], ALU.add, ALU.mult),
                      reads=[thi, v], writes=[bb])
                return (a, a2, bb)

            def lru_sqrt(ctx_, n):
                a, a2, bb = ctx_
                fw.op(act, lambda: S.activation(a2[:, 0:n], a2[:, 0:n], AF.Sqrt, bias=1.0, scale=-1.0), reads=[a2], writes=[a2])

            def lru_beta(lb, ctx_, kc, n, st, reverse):
                a, a2, bb = ctx_
                h = lb.h[lb.ih]; lb.ih ^= 1
                fw.op(dve, lambda: V.scalar_tensor_tensor(bb[:, 0:n], bb[:, 0:n], 0.5, a2[:, 0:n], ALU.mult, ALU.mult),
                      reads=[bb, a2], writes=[bb])
                if reverse:
                    fw.op(dve, lambda: V.tensor_tensor_scan(h[:, 0:n][:, ::-1], a[:, 0:n][:, ::-1], bb[:, 0:n][:, ::-1],
                                                            st[kc][:, 0:1], ALU.mult, ALU.add), reads=[a, bb, st[kc]], writes=[h])
                    fw.op(pool, lambda: G.tensor_copy(st[kc][:, 0:1], h[:, 0:1]), reads=[h], writes=[st[kc]])
                else:
                    fw.op(dve, lambda: V.tensor_tensor_scan(h[:, 0:n], a[:, 0:n], bb[:, 0:n], st[kc][:, 0:1], ALU.mult, ALU.add),
                          reads=[a, bb, st[kc]], writes=[h])
                    fw.op(pool, lambda: G.tensor_copy(st[kc][:, 0:1], h[:, n - 1:n]), reads=[h], writes=[st[kc]])
                return h

            def lru_group(lb, d, kcs, uext, n, st, reverse, cb=None, cba=None):
                ctxs = []
                for kc in kcs:
                    ctxs.append(lru_alpha(lb, d, kc, uext, n, reverse))
                    if cba is not None:
                        cba(kc)
                for c_ in ctxs:
                    lru_sqrt(c_, n)
                for kc, c_ in zip(kcs, ctxs):
                    h = lru_beta(lb, c_, kc, n, st, reverse)
                    if cb is not None:
                        cb(kc, h)

            with ExitStack() as es1:
                fw.es = es1
                w_lx = fw.sb("w_lx", [128, 8, D], BF16)
                load_w(w_lx, win_d[:, 3 * D:4 * D], D)
                hb = HxT(TB, "a")
                pu = [fw.ps(f"pu{i}", [128, TB], F32) for i in range(2)]
                lb = LruBufs(8, extra=pu)
                uext = [fw.sb(f"uext{i}", [128, 8, TB + 6], BF16) for i in range(2)]
                for u in uext:
                    fw.op(pool, lambda: G.memset(u[:], 0.0), writes=[u])
                for k_ in range(8):
                    fw.op(pool, lambda: G.tensor_copy(stA[k_][:], zst[:, 0:1]), reads=[zst], writes=[stA[k_]])
                    fw.op(pool, lambda: G.tensor_copy(stB[k_][:], zst[:, 0:1]), reads=[zst], writes=[stB[k_]])
                hcT = hb.make(ctx_d, 0, 256, G1c, SH1c)
                ue = uext[0]
                for kc in range(8):
                    p = pu[kc % 2]
                    proj(p[:, 0:256], p, w_lx, kc * 128, hcT, 0, 256)
                    fw.op(dve, lambda: V.tensor_copy(ue[:, kc, 3:259], p[:, 0:256]), reads=[p], writes=[ue])
                lru_group(lb, 0, list(range(8)), ue, 256, stA, False)
                lru_group(lb, 1, list(range(8)), ue, 256, stB, True)
                fw.op(pool, lambda: G.memset(ue[:], 0.0), writes=[ue])
                def do_proj1(nb, hxT_, kc):
                    p = pu[kc % 2]
                    proj(p[:], p, w_lx, kc * 128, hxT_, 0, TB)
                    fw.op(dve, lambda: V.tensor_copy(uext[nb % 2][:, kc, 3:3 + TB], p[:]), reads=[p], writes=[uext[nb % 2]])
                order = list(range(15, -1, -1))
                hxT = hb.make(x_d, order[0] * TB, TB, G1, SH1)
                for kc in range(8):
                    do_proj1(order[0], hxT, kc)
                for idx, n in enumerate(order):
                    ue = uext[n % 2]
                    if idx + 1 < len(order):
                        nn = order[idx + 1]
                        nxt, gnx = hb.start(x_d, nn * TB, TB, G1, SH1)
                    else:
                        nn, nxt, gnx = None, None, iter(())

                    def cba1(kc, gnx=gnx):
                        next(gnx, None)
                        if kc == 7:
                            drain(gnx)

                    def cb1(kc, h, n=n, nn=nn, nxt=nxt):
                        if n < 8:
                            fw.dma(pool, HB_d[:, kc, n * TB:(n + 1) * TB], h[:], h, reads=[h], writes=[HB_t[n]])
                        if nn is not None:
                            do_proj1(nn, nxt, kc)
                    lru_group(lb, 1, list(range(8)), ue, TB, stB, True, cb1, cba1)
                    drain(gnx)
                    if nn is not None:
                        un = uext[nn % 2]
                        fw.op(pool, lambda: G.tensor_copy(un[:, :, 3 + TB:6 + TB], ue[:, :, 3:6]), reads=[ue], writes=[un])
                fw.barrier()
            esC.close()
            fw.es = esL

            with ExitStack() as es2:
                fw.es = es2
                w_lx = fw.sb("w_lx", [128, 8, D], BF16); w_lg = fw.sb("w_lg", [128, 8, D], BF16)
                load_w(w_lx, win_d[:, 3 * D:4 * D], D); load_w(w_lg, win_d[:, 4 * D:5 * D], D)
                hb = HxT(TB, "b")
                pu = [fw.ps(f"pu{i}", [128, TB], F32) for i in range(2)]
                lb = LruBufs(4, extra=pu)
                uext = [fw.sb(f"uext{i}", [128, 8, TB + 6], BF16) for i in range(2)]
                for u in uext:
                    fw.op(pool, lambda: G.memset(u[:], 0.0), writes=[u])
                hbl = [fw.sb(f"hbl{i}", [128, 8, TB], BF16) for i in range(1)]
                ybin = [fw.sb(f"ybin{i}", [128, 8, TB], BF16) for i in range(2)]
                mk = lambda nm, dt: [fw.sb(f"{nm}{i}", [128, TB], dt) for i in range(2)]
                xs_, x2_, th_, hs_ = mk("gxs", F32), mk("gx2", F32), mk("gth", F32), mk("ghs", F32)
                def do_proj2(nb, hxT_, kc):
                    p = pu[kc % 2]
                    proj(p[:], p, w_lx, kc * 128, hxT_, 0, TB)
                    fw.op(dve, lambda: V.tensor_copy(uext[nb % 2][:, kc, 3:3 + TB], p[:]), reads=[p], writes=[uext[nb % 2]])
                hxT = hb.make(x_d, 0, TB, G1, SH1)
                for kc in range(8):
                    do_proj2(0, hxT, kc)
                nxt = None
                for n in range(8):
                    if n > 0:
                        hxT = nxt
                    next(pcg, None); next(pcg, None)
                    ue = uext[n % 2]
                    if n + 1 < 8:
                        nn = n + 1
                        nxt, gnx = hb.start(x_d, nn * TB, TB, G1, SH1)
                    else:
                        nn, nxt, gnx = None, None, iter(())
                    hbt = hbl[0]
                    fw.dma(sp, hbt[:], HB_d[:, :, n * TB:(n + 1) * TB], hbt, reads=[HB_t[n]], writes=[hbt])
                    yb = ybin[n % 2]
                    def cb2(kc, h, hxT=hxT, hbt=hbt, yb=yb, gnx=gnx, nn=nn, nxt=nxt):
                        i = kc % 2
                        hs = hs_[i]
                        fw.op(pool, lambda: G.tensor_tensor(hs[:], h[:], hbt[:, kc, :], ALU.add), reads=[h, hbt], writes=[hs])
                        p = pu[kc % 2]
                        proj(p[:], p, w_lg, kc * 128, hxT, 0, TB)
                        xs, x2, th = xs_[i], x2_[i], th_[i]
                        fw.op(act, lambda: S.copy(xs[:], p[:]), reads=[p], writes=[xs])
                        fw.op(act, lambda: S.activation(x2[:], p[:], AF.Square), reads=[p], writes=[x2])
                        fw.op(dve, lambda: V.tensor_scalar(x2[:], x2[:], GC0 * GC1, GC0, ALU.mult, ALU.add), reads=[x2], writes=[x2])
                        fw.op(dve, lambda: V.tensor_tensor(x2[:], x2[:], xs[:], ALU.mult), reads=[x2, xs], writes=[x2])
                        fw.op(act, lambda: S.activation(th[:], x2[:], AF.Tanh), reads=[x2], writes=[th])
                        fw.op(dve, lambda: V.scalar_tensor_tensor(th[:], th[:], 1.0, xs[:], ALU.add, ALU.mult), reads=[th, xs], writes=[th])
                        fw.op(dve, lambda: V.scalar_tensor_tensor(yb[:, kc, :], th[:], 0.5, hs[:], ALU.mult, ALU.mult),
                              reads=[th, hs], writes=[yb])
                        if nn is not None:
                            do_proj2(nn, nxt, kc)

                    def cba2(kc, gnx=gnx):
                        next(gnx, None); next(gnx, None)
                        if kc >= 3:
                            drain(gnx)
                    lru_group(lb, 0, [0, 1, 2, 3], ue, TB, stA, False, cb2, cba2)
                    lru_group(lb, 0, [4, 5, 6, 7], ue, TB, stA, False, cb2, cba2)
                    drain(gnx)
                    if nn is not None:
                        un = uext[nn % 2]
                        fw.op(pool, lambda: G.tensor_copy(un[:, :, 0:3], ue[:, :, TB:TB + 3]), reads=[ue], writes=[un])
                    fw.dma(pool, YI_d[:, :, n * TB:(n + 1) * TB], yb[:], yb, reads=[yb], writes=[YI_t[n]])
                fw.barrier()
            esL.close()
            fw.es = esM

            with ExitStack() as es2:
                fw.es = es2
                w_mb = fw.sb("w_mb", [128, 8, D], BF16); w_ob = fw.sb("w_ob", [128, 8, D], BF16)
                load_w(w_mb, win_d[:, 6 * D:7 * D], D); load_w(w_ob, woutb_d, D)
                hb = HxT(TB, "b2")
                pu = [fw.ps(f"pu{i}", [128, TB], F32) for i in range(2)]
                pyb = [fw.ps(f"pyb{i}", [128, TB], F32) for i in range(2)]
                ybin = [fw.sb(f"ybin{i}", [128, 8, TB], BF16) for i in range(2)]
                gyb = [fw.sb(f"gyb{i}", [128, 8, TB], BF16) for i in range(2)]
                sg_ = [fw.sb(f"gsg{i}", [128, TB], F32) for i in range(2)]
                for n, hxT, gnx in pipelined(hb, list(range(8)), TB):
                    next(pcg, None); next(pcg, None)
                    yb = ybin[n % 2]
                    fw.dma(sp, yb[:], YI_d[:, :, n * TB:(n + 1) * TB], yb, reads=[YI_t[n]], writes=[yb])
                    gy = gyb[n % 2]
                    for mo in range(8):
                        i = mo % 2
                        py = pyb[i]
                        for kc in range(8):
                            fw.op(pe, lambda: PE.matmul(py[:], w_ob[:, kc, mo * 128:(mo + 1) * 128], yb[:, kc, :],
                                                        start=(kc == 0), stop=(kc == 7)), reads=[w_ob, yb], writes=[py], inc=(kc == 7))
                        p = pu[i]
                        proj(p[:], p, w_mb, mo * 128, hxT, 0, TB)
                        sg = sg_[i]
                        fw.op(act, lambda: S.activation(sg[:], p[:], AF.Tanh, bias=sc[:, S_HBM + 8 + mo:S_HBM + 9 + mo], scale=0.5),
                              reads=[p, sc], writes=[sg])
                        fw.op(dve, lambda: V.scalar_tensor_tensor(gy[:, mo, :], sg[:], 1.0, py[:], ALU.add, ALU.mult),
                              reads=[sg, py], writes=[gy])
                        next(gnx, None)
                    fw.dma(pool, YB_d[:, :, n * TB:(n + 1) * TB], gy[:], gy, reads=[gy], writes=[YB_t[n]])
                fw.barrier()
            fw.es = esM


            with ExitStack() as es3:
                fw.es = es3
                w_gb = fw.sb("w_gb", [128, 8, D], BF16); w_gc = fw.sb("w_gc", [128, 8, D], BF16)
                w_xc = fw.sb("w_xc", [128, 8, D], BF16); w_oa = fw.sb("w_oa", [128, 8, D], BF16)
                load_w(w_gc, win_d[:, 1 * D:2 * D], D); load_w(w_xc, win_d[:, 2 * D:3 * D], D)
                load_w(w_gb, win_d[:, 0:D], D); load_w(w_oa, wouta_d, D)
                hb = HxT(TB + 64, "c")
                pext = [fw.sb(f"pext{i}", [128, 8, TB + 128], BF16) for i in range(2)]
                for u in pext:
                    fw.op(pool, lambda: G.memset(u[:], 0.0), writes=[u])
                pg = [fw.ps(f"pg{i}", [128, TB], F32) for i in range(2)]
                px = [fw.ps(f"px{i}", [128, TB], F32) for i in range(2)]
                ph = [fw.ps(f"ph{i}", [128, 128], F32) for i in range(2)]
                mk = lambda nm, dt, w=TB: [fw.sb(f"{nm}{i}", [128, w], dt) for i in range(2)]
                gcs, gch, q_ = mk("gcs", F32), mk("gch", F32, 64), mk("cq", F32)
                yain = [fw.sb(f"yain{i}", [128, 8, TB], BF16) for i in range(2)]
                yat = [fw.sb(f"yat{i}", [128, 8, TB], BF16) for i in range(2)]
                for n, hxT, gnx in pipelined(hb, list(range(8)), TB + 64):
                    next(pcg, None); next(pcg, None)
                    pe_t = pext[n % 2]; pprev = pext[(n + 1) % 2]
                    for kc in range(8):
                        i = kc % 2
                        proj(pg[i][:], pg[i], w_gc, kc * 128, hxT, 0, TB)
                        proj(px[i][:], px[i], w_xc, kc * 128, hxT, 0, TB)
                        fw.op(act, lambda: S.copy(gcs[i][:], pg[i][:]), reads=[pg[i]], writes=[gcs[i]])
                        fw.op(dve, lambda: V.tensor_tensor(pe_t[:, kc, 64:64 + TB], gcs[i][:], px[i][:], ALU.mult),
                              reads=[gcs[i], px[i]], writes=[pe_t])
                        if kc >= 4:
                            proj(ph[i][:, 0:64], ph[i], w_gc, kc * 128, hxT, TB, 64)
                            proj(ph[i][:, 64:128], ph[i], w_xc, kc * 128, hxT, TB, 64)
                            fw.op(act, lambda: S.copy(gch[i][:], ph[i][:, 0:64]), reads=[ph[i]], writes=[gch[i]])
                            fw.op(dve, lambda: V.tensor_tensor(pe_t[:, kc, 64 + TB:128 + TB], gch[i][:], ph[i][:, 64:128], ALU.mult),
                                  reads=[gch[i], ph[i]], writes=[pe_t])
                    if n > 0:
                        fw.op(pool, lambda: G.tensor_copy(pe_t[:, 4:8, 0:64], pprev[:, 4:8, TB:TB + 64]), reads=[pprev], writes=[pe_t])
                    else:
                        fw.op(pool, lambda: G.memset(pe_t[:, 4:8, 0:64], 0.0), writes=[pe_t])
                    ya = yain[n % 2]
                    for kc in range(8):
                        i = kc % 2
                        q = q_[i]
                        w0 = vT1[:, C_CAW + kc:C_CAW + kc + 1]; w1_ = vT1[:, C_CAW + 8 + kc:C_CAW + 9 + kc]
                        w2_ = vT1[:, C_CAW + 16 + kc:C_CAW + 17 + kc]
                        fw.op(dve, lambda: V.tensor_scalar(q[:], pe_t[:, kc, 64:64 + TB], w1_, None, ALU.mult), reads=[pe_t, vT1], writes=[q])
                        if kc < 4:
                            pb = pe_t.t[:, kc, 64:64 + TB].rearrange("p (r c) -> p r c", c=64)
                            qv = q.t[:].rearrange("p (r c) -> p r c", c=64)
                            fw.op(dve, lambda: V.scalar_tensor_tensor(qv[:, :, 1:64], pb[:, :, 0:63], w0, qv[:, :, 1:64], ALU.mult, ALU.add),
                                  reads=[pe_t, q, vT1], writes=[q])
                            fw.op(dve, lambda: V.scalar_tensor_tensor(qv[:, :, 0:63], pb[:, :, 1:64], w2_, qv[:, :, 0:63], ALU.mult, ALU.add),
                                  reads=[pe_t, q, vT1], writes=[q])
                        else:
                            fw.op(dve, lambda: V.scalar_tensor_tensor(q[:], pe_t[:, kc, 0:TB], w0, q[:], ALU.mult, ALU.add),
                                  reads=[pe_t, q, vT1], writes=[q])
                            fw.op(dve, lambda: V.scalar_tensor_tensor(q[:], pe_t[:, kc, 128:128 + TB], w2_, q[:], ALU.mult, ALU.add),
                                  reads=[pe_t, q, vT1], writes=[q])
                        proj(pg[i][:], pg[i], w_gb, kc * 128, hxT, 0, TB)
                        fw.op(dve, lambda: V.tensor_tensor(ya[:, kc, :], q[:], pg[i][:], ALU.mult), reads=[q, pg[i]], writes=[ya])
                        next(gnx, None)
                    yt = yat[n % 2]
                    for mo in range(8):
                        i = mo % 2
                        for kc in range(8):
                            fw.op(pe, lambda: PE.matmul(px[i][:], w_oa[:, kc, mo * 128:(mo + 1) * 128], ya[:, kc, :],
                                                        start=(kc == 0), stop=(kc == 7)), reads=[w_oa, ya], writes=[px[i]], inc=(kc == 7))
                        fw.op(act, lambda: S.copy(yt[:, mo, :], px[i][:]), reads=[px[i]], writes=[yt])
                    fw.dma(pool, YA_d[:, :, n * TB:(n + 1) * TB], yt[:], yt, reads=[yt], writes=[YA_t[n]])
                fw.barrier()
            fw.es = esM

            with ExitStack() as es4:
                fw.es = es4
                w_ma = fw.sb("w_ma", [128, 8, D], BF16); w_oo = fw.sb("w_oo", [128, 8, D], BF16)
                load_w(w_ma, win_d[:, 5 * D:6 * D], D); load_w(w_oo, wo_d, D)
                hb = HxT(TB, "d")
                pm = [fw.ps(f"pm{i}", [128, TB], F32) for i in range(2)]
                pmx = [fw.ps(f"pmx{i}", [128, TB], F32) for i in range(2)]
                yab = [fw.sb(f"yab{i}", [128, 8, TB], BF16) for i in range(2)]
                ybb = [fw.sb(f"ybb{i}", [128, 8, TB], BF16) for i in range(2)]
                mg = [fw.sb(f"mg{i}", [128, 8, TB], BF16) for i in range(2)]
                mk = lambda nm, dt, w=TB: [fw.sb(f"{nm}{i}", [128, w], dt) for i in range(2)]
                sga, tt_ = mk("sga", F32), mk("mtt", F32)
                xr = [fw.sb(f"xr{i}", [128, D], F32) for i in range(2)]
                x1o = [fw.sb(f"x1o{i}", [128, D], F32) for i in range(2)]
                for n, hxT, gnx in pipelined(hb, list(range(8)), TB):
                    next(pcg, None); next(pcg, None)
                    ya = yab[n % 2]; yb = ybb[n % 2]; m = mg[n % 2]
                    fw.dma(sp, ya[:], YA_d[:, :, n * TB:(n + 1) * TB], ya, reads=[YA_t[n]], writes=[ya])
                    fw.dma(sp, yb[:], YB_d[:, :, n * TB:(n + 1) * TB], yb, reads=[YB_t[n]], writes=[yb])
                    for mo in range(8):
                        i = mo % 2
                        proj(pm[i][:], pm[i], w_ma, mo * 128, hxT, 0, TB)
                        fw.op(act, lambda: S.activation(sga[i][:], pm[i][:], AF.Tanh, bias=sc[:, S_HBM + mo:S_HBM + mo + 1], scale=0.5),
                              reads=[pm[i], sc], writes=[sga[i]])
                        fw.op(dve, lambda: V.scalar_tensor_tensor(tt_[i][:], sga[i][:], 1.0, ya[:, mo, :], ALU.add, ALU.mult),
                              reads=[sga[i], ya], writes=[tt_[i]])
                        fw.op(dve, lambda: V.tensor_tensor(m[:, mo, :], tt_[i][:], yb[:, mo, :], ALU.add), reads=[tt_[i], yb], writes=[m])
                        next(gnx, None)
                    for tt in range(4):
                        tile_i = n * 4 + tt
                        xt = xr[tt % 2]; xo = x1o[tt % 2]
                        fw.dma(sp, xt[:], x_d[tile_i * 128:(tile_i + 1) * 128, :], xt, writes=[xt])
                        for hh in range(2):
                            p = pmx[hh]
                            for kc in range(8):
                                fw.op(pe, lambda: PE.matmul(p[:], m[:, kc, tt * 128:(tt + 1) * 128], w_oo[:, kc, hh * 512:(hh + 1) * 512],
                                                            start=(kc == 0), stop=(kc == 7)), reads=[m, w_oo], writes=[p], inc=(kc == 7))
                            cs = slice(hh * 512, hh * 512 + 512)
                            fw.op(dve, lambda: V.tensor_tensor(xo[:, cs], p[:], HG1[:, cs], ALU.mult), reads=[p, HG1], writes=[xo])
                            fw.op(pool, lambda: G.tensor_tensor(xo[:, cs], xo[:, cs], xt[:, cs], ALU.add), reads=[xo, xt], writes=[xo])
                        fw.dma(pool, X1_d[tile_i * 128:(tile_i + 1) * 128, :], xo[:], xo, reads=[xo], writes=[X1_t[tile_i]])
                fw.barrier()
            fw.es = esM
        fw.es = es0

        with ExitStack() as esE:
          if STAGE == "full":
            fw.es = esE
            NS = 64
            I32 = mybir.dt.int32
            for _ in pcg:
                pass
            XS_t = T(None, "XSall"); FXS_t = [T(None, f"FXS{n}") for n in range(32)]
            YS_t = [T(None, f"YS{n}") for n in range(NS)]
            rw = fw.sb("rw", [128, 8, NEXP], F32)
            rbb = fw.sb("rbb", [128, NEXP], F32)
            b2s = fw.sb("b2s", [NEXP, D], F32)
            LG = fw.sb("LG", [128, 32, NEXP], F32); RANK = fw.sb("RANK", [128, 32, NEXP], F32)
            GD = fw.sb("GD", [128, 32, NEXP], F32); MX8 = fw.sb("MX8", [128, 32, 8], F32)
            G4h = fw.sb("G4h", [128, 32, 4], F32); POS4f = fw.sb("POS4f", [128, 32, 4], F32)
            POS4 = fw.sb("POS4", [128, 32, 4], I32)
            cnt = fw.sb("cnt", [128, NEXP], F32); pcn = fw.sb("pcn", [128, NEXP], F32)
            pend = fw.sb("pend", [128, NEXP], F32); pstart = fw.sb("pstart", [128, NEXP], F32)
            esl = fw.sb("esl", [128, NS], F32); wfl = fw.sb("wfl", [128, NS], F32)
            widx = fw.sb("widx", [128, NS], I32); widx2 = fw.sb("widx2", [128, NS], I32); eidx = fw.sb("eidx", [128, NS], I32)
            Ustr = fw.sb("Ustr", [128, 128], F32); iop = fw.sb("iop", [128, 1], F32)
            onesb = fw.sb("onesb", [1, TB], BF16)
            j32 = fw.sb("j32", [128, NEXP], F32)
            fw.dma(sp, rw[:], rw_d.rearrange("(kc p) e -> p kc e", p=128), rw, writes=[rw])
            fw.dma(sp, rbb[:], rb_d[0:1, :].partition_broadcast(128), rbb, writes=[rbb])
            fw.dma(sp, b2s[:], b2_d, b2s, writes=[b2s])
            B1T_t = T(None, "B1Tt")
            with ExitStack() as esb:
                fw.es = esb
                b1r = fw.sb("b1r", [NEXP, 2 * D], F32)
                pb1 = fw.ps("pb1", [128, 16 * NEXP], F32)
                b1Te = fw.sb("b1Te", [128, NEXP, 16], F32)
                fw.dma(sp, b1r[:], b1_d, b1r, writes=[b1r])
                b1v_ = b1r.t[:].rearrange("e (j p two) -> e j two p", p=128, two=2)
                for j in range(8):
                    for two in range(2):
                        s_ = j * 2 + two
                        fw.op(pe, lambda: PE.transpose(pb1[:, s_ * NEXP:(s_ + 1) * NEXP], b1v_[:, j, two, :], identf[0:NEXP, 0:NEXP]),
                              reads=[b1r, identf], writes=[pb1], inc=(s_ == 15))
                fw.op(dve, lambda: V.tensor_copy(b1Te.t[:].rearrange("p e s -> p s e"), pb1.t[:].rearrange("p (s e) -> p s e", e=NEXP)),
                      reads=[pb1], writes=[b1Te])
                fw.dma(sp, B1T_d.rearrange("(e p) s -> p e s", p=128), b1Te[:], b1Te, reads=[b1Te], writes=[B1T_t])
                fw.barrier()
            fw.es = esE
            fw.op(pool, lambda: G.memset(Ustr[:], 1.0), writes=[Ustr])
            fw.op(pool, lambda: G.affine_select(Ustr[:], Ustr[:], [[1, 128]], ALU.is_gt, 0.0, base=0, channel_multiplier=-1),
                  reads=[Ustr], writes=[Ustr])
            fw.op(pool, lambda: G.iota(iop[:], [[0, 1]], base=0, channel_multiplier=1, allow_small_or_imprecise_dtypes=True), writes=[iop])
            fw.op(pool, lambda: G.memset(onesb[:], 1.0), writes=[onesb])
            fw.op(pool, lambda: G.memset(cnt[:], 0.0), writes=[cnt])

            def idma(out, in_, semt, in_off=None, out_off=None, eoff=0, reads=(), writes=()):
                pool.wait(fw._deps(reads, writes))
                if semt.dsem is None:
                    semt.dsem = fw.newsem("d_" + semt.name)
                    fw.dtiles.append(semt)
                inst = G.indirect_dma_start(out=out, out_offset=out_off, in_=in_, in_offset=in_off, element_offset=eoff)
                semt.dcnt += 16
                inst.then_inc(semt.dsem.h, 16)
                d = (semt.dsem, semt.dcnt)
                for t in reads:
                    t.r[semt.dsem.id] = d
                for t in writes:
                    t.w = d; t.r = {}

            with ExitStack() as esR:
                fw.es = esR
                G2 = fw.sb("G2", [128, D], F32); SH2 = fw.sb("SH2", [128, D], F32)
                for i_, t_ in enumerate((G2, SH2)):
                    fw.dma(sp, t_[:], MB_d[i_], t_, reads=[MB_t], writes=[t_])
                zt = fw.sb("zt", [128, 8192], BF16)
                fw.op(pool, lambda: G.memset(zt[:], 0.0), writes=[zt])
                XSv = XS_d.rearrange("(a p r) n -> a p (r n)", p=128, r=8)
                for a_ in range(NS * TB // 1024):
                    fw.dma(act, XSv[a_], zt[:], zt, reads=[zt])
                XS_t.w = (zt.dsem, zt.dcnt)
                nbf = NormBufs(F32, "f")
                ptf = fw.ps("ptf", [128, D], F32)
                fxf = [fw.sb(f"fxf{i}", [128, 8, 128], F32) for i in range(2)]
                fxb = [fw.sb(f"fxb{i}", [128, D], BF16) for i in range(2)]
                plg = fw.ps("plg", [128, 128], F32); prk = fw.ps("prk", [128, 128], F32); pcn_ = fw.ps("pcnp", [128, 128], F32)
                nmx_ = fw.sb("nmx_", [128, 1], F32); msk = fw.sb("msk", [128, NEXP], F32); ex = fw.sb("ex", [128, NEXP], F32)
                den = fw.sb("den", [128, 1], F32); e4 = fw.sb("e4", [128, 4], F32); den4 = fw.sb("den4", [128, 1], F32)
                fx_next, _ = norm_tile(nbf, X1_d[0:128, :], 128, G2, SH2, src_dep=[X1_t[0]])
                for ti in range(32):
                    fx = fx_next
                    if ti + 1 < 32:
                        fx_next, _ = norm_tile(nbf, X1_d[(ti + 1) * 128:(ti + 2) * 128, :], 128, G2, SH2, src_dep=[X1_t[ti + 1]])
                    fb = fxb[ti % 2]
                    fw.op(pool, lambda: G.tensor_copy(fb[:], fx[:]), reads=[fx], writes=[fb])
                    fw.dma(sp, FXS_d[ti * 128:(ti + 1) * 128, :], fb[:], fb, reads=[fb], writes=[FXS_t[ti]])
                    for kc in range(8):
                        fw.op(pe, lambda: PE.transpose(ptf[:, kc * 128:(kc + 1) * 128], fx[:, kc * 128:(kc + 1) * 128], identf[:]),
                              reads=[fx, identf], writes=[ptf], inc=(kc == 7))
                    ff = fxf[ti % 2]
                    fw.op(act, lambda: S.copy(ff.t[:].rearrange("p k n -> p (k n)"), ptf[:]), reads=[ptf], writes=[ff])
                    for kc in range(8):
                        fw.op(pe, lambda: PE.matmul(plg[:, 0:NEXP], ff[:, kc, :], rw[:, kc, :], start=(kc == 0), stop=(kc == 7)),
                              reads=[ff, rw], writes=[plg], inc=(kc == 7))
                    lg = LG[:, ti, :]; mx8 = MX8[:, ti, :]
                    fw.op(dve, lambda: V.tensor_tensor(lg, plg[:, 0:NEXP], rbb[:], ALU.add), reads=[plg, rbb], writes=[LG])
                    fw.op(dve, lambda: V.max(mx8, lg), reads=[LG], writes=[MX8])
                    fw.op(dve, lambda: V.tensor_scalar(msk[:], lg, MX8[:, ti, 3:4], None, ALU.is_ge), reads=[LG, MX8], writes=[msk])
                    fw.op(dve, lambda: V.tensor_scalar(nmx_[:], MX8[:, ti, 0:1], -1.0, None, ALU.mult), reads=[MX8], writes=[nmx_])
                    fw.op(act, lambda: S.activation(ex[:], lg, AF.Exp, bias=nmx_[:, 0:1], scale=1.0), reads=[LG, nmx_], writes=[ex])
                    fw.op(act, lambda: S.activation(e4[:], MX8[:, ti, 0:4], AF.Exp, bias=nmx_[:, 0:1], scale=1.0, accum_out=den4[:]),
                          reads=[MX8, nmx_], writes=[e4, den4])
                    fw.op(dve, lambda: V.reciprocal(den[:], den4[:]), reads=[den4], writes=[den])
                    fw.op(dve, lambda: V.tensor_scalar(G4h[:, ti, :], e4[:], den[:, 0:1], 0.5, ALU.mult, ALU.mult), reads=[e4, den], writes=[G4h])
                    fw.op(dve, lambda: V.scalar_tensor_tensor(GD[:, ti, :], ex[:], den[:, 0:1], msk[:], ALU.mult, ALU.mult),
                          reads=[ex, den, msk], writes=[GD])
                    fw.op(pe, lambda: PE.matmul(prk[:, 0:NEXP], Ustr[:], msk[:], start=True, stop=True), reads=[Ustr, msk], writes=[prk])
                    fw.op(pe, lambda: PE.matmul(pcn_[:, 0:NEXP], ones[:], msk[:], start=True, stop=True), reads=[ones, msk], writes=[pcn_])
                    fw.op(dve, lambda: V.tensor_tensor(RANK[:, ti, :], prk[:, 0:NEXP], cnt[:], ALU.add), reads=[prk, cnt], writes=[RANK])
                    fw.op(dve, lambda: V.tensor_tensor(cnt[:], pcn_[:, 0:NEXP], cnt[:], ALU.add), reads=[pcn_, cnt], writes=[cnt])
                fw.op(dve, lambda: V.tensor_scalar(pcn[:], cnt[:], 0.0, None, ALU.is_gt), reads=[cnt], writes=[pcn])
                for j in range(1, 8):
                    fw.op(dve, lambda: V.scalar_tensor_tensor(pcn[:], cnt[:], 512.0 * j, pcn[:], ALU.is_gt, ALU.add), reads=[cnt, pcn], writes=[pcn])
                fw.op(dve, lambda: V.tensor_scalar(pcn[:], pcn[:], 512.0, None, ALU.mult), reads=[pcn], writes=[pcn])
                fw.op(dve, lambda: V.tensor_tensor_scan(pend[:], ones[:, 0:NEXP], pcn[:], 0.0, ALU.mult, ALU.add), reads=[ones, pcn], writes=[pend])
                fw.op(dve, lambda: V.tensor_tensor(pstart[:], pend[:], pcn[:], ALU.subtract), reads=[pend, pcn], writes=[pstart])
                for s_ in range(NS):
                    fw.op(dve, lambda: V.tensor_scalar(j32[:], pend[:], 512.0 * s_, 0.0, ALU.is_le, ALU.add, accum_out=esl[:, s_:s_ + 1]),
                          reads=[pend], writes=[j32, esl])
                fw.op(dve, lambda: V.tensor_scalar(esl[:], esl[:], float(NEXP - 1), None, ALU.min), reads=[esl], writes=[esl])
                fw.op(dve, lambda: V.tensor_copy(eidx[:], esl[:]), reads=[esl], writes=[eidx])
                fw.op(dve, lambda: V.tensor_scalar(wfl[:], esl[:], 128.0, iop[:, 0:1], ALU.mult, ALU.add), reads=[esl, iop], writes=[wfl])
                fw.op(dve, lambda: V.tensor_copy(widx[:], wfl[:]), reads=[wfl], writes=[widx])
                posf = fw.sb("posf", [128, NEXP], F32)
                fbsc = [T(None, f"fbsc{i}") for i in range(2)]
                for ti in range(32):
                    fw.op(dve, lambda: V.tensor_tensor(posf[:], RANK[:, ti, :], pstart[:], ALU.add), reads=[RANK, pstart], writes=[posf])
                    for k in range(4):
                        fw.op(dve, lambda: V.scalar_tensor_tensor(j32[:], LG[:, ti, :], MX8[:, ti, k:k + 1], posf[:], ALU.is_equal, ALU.mult,
                                                                  accum_out=POS4f[:, ti, k:k + 1]), reads=[LG, MX8, posf], writes=[j32, POS4f])
                fw.op(dve, lambda: V.tensor_copy(POS4[:], POS4f[:]), reads=[POS4f], writes=[POS4])
                for ti in range(32):
                    fb = fxb[ti % 2]
                    fw.dma(sp, fb[:], FXS_d[ti * 128:(ti + 1) * 128, :], fb, reads=[FXS_t[ti]], writes=[fb])
                    for k in range(4):
                        idma(XS_d, fb[:], fbsc[ti % 2], out_off=bass.IndirectOffsetOnAxis(ap=POS4[:, ti, k:k + 1], axis=0), reads=[fb, POS4, XS_t])
                fw.barrier()
            fw.es = esE

            with ExitStack() as esS:
                fw.es = esS
                w1s = [fw.sb(f"w1s{i}", [128, 8, 2 * D], BF16) for i in range(2)]
                w2s = [fw.sb(f"w2s{i}", [128, 8, D], BF16) for i in range(2)]
                b1s = [fw.sb(f"b1s{i}", [128, 16], F32) for i in range(2)]
                xr = [fw.sb(f"xr{i}", [128, 4, D], BF16) for i in range(2)]
                xT = [fw.sb(f"xT{i}", [128, 8, TB], BF16) for i in range(2)]
                ptr = [fw.ps(f"ptrE{i}", [128, D], BF16) for i in range(2)]
                pgl = [fw.ps(f"pgl{i}", [128, TB], F32) for i in range(2)]
                pyy = [fw.ps(f"pyy{i}", [128, TB], F32) for i in range(2)]
                mk = lambda nm, dt: [fw.sb(f"{nm}{i}", [128, TB], dt) for i in range(2)]
                gl_, th_, l1_ = mk("egl", F32), mk("eth", F32), mk("el1", F32)
                actT = [fw.sb(f"actT{i}", [128, 8, TB], BF16) for i in range(2)]
                ysb = [fw.sb(f"ysb{i}", [128, D], F32) for i in range(2)]
                w1f = WB1_d.rearrange("e p n -> (e p) n"); w2f = WB2_d.rearrange("e p n -> (e p) n")
                pool.wait([(wbsem, wbcnt[0])])

                def load_slot(s_):
                    ws = s_ % 2
                    off = bass.IndirectOffsetOnAxis(ap=widx[:, s_:s_ + 1], axis=0)
                    idma(b1s[ws][:], B1T_d, b1s[ws], in_off=off, reads=[widx, B1T_t], writes=[b1s[ws]])
                    idma(w1s[ws].t[:].rearrange("p k n -> p (k n)"), w1f, w1s[ws], in_off=off, reads=[widx], writes=[w1s[ws]])
                    idma(w2s[ws].t[:].rearrange("p k n -> p (k n)"), w2f, w2s[ws], in_off=off, reads=[widx], writes=[w2s[ws]])
                    fw.dma(sp, xr[ws][:], XS_d[s_ * TB:(s_ + 1) * TB, :].rearrange("(t p) n -> p t n", p=128), xr[ws], writes=[xr[ws]])

                def slot_transposes(sx):
                    xtx = xT[sx % 2]; xrx = xr[sx % 2]
                    for t in range(4):
                        p = ptr[t % 2]
                        for kc in range(8):
                            fw.op(pe, lambda: PE.transpose(p[:, kc * 128:(kc + 1) * 128], xrx[:, t, kc * 128:(kc + 1) * 128], identb[:]),
                                  reads=[xrx, identb], writes=[p], inc=(kc == 7))
                        fw.op(act, lambda: S.copy(xtx[:, :, t * 128:(t + 1) * 128], p.t[:].rearrange("p (k n) -> p k n", k=8)),
                              reads=[p], writes=[xtx])

                load_slot(0)
                slot_transposes(0)
                for s_ in range(NS):
                    ws = s_ % 2
                    if s_ + 1 < NS:
                        load_slot(s_ + 1)
                    xt_ = xT[ws]; xr_ = xr[ws]
                    at = actT[ws]
                    w1v = w1s[ws].t[:].rearrange("p k (m two) -> p k m two", two=2)
                    for j in range(8):
                        i = j % 2
                        for two in range(2):
                            p = pgl[two]
                            for kc in range(8):
                                fw.op(pe, lambda: PE.matmul(p[:], w1v[:, kc, j * 128:(j + 1) * 128, two], xt_[:, kc, :],
                                                            start=(kc == 0), stop=(kc == 7)), reads=[w1s[ws], xt_], writes=[p], inc=(kc == 7))
                        gl, th, l1 = gl_[i], th_[i], l1_[i]
                        fw.op(dve, lambda: V.tensor_scalar(gl[:], pgl[0][:], b1s[ws][:, 2 * j:2 * j + 1], 7.0, ALU.add, ALU.min),
                              reads=[pgl[0], b1s[ws]], writes=[gl])
                        fw.op(dve, lambda: V.tensor_scalar(l1[:], pgl[1][:], b1s[ws][:, 2 * j + 1:2 * j + 2], 7.0, ALU.add, ALU.min),
                              reads=[pgl[1], b1s[ws]], writes=[l1])
                        fw.op(act, lambda: S.activation(th[:], gl[:], AF.Tanh, scale=0.851), reads=[gl], writes=[th])
                        fw.op(act, lambda: S.activation(l1[:], l1[:], AF.Relu, bias=7.0, scale=1.0), reads=[l1], writes=[l1])
                        fw.op(dve, lambda: V.scalar_tensor_tensor(th[:], th[:], 1.0, gl[:], ALU.add, ALU.mult), reads=[th, gl], writes=[th])
                        fw.op(dve, lambda: V.scalar_tensor_tensor(at[:, j, :], l1[:], -6.0, th[:], ALU.add, ALU.mult),
                              reads=[l1, th], writes=[at])
                    if s_ + 1 < NS:
                        slot_transposes(s_ + 1)
                    for tt in range(4):
                        yb_ = ysb[tt % 2]
                        for hh in range(2):
                            py = pyy[hh]
                            for kc in range(8):
                                fw.op(pe, lambda: PE.matmul(py[:], at[:, kc, tt * 128:(tt + 1) * 128], w2s[ws][:, kc, hh * 512:(hh + 1) * 512],
                                                            start=(kc == 0), stop=(kc == 7)), reads=[at, w2s[ws]], writes=[py], inc=(kc == 7))
                            fw.op(act, lambda: S.copy(yb_[:, hh * 512:(hh + 1) * 512], py[:]), reads=[py], writes=[yb_])
                        r0 = s_ * TB + tt * 128
                        fw.dma(sp, YS_d[r0:r0 + 128, :], yb_[:], yb_, reads=[yb_], writes=[YS_t[s_]])
                fw.barrier()
            fw.es = esE

            with ExitStack() as esC2:
                fw.es = esC2
                GATE2 = fw.sb("GATE2", [128, D], F32); NF = fw.sb("NF", [128, D], F32)
                fw.dma(sp, GATE2[:], MB_d[2], GATE2, reads=[MB_t], writes=[GATE2])
                fw.dma(sp, NF[:], nfin_d[0:1, :].partition_broadcast(128), NF, writes=[NF])
                plg = fw.ps("plgC", [128, 128], F32)
                pyy = [fw.ps(f"pyyC{i}", [128, TB], F32) for i in range(2)]
                ghT = [fw.sb(f"ghT{i}", [NEXP, 128], F32) for i in range(2)]
                yk = [fw.sb(f"yk{i}", [128, D], F32) for i in range(8)]
                accs = [fw.sb(f"accs{i}", [128, D], F32) for i in range(2)]
                x1r = [fw.sb(f"x1r{i}", [128, D], F32) for i in range(2)]
                junk = fw.sb("junkC", [128, D], BF16)
                ssC = [fw.sb(f"ssC{i}", [128, 1], F32) for i in range(2)]
                rsC = [fw.sb(f"rsC{i}", [128, 1], F32) for i in range(2)]
                def cgather(ti):
                    for k in range(4):
                        y_ = yk[(ti % 2) * 4 + k]
                        idma(y_[:], YS_d, y_, in_off=bass.IndirectOffsetOnAxis(ap=POS4[:, ti, k:k + 1], axis=0), reads=[POS4] + YS_t, writes=[y_])
                cgather(0)
                for ti in range(32):
                    i = ti % 2
                    if ti + 1 < 32:
                        cgather(ti + 1)
                    ac = accs[i]; xo = x1r[i]; ss = ssC[i]; rs = rsC[i]; gT = ghT[i]
                    fw.op(pe, lambda: PE.transpose(plg[0:NEXP, :], GD[:, ti, :], identf[:]), reads=[GD, identf], writes=[plg])
                    fw.op(act, lambda: S.copy(gT[:], plg[0:NEXP, :]), reads=[plg], writes=[gT])
                    for hh in range(2):
                        py = pyy[hh]
                        fw.op(pe, lambda: PE.matmul(py[:], gT[:], b2s[:, hh * 512:(hh + 1) * 512], start=True, stop=True),
                              reads=[gT, b2s], writes=[py])
                        fw.op(act, lambda: S.copy(ac[:, hh * 512:(hh + 1) * 512], py[:]), reads=[py], writes=[ac])
                    fw.dma(sp, xo[:], X1_d[ti * 128:(ti + 1) * 128, :], xo, reads=[X1_t[ti]], writes=[xo])
                    for k in range(4):
                        y_ = yk[(ti % 2) * 4 + k]
                        fw.op(dve, lambda: V.scalar_tensor_tensor(ac[:], y_[:], G4h[:, ti, k:k + 1], ac[:], ALU.mult, ALU.add),
                              reads=[y_, G4h, ac], writes=[ac])
                    fw.op(dve, lambda: V.tensor_tensor(ac[:], ac[:], GATE2[:], ALU.mult), reads=[ac, GATE2], writes=[ac])
                    fw.op(pool, lambda: G.tensor_tensor(ac[:], ac[:], xo[:], ALU.add), reads=[ac, xo], writes=[ac])
                    fw.op(act, lambda: S.activation(junk[:], ac[:], AF.Square, scale=1.0 / 32.0, accum_out=ss[:]), reads=[ac], writes=[junk, ss])
                    fw.op(pool, lambda: G.tensor_scalar(rs[:], ss[:], EPS, None, ALU.add), reads=[ss], writes=[rs])
                    fw.op(pool, lambda: G.tensor_tensor(rs[:], rs[:], mhalf[:], ALU.pow), reads=[rs, mhalf], writes=[rs])
                    fw.op(dve, lambda: V.scalar_tensor_tensor(xo[:], ac[:], rs[:, 0:1], NF[:], ALU.mult, ALU.mult), reads=[ac, rs, NF], writes=[xo])
                    fw.dma(sp, out_d[ti * 128:(ti + 1) * 128, :], xo[:], xo, reads=[xo])
                fw.barrier()
            fw.es = esE
            fw.barrier()
        fw.es = es0

    return nc


_NC = None


def kernel(**inp):
    global _NC
    f = lambda a: np.ascontiguousarray(np.asarray(a, dtype=np.float32))
    def wl2(a):
        a = np.asarray(a, dtype=np.float32)
        E_ = a.shape[0]
        return np.ascontiguousarray(a.reshape(E_, 4, 2, 128, D).transpose(0, 1, 3, 2, 4)).reshape(E_, 512, 2 * D)
    x = f(inp["x"]); ctx = f(inp["ctx"]); c = f(inp["c"]); c_ctx = f(inp["c_ctx"])
    if _NC is None:
        _NC = build_nc()
    nc = _NC
    shared = {
        "w_ada": f(inp["w_ada"][0]), "b_ada": f(inp["b_ada"][0]).reshape(1, -1),
        "norm_mix": f(inp["norm_mix"][0]).reshape(1, -1), "w_in": f(inp["w_in"][0]),
        "w_out_a": f(inp["w_out_a"][0]), "w_out_b": f(inp["w_out_b"][0]), "b_merge": f(inp["b_merge"][0]),
        "w_o": f(inp["w_o"][0]), "norm_ffn": f(inp["norm_ffn"][0]).reshape(1, -1),
        "router_w": f(inp["router_w"][0]), "router_b": f(inp["router_b"][0]).reshape(1, -1),
        "w1": f(inp["w1"][0] if STAGE == "full" else inp["w1"][0][:1]), "b1": f(inp["b1"][0]),
        "w2": f(inp["w2"][0] if STAGE == "full" else inp["w2"][0][:1]), "b2": f(inp["b2"][0]),
        "norm_final": f(inp["norm_final"]).reshape(1, -1),
    }
    lru_keys = ["lru_conv_w", "lru_conv_b", "lru_wa", "lru_ba", "lru_wx", "lru_bx", "lru_lambda"]
    in_maps = []
    for k in CORES:
        b, half = k // 2, k % 2
        m = dict(shared)
        if half == 0:
            m["x"] = x[b]; m["ctx"] = ctx[b]
            m["conv_a_w"] = f(inp["conv_a_w"][0])
            for key in lru_keys:
                m[key] = f(inp[key][0])
        else:
            m["x"] = np.ascontiguousarray(x[b][::-1]); m["ctx"] = np.ascontiguousarray(ctx[b][::-1])
            m["conv_a_w"] = np.ascontiguousarray(f(inp["conv_a_w"][0])[::-1])
            for key in lru_keys:
                m[key] = np.ascontiguousarray(f(inp[key][0])[::-1])
        m["cvec"] = np.ascontiguousarray(np.stack([c[b], c_ctx], axis=0))
        in_maps.append(m)
    res = run_bass_kernel_spmd(nc, in_maps, core_ids=list(range(len(CORES))))
    out = np.zeros((4, NTOK, D), np.float32)
    for i_, k in enumerate(CORES):
        b, half = k // 2, k % 2
        o = np.asarray(res.results[i_]["out"], dtype=np.float32)
        if half == 0:
            out[b, :OWN] = o
        else:
            out[b, OWN:] = o[::-1]
    if DEBUG:
        kernel.debug = res.results
    return out
```

```python
import numpy as np
from contextlib import ExitStack
import concourse.bass as bass
import concourse.mybir as mybir
from concourse.bass_utils import run_bass_kernel_spmd

F32 = mybir.dt.float32
BF16 = mybir.dt.bfloat16
AF = mybir.ActivationFunctionType
ALU = mybir.AluOpType

D = 1024
NTOK = 8192
OWN = 4096
TB = 512
NEXP = 32
EPS = 1e-6
GC0 = 0.7978845608028654
GC1 = 0.044715
DEBUG = False
STAGE = "full"
CORES = list(range(8))


class T:
    def __init__(self, t, name):
        self.t = t; self.name = name
        self.w = None; self.r = {}
        self.dsem = None; self.dcnt = 0

    def __getitem__(self, k):
        return self.t[k]


class Sem:
    uid = 0

    def __init__(self, h):
        self.h = h
        Sem.uid += 1
        self.id = Sem.uid


class E:
    def __init__(self, fw, name, eng):
        self.fw = fw; self.name = name; self.eng = eng
        self.sem = fw.newsem("e_" + name, fw.es0); self.count = 0
        self.seen = {}

    def wait(self, deps):
        best = {}
        for (s, v) in deps:
            if s.id not in best or best[s.id][1] < v:
                best[s.id] = (s, v)
        for k, (s, v) in best.items():
            if s is self.sem and self.name == "pe":
                continue
            if self.seen.get(k, 0) >= v:
                continue
            self.eng.wait_ge(s.h, v)
            self.seen[k] = v


class FW:
    def __init__(self, nc, es0):
        self.nc = nc; self.es0 = es0; self.es = es0
        self.nname = 0
        self.pe = E(self, "pe", nc.tensor)
        self.act = E(self, "act", nc.scalar)
        self.dve = E(self, "dve", nc.vector)
        self.pool = E(self, "pool", nc.gpsimd)
        self.sp = E(self, "sp", nc.sync)
        self.engs = [self.pe, self.act, self.dve, self.pool, self.sp]
        self.dtiles = []
        self.nname = 0

    def newsem(self, name, es=None):
        self.nname += 1
        return Sem((es or self.es).enter_context(self.nc.semaphore(f"{name}_{self.nname}")))

    def sb(self, name, shape, dt):
        self.nname += 1
        return T(self.es.enter_context(self.nc.sbuf_tensor(f"{name}_{self.nname}", shape, dt)), name)

    def ps(self, name, shape, dt):
        self.nname += 1
        return T(self.es.enter_context(self.nc.psum_tensor(f"{name}_{self.nname}", shape, dt)), name)

    def _deps(self, reads, writes):
        deps = []
        for t in reads:
            if t.w: deps.append(t.w)
        for t in writes:
            if t.w: deps.append(t.w)
            deps += list(t.r.values())
        return deps

    def op(self, e, fn, reads=(), writes=(), inc=True):
        e.wait(self._deps(reads, writes))
        inst = fn()
        if inc:
            e.count += 1
            inst.then_inc(e.sem.h, 1)
            d = (e.sem, e.count)
        else:
            d = (e.sem, e.count + 1)
        for t in reads:
            t.r[e.sem.id] = d
        for t in writes:
            t.w = d; t.r = {}
        return inst

    def dma(self, q, out, in_, semt, reads=(), writes=(), **kw):
        q.wait(self._deps(reads, writes))
        if semt.dsem is None:
            semt.dsem = self.newsem("d_" + semt.name)
            self.dtiles.append(semt)
        inst = q.eng.dma_start(out=out, in_=in_, **kw)
        semt.dcnt += 16
        inst.then_inc(semt.dsem.h, 16)
        d = (semt.dsem, semt.dcnt)
        for t in reads:
            t.r[semt.dsem.id] = d
        for t in writes:
            t.w = d; t.r = {}
        return inst

    def barrier(self):
        deps = [(e.sem, e.count) for e in self.engs if e.count > 0]
        deps += [(t.dsem, t.dcnt) for t in self.dtiles if t.dcnt > 0]
        for e in self.engs:
            e.wait(deps)
        self.dtiles = []


def build_nc():
    nc = bass.Bass("TRN2", target_bir_lowering=False)

    def din(name, shape):
        return nc.dram_tensor(name, shape, F32, kind="ExternalInput").ap()

    x_d = din("x", [NTOK, D]); ctx_d = din("ctx", [256, D]); cv_d = din("cvec", [2, D])
    wada_d = din("w_ada", [D, 6 * D]); bada_d = din("b_ada", [1, 6 * D])
    nmix_d = din("norm_mix", [1, D]); win_d = din("w_in", [D, 7 * D])
    caw_d = din("conv_a_w", [3, D]); wouta_d = din("w_out_a", [D, D])
    lcw_d = din("lru_conv_w", [2, 4, D]); lcb_d = din("lru_conv_b", [2, D])
    lwa_d = din("lru_wa", [2, 16, 64, 64]); lba_d = din("lru_ba", [2, D])
    lwx_d = din("lru_wx", [2, 16, 64, 64]); lbx_d = din("lru_bx", [2, D])
    lam_d = din("lru_lambda", [2, D]); woutb_d = din("w_out_b", [D, D])
    bm_d = din("b_merge", [2, D]); wo_d = din("w_o", [D, D]); nffn_d = din("norm_ffn", [1, D])
    rw_d = din("router_w", [D, NEXP]); rb_d = din("router_b", [1, NEXP])
    NE_ = NEXP if STAGE == "full" else 1
    w1_d = din("w1", [NE_, D, 2 * D]); b1_d = din("b1", [NEXP, 2 * D])
    w2_d = din("w2", [NE_, D, D]); b2_d = din("b2", [NEXP, D]); nfin_d = din("norm_final", [1, D])
    out_d = nc.dram_tensor("out", [OWN, D], F32, kind="ExternalOutput").ap()
    HB_d = nc.dram_tensor("HB", [128, 8, OWN], BF16).ap()
    YB_d = nc.dram_tensor("YB", [128, 8, OWN], BF16).ap()
    YA_d = nc.dram_tensor("YA", [128, 8, OWN], BF16).ap()
    YI_d = nc.dram_tensor("YI", [128, 8, OWN], BF16).ap()
    MB_d = nc.dram_tensor("MB", [3, 128, D], F32).ap()
    XS_d = nc.dram_tensor("XS", [64 * 512, D], BF16).ap()
    YS_d = nc.dram_tensor("YS", [64 * 512, D], F32).ap()
    FXS_d = nc.dram_tensor("FXS", [OWN, D], BF16).ap()
    WB1_d = nc.dram_tensor("WB1", [NEXP, 128, 8 * 2 * D], BF16).ap()
    WB2_d = nc.dram_tensor("WB2", [NEXP, 128, 8 * D], BF16).ap()
    B1T_d = nc.dram_tensor("B1T", [NEXP * 128, 16], F32).ap()
    if DEBUG:
        X1_d = nc.dram_tensor("X1", [OWN, D], F32, kind="ExternalOutput").ap()
    else:
        X1_d = nc.dram_tensor("X1", [OWN, D], F32).ap()

    with ExitStack() as es0:
        fw = FW(nc, es0)
        es0.enter_context(nc.Block())
        pe, act, dve, pool, sp = fw.pe, fw.act, fw.dve, fw.pool, fw.sp
        V, S, G, PE = nc.vector, nc.scalar, nc.gpsimd, nc.tensor
        HB_t = [T(None, f"HB{n}") for n in range(8)]
        YB_t = [T(None, f"YB{n}") for n in range(8)]
        YA_t = [T(None, f"YA{n}") for n in range(8)]
        YI_t = [T(None, f"YI{n}") for n in range(8)]
        MB_t = T(None, "MB")
        X1_t = [T(None, f"X1{n}") for n in range(32)]

        def wview(ap2d):
            return ap2d.rearrange("(kc p) n -> p kc n", p=128)

        identf = fw.sb("identf", [128, 128], F32)
        identb = fw.sb("identb", [128, 128], BF16)
        ones = fw.sb("ones", [128, 128], F32)
        mhalf = fw.sb("mhalf", [128, 1], F32)
        fw.op(pool, lambda: G.memset(ones[:], 1.0), writes=[ones])
        fw.op(pool, lambda: G.memset(mhalf[:], -0.5), writes=[mhalf])
        fw.op(pool, lambda: G.memset(identf[:], 1.0), writes=[identf])
        fw.op(pool, lambda: G.affine_select(identf[:], identf[:], [[-1, 128]], ALU.is_equal, 0.0, base=0,
                                            channel_multiplier=1), reads=[identf], writes=[identf])
        fw.op(dve, lambda: V.tensor_copy(identb[:], identf[:]), reads=[identf], writes=[identb])

        class NormBufs:
            def __init__(self, dt_out, tag):
                self.i = 0
                self.xt = [fw.sb(f"xt{tag}{i}", [128, D], F32) for i in range(2)]
                self.junk = fw.sb(f"junk{tag}", [128, D], BF16)
                self.ss = [fw.sb(f"ss{tag}{i}", [128, 1], F32) for i in range(2)]
                self.rs = [fw.sb(f"rs{tag}{i}", [128, 1], F32) for i in range(2)]
                self.t1 = [fw.sb(f"t1{tag}{i}", [128, D], F32) for i in range(2)]
                self.hx = [fw.sb(f"hx{tag}{i}", [128, D], dt_out) for i in range(2)]

        def norm_load(nb, src_ap, nr, src_dep=()):
            i = nb.i; nb.i ^= 1
            xt = nb.xt[i]
            fw.dma(sp, xt[0:nr, :], src_ap, xt, reads=list(src_dep), writes=[xt])
            return i

        def norm_tile(nb, src_ap, nr, Gt, SHt, src_dep=()):
            i = norm_load(nb, src_ap, nr, src_dep)
            return norm_compute(nb, i, nr, Gt, SHt)

        def norm_compute(nb, i, nr, Gt, SHt):
            xt, ss, rs, t1, hx = nb.xt[i], nb.ss[i], nb.rs[i], nb.t1[i], nb.hx[i]
            fw.op(act, lambda: S.activation(nb.junk[0:nr, :], xt[0:nr, :], AF.Square, scale=1.0 / 32.0,
                                            accum_out=ss[0:nr, :]), reads=[xt], writes=[nb.junk, ss])
            fw.op(pool, lambda: G.tensor_scalar(rs[0:nr, :], ss[0:nr, :], EPS, None, ALU.add), reads=[ss], writes=[rs])
            fw.op(pool, lambda: G.tensor_tensor(rs[0:nr, :], rs[0:nr, :], mhalf[0:nr, :], ALU.pow),
                  reads=[rs, mhalf], writes=[rs])
            fw.op(dve, lambda: V.scalar_tensor_tensor(t1[0:nr, :], xt[0:nr, :], rs[0:nr, 0:1], Gt[0:nr, :],
                                                      ALU.mult, ALU.mult), reads=[xt, rs, Gt], writes=[t1])
            fw.op(pool, lambda: G.tensor_tensor(hx[0:nr, :], t1[0:nr, :], SHt[0:nr, :], ALU.add),
                  reads=[t1, SHt], writes=[hx])
            return hx, xt

        class HxT:
            def __init__(self, ncols, tag):
                self.nb = NormBufs(BF16, tag)
                self.ptr = [fw.ps(f"ptr{tag}{i}", [128, D], BF16) for i in range(2)]
                self.out = [fw.sb(f"hxT{tag}{i}", [128, 8, ncols], BF16) for i in range(2)]
                self.i = 0; self.pi = 0

            def start(self, src_d, row0, ntok, Gt, SHt):
                o = self.out[self.i]; self.i ^= 1

                def gen():
                    nb = self.nb
                    tiles = []
                    c = 0
                    while c < ntok:
                        tiles.append((c, min(128, ntok - c)))
                        c += 128
                    nt = len(tiles)
                    st_ = {}

                    def stage(t, s_):
                        c, nr = tiles[t]
                        if s_ == -1:
                            st_[t] = norm_load(nb, src_d[row0 + c: row0 + c + nr, :], nr)
                            return
                        i = st_[t]
                        xt, ss, rs, t1, hx = nb.xt[i], nb.ss[i], nb.rs[i], nb.t1[i], nb.hx[i]
                        if s_ == 0:
                            fw.op(act, lambda: S.activation(nb.junk[0:nr, :], xt[0:nr, :], AF.Square, scale=1.0 / 32.0,
                                                            accum_out=ss[0:nr, :]), reads=[xt], writes=[nb.junk, ss])
                            fw.op(pool, lambda: G.tensor_scalar(rs[0:nr, :], ss[0:nr, :], EPS, None, ALU.add), reads=[ss], writes=[rs])
                            fw.op(pool, lambda: G.tensor_tensor(rs[0:nr, :], rs[0:nr, :], mhalf[0:nr, :], ALU.pow),
                                  reads=[rs, mhalf], writes=[rs])
                        elif s_ == 1:
                            fw.op(dve, lambda: V.scalar_tensor_tensor(t1[0:nr, :], xt[0:nr, :], rs[0:nr, 0:1], Gt[0:nr, :],
                                                                      ALU.mult, ALU.mult), reads=[xt, rs, Gt], writes=[t1])
                            fw.op(pool, lambda: G.tensor_tensor(hx[0:nr, :], t1[0:nr, :], SHt[0:nr, :], ALU.add),
                                  reads=[t1, SHt], writes=[hx])
                        elif s_ == 2:
                            p = self.ptr[t % 2]
                            for kc in range(8):
                                fw.op(pe, lambda: PE.transpose(p[:, kc * 128: kc * 128 + nr], hx[0:nr, kc * 128:(kc + 1) * 128],
                                                               identb[0:nr, 0:nr]), reads=[hx, identb], writes=[p], inc=(kc == 7))
                        else:
                            p = self.ptr[t % 2]
                            pv = p.t[:].rearrange("p (k n) -> p k n", k=8)
                            fw.op(act, lambda: S.copy(o[:, :, c:c + nr], pv[:, :, 0:nr]), reads=[p], writes=[o])

                    stage(0, -1)
                    ncalls = 2 * (nt - 1) + 4
                    for ci_ in range(ncalls):
                        for t in range(nt):
                            s_ = ci_ - 2 * t
                            if s_ == 0 and t + 1 < nt:
                                stage(t + 1, -1)
                            if 0 <= s_ <= 3:
                                stage(t, s_)
                        yield
                return o, gen()

            def make(self, src_d, row0, ntok, Gt, SHt):
                o, g = self.start(src_d, row0, ntok, Gt, SHt)
                for _ in g:
                    pass
                return o

        def drain(g):
            for _ in g:
                pass

        def pipelined(hb, order, ntok):
            hxT = hb.make(x_d, order[0] * TB, ntok, G1h[0], SH1h[0])
            for idx, n in enumerate(order):
                if idx + 1 < len(order):
                    nxt, g = hb.start(x_d, order[idx + 1] * TB, ntok, G1h[0], SH1h[0])
                else:
                    nxt, g = None, iter(())
                yield n, hxT, g
                drain(g)
                hxT = nxt

        G1h = [None]; SH1h = [None]

        def proj(ps_ap, pst, wt, col0, hxT, n0, n):
            for kc in range(8):
                fw.op(pe, lambda: PE.matmul(ps_ap, wt[:, kc, col0:col0 + 128], hxT[:, kc, n0:n0 + n],
                                            start=(kc == 0), stop=(kc == 7)), reads=[wt, hxT], writes=[pst], inc=(kc == 7))

        def load_w(tile, ap2d, n):
            for c0 in range(0, n, 512):
                fw.dma(pool, tile[:, :, c0:c0 + 512], wview(ap2d[:, c0:c0 + 512]), tile, writes=[tile])

        wbsem = fw.newsem("wbsem", es0)
        wbcnt = [0]

        def precast_gen():
            if STAGE != "full":
                return
            for e in range(NEXP):
                inst = G.dma_start(out=WB1_d[e].rearrange("p (k n) -> p k n", k=8), in_=w1_d[e].rearrange("(k p) n -> p k n", p=128))
                inst.then_inc(wbsem.h, 16); wbcnt[0] += 16
                yield
                inst = G.dma_start(out=WB2_d[e].rearrange("p (k n) -> p k n", k=8), in_=w2_d[e].rearrange("(k p) n -> p k n", p=128))
                inst.then_inc(wbsem.h, 16); wbcnt[0] += 16
                yield
        pcg = precast_gen()

        with ExitStack() as esM:
            fw.es = esM
            G1 = fw.sb("G1", [128, D], F32); SH1 = fw.sb("SH1", [128, D], F32)
            HG1 = fw.sb("HG1", [128, D], F32)
            G1h[0] = G1; SH1h[0] = SH1
            vT1 = fw.sb("vT1", [128, 128], F32); vT2 = fw.sb("vT2", [128, 64], F32)
            sc = fw.sb("sc", [128, 96], F32)
            stA = [fw.sb(f"stA{i}", [128, 1], F32) for i in range(8)]; stB = [fw.sb(f"stB{i}", [128, 1], F32) for i in range(8)]
            zst = fw.sb("zst", [128, 8], F32)
            esL = ExitStack()
            fw.es = esL
            Dg = fw.sb("Dg", [128, 64, 128], BF16)
            Wg = fw.sb("Wg", [128, 32, 128], BF16)
            esC = ExitStack()
            fw.es = esC
            G1c = fw.sb("G1c", [128, D], F32); SH1c = fw.sb("SH1c", [128, D], F32)
            C_C, C_CC, C_CB, C_BA, C_BX, C_LAM, C_BM, C_CAW = 0, 8, 16, 32, 48, 64, 80, 96
            S_HBA, S_HBX, S_CS, S_HC, S_HBM = 0, 16, 32, 48, 64

            with ExitStack() as esA:
                fw.es = esA
                vr1 = fw.sb("vr1", [128, 128], F32); vr2 = fw.sb("vr2", [64, 128], F32)
                G2 = fw.sb("G2", [128, D], F32); SH2 = fw.sb("SH2", [128, D], F32); GATE2 = fw.sb("GATE2", [128, D], F32)
                fw.op(pool, lambda: G.memset(vr1[:], 0.0), writes=[vr1])
                rows = [(cv_d, C_C, 16), (lcb_d, C_CB, 16), (lba_d, C_BA, 16), (lbx_d, C_BX, 16), (lam_d, C_LAM, 16),
                        (bm_d, C_BM, 16), (caw_d, C_CAW, 24)]
                for ap, r0, n in rows:
                    fw.dma(sp, vr1[r0:r0 + n, :], ap.rearrange("d (k p) -> (d k) p", p=128), vr1, writes=[vr1])
                fw.dma(sp, vr2[:, :], lcw_d.rearrange("d j (k p) -> (d j k) p", p=128), vr2, writes=[vr2])
                pt = fw.ps("pt0", [128, 128], F32)
                fw.op(pe, lambda: PE.transpose(pt[:, 0:120], vr1[0:120, :], identf[0:120, 0:120]), reads=[vr1, identf], writes=[pt])
                fw.op(dve, lambda: V.tensor_copy(vT1[:, 0:120], pt[:, 0:120]), reads=[pt], writes=[vT1])
                fw.op(pe, lambda: PE.transpose(pt[:, 0:64], vr2[0:64, :], identf[0:64, 0:64]), reads=[vr2, identf], writes=[pt])
                fw.op(dve, lambda: V.tensor_copy(vT2[:, :], pt[:, 0:64]), reads=[pt], writes=[vT2])
                fw.op(dve, lambda: V.tensor_scalar(sc[:, S_HBA:S_HBA + 32], vT1[:, C_BA:C_BA + 32], 0.5, None, ALU.mult),
                      reads=[vT1], writes=[sc])
                fw.op(dve, lambda: V.tensor_scalar(sc[:, S_HBM:S_HBM + 16], vT1[:, C_BM:C_BM + 16], 0.5, None, ALU.mult),
                      reads=[vT1], writes=[sc])
                tl = fw.sb("tl", [128, 16], F32)
                fw.op(act, lambda: S.activation(tl[:], vT1[:, C_LAM:C_LAM + 16], AF.Exp, scale=-1.0), reads=[vT1], writes=[tl])
                fw.op(act, lambda: S.activation(tl[:], tl[:], AF.Ln, bias=1.0, scale=1.0), reads=[tl], writes=[tl])
                fw.op(dve, lambda: V.tensor_scalar(sc[:, S_CS:S_CS + 16], tl[:], -8.0, None, ALU.mult), reads=[tl], writes=[sc])
                fw.op(dve, lambda: V.tensor_scalar(sc[:, S_HC:S_HC + 16], tl[:], -4.0, None, ALU.mult), reads=[tl], writes=[sc])
                fw.op(pool, lambda: G.memset(zst[:], 0.0), writes=[zst])
                for idx in range(64):
                    fw.op(dve, lambda: V.tensor_scalar(Dg[:, idx, :], identf[:], vT2[:, idx:idx + 1], None, ALU.mult),
                          reads=[identf, vT2], writes=[Dg])
                fw.op(pool, lambda: G.memset(Wg[:], 0.0), writes=[Wg])
                for g, wd in enumerate((lwa_d, lwx_d)):
                    for d in range(2):
                        base = (g * 2 + d) * 8
                        src = wd[d].rearrange("(kc two) k j -> two k kc j", two=2)
                        for h in range(2):
                            fw.dma(pool, Wg[64 * h:64 * h + 64, base:base + 8, 64 * h:64 * h + 64], src[h], Wg, writes=[Wg])
                sil = fw.sb("sil", [128, 16], F32); th0 = fw.sb("th0", [128, 16], F32)
                fw.op(act, lambda: S.activation(th0[:], vT1[:, 0:16], AF.Tanh, scale=0.5), reads=[vT1], writes=[th0])
                fw.op(dve, lambda: V.tensor_scalar(th0[:], th0[:], 0.5, 0.5, ALU.mult, ALU.add), reads=[th0], writes=[th0])
                fw.op(dve, lambda: V.tensor_tensor(sil[:], th0[:], vT1[:, 0:16], ALU.mult), reads=[th0, vT1], writes=[sil])
                Sx = fw.sb("Sx", [128, 16, 128], F32)
                for k in range(16):
                    fw.op(dve, lambda: V.tensor_scalar(Sx[:, k, :], ones[:], sil[:, k:k + 1], None, ALU.mult),
                          reads=[ones, sil], writes=[Sx])
                nmx = fw.sb("nmx", [128, D], F32); nfx = fw.sb("nfx", [128, D], F32)
                fw.dma(sp, nmx[:], nmix_d[0:1, :].partition_broadcast(128), nmx, writes=[nmx])
                fw.dma(sp, nfx[:], nffn_d[0:1, :].partition_broadcast(128), nfx, writes=[nfx])
                wad = [fw.sb(f"wad{i}", [128, 8, 512], F32) for i in range(2)]
                bad = [fw.sb(f"bad{i}", [128, 512], F32) for i in range(2)]
                pa = [fw.ps(f"pa{i}", [128, 512], F32) for i in range(2)]
                tmpg = fw.sb("tmpg", [128, 512], F32)
                dests = {0: (SH1, 0), 1: (G1, 1), 2: (HG1, 2), 3: (SH2, 0), 4: (G2, 1), 5: (GATE2, 0)}
                cdests = {0: (SH1c, 0), 1: (G1c, 1)}
                for g in range(12):
                    wt = wad[g % 2]; bt = bad[g % 2]
                    fw.dma(sp, wt[:], wview(wada_d[:, g * 512:(g + 1) * 512]), wt, writes=[wt])
                    fw.dma(sp, bt[:], bada_d[0:1, g * 512:(g + 1) * 512].partition_broadcast(128), bt, writes=[bt])
                    for which in range(2):
                        if which == 1 and g >= 4:
                            continue
                        p = pa[which]
                        for kc in range(8):
                            fw.op(pe, lambda: PE.matmul(p[:], Sx[:, which * 8 + kc, :], wt[:, kc, :], start=(kc == 0), stop=(kc == 7)),
                                  reads=[Sx, wt], writes=[p], inc=(kc == 7))
                        dt_, kind = (dests if which == 0 else cdests)[g // 2]
                        cs = slice((g % 2) * 512, (g % 2) * 512 + 512)
                        if kind == 0:
                            fw.op(dve, lambda: V.tensor_tensor(dt_[:, cs], p[:], bt[:], ALU.add), reads=[p, bt], writes=[dt_])
                        elif kind == 2:
                            fw.op(dve, lambda: V.tensor_tensor(tmpg[:], p[:], bt[:], ALU.add), reads=[p, bt], writes=[tmpg])
                            fw.op(dve, lambda: V.tensor_scalar(dt_[:, cs], tmpg[:], 0.5, None, ALU.mult), reads=[tmpg], writes=[dt_])
                        else:
                            nrm = nmx if (which == 1 or g // 2 == 1) else nfx
                            fw.op(dve, lambda: V.tensor_tensor(tmpg[:], p[:], bt[:], ALU.add), reads=[p, bt], writes=[tmpg])
                            fw.op(dve, lambda: V.scalar_tensor_tensor(dt_[:, cs], tmpg[:], 1.0, nrm[:, cs], ALU.add, ALU.mult),
                                  reads=[tmpg, nrm], writes=[dt_])
                for i_, t_ in enumerate((G2, SH2, GATE2)):
                    fw.dma(pool, MB_d[i_], t_[:], t_, reads=[t_], writes=[MB_t])
                fw.barrier()
            fw.es = esC

            class LruBufs:
                def __init__(self, nset=2, extra=None):
                    self.i2 = 0; self.ig = 0; self.ih = 0; self.nset = nset
                    mk = lambda nm, dt, k=2: [fw.sb(f"{nm}{i}", [128, TB], dt) for i in range(k)]
                    self.v = mk("lv", BF16); self.thr = mk("lthr", F32); self.thi = mk("lthi", F32)
                    self.a = mk("la", F32, nset); self.a2 = mk("la2", F32, nset); self.bb = mk("lbb", F32, nset)
                    self.h = mk("lh", F32)
                    self.pv = [fw.ps(f"lpv{i}", [128, TB], F32) for i in range(2)]
                    self.pr = [fw.ps(f"lpr{i}", [128, TB], F32) for i in range(1)]
                    self.pi_ = [fw.ps(f"lpi{i}", [128, TB], F32) for i in range(1)]
                    if extra is not None:
                        self.pr.append(extra[0]); self.pi_.append(extra[1])

            def lru_alpha(lb, d, kc, uext, n, reverse):
                i = lb.i2; lb.i2 ^= 1
                g_ = lb.ig; lb.ig = (lb.ig + 1) % lb.nset
                v, thr, thi = lb.v[i], lb.thr[i], lb.thi[i]
                a, a2, bb = lb.a[g_], lb.a2[g_], lb.bb[g_]
                pv, pr, pi_ = lb.pv[i], lb.pr[i % len(lb.pr)], lb.pi_[i % len(lb.pi_)]
                ci = d * 8 + kc
                for j in range(4):
                    off = (6 - j) if reverse else j
                    fw.op(pe, lambda: PE.matmul(pv[:, 0:n], Dg[:, d * 32 + j * 8 + kc, :], uext[:, kc, off:off + n],
                                                start=(j == 0), stop=(j == 3)), reads=[Dg, uext], writes=[pv], inc=(j == 3))
                fw.op(dve, lambda: V.tensor_scalar(v[:, 0:n], pv[:, 0:n], vT1[:, C_CB + ci:C_CB + ci + 1], None, ALU.add),
                      reads=[pv, vT1], writes=[v])
                fw.op(pe, lambda: PE.matmul(pr[:, 0:n], Wg[:, (0 * 2 + d) * 8 + kc, :], v[:, 0:n], start=True, stop=True),
                      reads=[Wg, v], writes=[pr])
                fw.op(pe, lambda: PE.matmul(pi_[:, 0:n], Wg[:, (1 * 2 + d) * 8 + kc, :], v[:, 0:n], start=True, stop=True),
                      reads=[Wg, v], writes=[pi_])
                fw.op(act, lambda: S.activation(thr[:, 0:n], pr[:, 0:n], AF.Tanh, bias=sc[:, S_HBA + ci:S_HBA + ci + 1], scale=0.5),
                      reads=[pr, sc], writes=[thr])
                fw.op(act, lambda: S.activation(thi[:, 0:n], pi_[:, 0:n], AF.Tanh, bias=sc[:, S_HBX + ci:S_HBX + ci + 1], scale=0.5),
                      reads=[pi_, sc], writes=[thi])
                fw.op(act, lambda: S.activation(a[:, 0:n], thr[:, 0:n], AF.Exp, bias=sc[:, S_HC + ci:S_HC + ci + 1],
                                                scale=sc[:, S_HC + ci:S_HC + ci + 1]), reads=[thr, sc], writes=[a])
                fw.op(pool, lambda: G.tensor_tensor(a2[:, 0:n], a[:, 0:n], a[:, 0:n], ALU.mult), reads=[a], writes=[a2])
                fw.op(dve, lambda: V.scalar_tensor_tensor(bb[:, 0:n], thi[:, 0:n], 1.0, v[:, 0:n], ALU.add, ALU.mult),
                      reads=[thi, v], writes=[bb])
                return (a, a2, bb)

            def lru_sqrt(ctx_, n):
                a, a2, bb = ctx_
                fw.op(act, lambda: S.activation(a2[:, 0:n], a2[:, 0:n], AF.Sqrt, bias=1.0, scale=-1.0), reads=[a2], writes=[a2])

            def lru_beta(lb, ctx_, kc, n, st, reverse):
                a, a2, bb = ctx_
                h = lb.h[lb.ih]; lb.ih ^= 1
                fw.op(dve, lambda: V.scalar_tensor_tensor(bb[:, 0:n], bb[:, 0:n], 0.5, a2[:, 0:n], ALU.mult, ALU.mult),
                      reads=[bb, a2], writes=[bb])
                if reverse:
                    fw.op(dve, lambda: V.tensor_tensor_scan(h[:, 0:n][:, ::-1], a[:, 0:n][:, ::-1], bb[:, 0:n][:, ::-1],
                                                            st[kc][:, 0:1], ALU.mult, ALU.add), reads=[a, bb, st[kc]], writes=[h])
                    fw.op(pool, lambda: G.tensor_copy(st[kc][:, 0:1], h[:, 0:1]), reads=[h], writes=[st[kc]])
                else:
                    fw.op(dve, lambda: V.tensor_tensor_scan(h[:, 0:n], a[:, 0:n], bb[:, 0:n], st[kc][:, 0:1], ALU.mult, ALU.add),
                          reads=[a, bb, st[kc]], writes=[h])
                    fw.op(pool, lambda: G.tensor_copy(st[kc][:, 0:1], h[:, n - 1:n]), reads=[h], writes=[st[kc]])
                return h

            def lru_group(lb, d, kcs, uext, n, st, reverse, cb=None, cba=None):
                ctxs = []
                for kc in kcs:
                    ctxs.append(lru_alpha(lb, d, kc, uext, n, reverse))
                    if cba is not None:
                        cba(kc)
                for c_ in ctxs:
                    lru_sqrt(c_, n)
                for kc, c_ in zip(kcs, ctxs):
                    h = lru_beta(lb, c_, kc, n, st, reverse)
                    if cb is not None:
                        cb(kc, h)

            with ExitStack() as es1:
                fw.es = es1
                w_lx = fw.sb("w_lx", [128, 8, D], BF16)
                load_w(w_lx, win_d[:, 3 * D:4 * D], D)
                hb = HxT(TB, "a")
                pu = [fw.ps(f"pu{i}", [128, TB], F32) for i in range(2)]
                lb = LruBufs(8, extra=pu)
                uext = [fw.sb(f"uext{i}", [128, 8, TB + 6], BF16) for i in range(2)]
                for u in uext:
                    fw.op(pool, lambda: G.memset(u[:], 0.0), writes=[u])
                for k_ in range(8):
                    fw.op(pool, lambda: G.tensor_copy(stA[k_][:], zst[:, 0:1]), reads=[zst], writes=[stA[k_]])
                    fw.op(pool, lambda: G.tensor_copy(stB[k_][:], zst[:, 0:1]), reads=[zst], writes=[stB[k_]])
                hcT = hb.make(ctx_d, 0, 256, G1c, SH1c)
                ue = uext[0]
                for kc in range(8):
                    p = pu[kc % 2]
                    proj(p[:, 0:256], p, w_lx, kc * 128, hcT, 0, 256)
                    fw.op(dve, lambda: V.tensor_copy(ue[:, kc, 3:259], p[:, 0:256]), reads=[p], writes=[ue])
                lru_group(lb, 0, list(range(8)), ue, 256, stA, False)
                lru_group(lb, 1, list(range(8)), ue, 256, stB, True)
                fw.op(pool, lambda: G.memset(ue[:], 0.0), writes=[ue])
                def do_proj1(nb, hxT_, kc):
                    p = pu[kc % 2]
                    proj(p[:], p, w_lx, kc * 128, hxT_, 0, TB)
                    fw.op(dve, lambda: V.tensor_copy(uext[nb % 2][:, kc, 3:3 + TB], p[:]), reads=[p], writes=[uext[nb % 2]])
                order = list(range(15, -1, -1))
                hxT = hb.make(x_d, order[0] * TB, TB, G1, SH1)
                for kc in range(8):
                    do_proj1(order[0], hxT, kc)
                for idx, n in enumerate(order):
                    ue = uext[n % 2]
                    if idx + 1 < len(order):
                        nn = order[idx + 1]
                        nxt, gnx = hb.start(x_d, nn * TB, TB, G1, SH1)
                    else:
                        nn, nxt, gnx = None, None, iter(())

                    def cba1(kc, gnx=gnx):
                        next(gnx, None)
                        if kc == 7:
                            drain(gnx)

                    def cb1(kc, h, n=n, nn=nn, nxt=nxt):
                        if n < 8:
                            fw.dma(pool, HB_d[:, kc, n * TB:(n + 1) * TB], h[:], h, reads=[h], writes=[HB_t[n]])
                        if nn is not None:
                            do_proj1(nn, nxt, kc)
                    lru_group(lb, 1, list(range(8)), ue, TB, stB, True, cb1, cba1)
                    drain(gnx)
                    if nn is not None:
                        un = uext[nn % 2]
                        fw.op(pool, lambda: G.tensor_copy(un[:, :, 3 + TB:6 + TB], ue[:, :, 3:6]), reads=[ue], writes=[un])
                fw.barrier()
            esC.close()
            fw.es = esL

            with ExitStack() as es2:
                fw.es = es2
                w_lx = fw.sb("w_lx", [128, 8, D], BF16); w_lg = fw.sb("w_lg", [128, 8, D], BF16)
                load_w(w_lx, win_d[:, 3 * D:4 * D], D); load_w(w_lg, win_d[:, 4 * D:5 * D], D)
                hb = HxT(TB, "b")
                pu = [fw.ps(f"pu{i}", [128, TB], F32) for i in range(2)]
                lb = LruBufs(4, extra=pu)
                uext = [fw.sb(f"uext{i}", [128, 8, TB + 6], BF16) for i in range(2)]
                for u in uext:
                    fw.op(pool, lambda: G.memset(u[:], 0.0), writes=[u])
                hbl = [fw.sb(f"hbl{i}", [128, 8, TB], BF16) for i in range(1)]
                ybin = [fw.sb(f"ybin{i}", [128, 8, TB], BF16) for i in range(2)]
                mk = lambda nm, dt: [fw.sb(f"{nm}{i}", [128, TB], dt) for i in range(2)]
                xs_, x2_, th_, hs_ = mk("gxs", F32), mk("gx2", F32), mk("gth", F32), mk("ghs", F32)
                def do_proj2(nb, hxT_, kc):
                    p = pu[kc % 2]
                    proj(p[:], p, w_lx, kc * 128, hxT_, 0, TB)
                    fw.op(dve, lambda: V.tensor_copy(uext[nb % 2][:, kc, 3:3 + TB], p[:]), reads=[p], writes=[uext[nb % 2]])
                hxT = hb.make(x_d, 0, TB, G1, SH1)
                for kc in range(8):
                    do_proj2(0, hxT, kc)
                nxt = None
                for n in range(8):
                    if n > 0:
                        hxT = nxt
                    next(pcg, None); next(pcg, None)
                    ue = uext[n % 2]
                    if n + 1 < 8:
                        nn = n + 1
                        nxt, gnx = hb.start(x_d, nn * TB, TB, G1, SH1)
                    else:
                        nn, nxt, gnx = None, None, iter(())
                    hbt = hbl[0]
                    fw.dma(sp, hbt[:], HB_d[:, :, n * TB:(n + 1) * TB], hbt, reads=[HB_t[n]], writes=[hbt])
                    yb = ybin[n % 2]
                    def cb2(kc, h, hxT=hxT, hbt=hbt, yb=yb, gnx=gnx, nn=nn, nxt=nxt):
                        i = kc % 2
                        hs = hs_[i]
                        fw.op(pool, lambda: G.tensor_tensor(hs[:], h[:], hbt[:, kc, :], ALU.add), reads=[h, hbt], writes=[hs])
                        p = pu[kc % 2]
                        proj(p[:], p, w_lg, kc * 128, hxT, 0, TB)
                        xs, x2, th = xs_[i], x2_[i], th_[i]
                        fw.op(act, lambda: S.copy(xs[:], p[:]), reads=[p], writes=[xs])
                        fw.op(act, lambda: S.activation(x2[:], p[:], AF.Square), reads=[p], writes=[x2])
                        fw.op(dve, lambda: V.tensor_scalar(x2[:], x2[:], GC0 * GC1, GC0, ALU.mult, ALU.add), reads=[x2], writes=[x2])
                        fw.op(dve, lambda: V.tensor_tensor(x2[:], x2[:], xs[:], ALU.mult), reads=[x2, xs], writes=[x2])
                        fw.op(act, lambda: S.activation(th[:], x2[:], AF.Tanh), reads=[x2], writes=[th])
                        fw.op(dve, lambda: V.scalar_tensor_tensor(th[:], th[:], 1.0, xs[:], ALU.add, ALU.mult), reads=[th, xs], writes=[th])
                        fw.op(dve, lambda: V.scalar_tensor_tensor(yb[:, kc, :], th[:], 0.5, hs[:], ALU.mult, ALU.mult),
                              reads=[th, hs], writes=[yb])
                        if nn is not None:
                            do_proj2(nn, nxt, kc)

                    def cba2(kc, gnx=gnx):
                        next(gnx, None); next(gnx, None)
                        if kc >= 3:
                            drain(gnx)
                    lru_group(lb, 0, [0, 1, 2, 3], ue, TB, stA, False, cb2, cba2)
                    lru_group(lb, 0, [4, 5, 6, 7], ue, TB, stA, False, cb2, cba2)
                    drain(gnx)
                    if nn is not None:
                        un = uext[nn % 2]
                        fw.op(pool, lambda: G.tensor_copy(un[:, :, 0:3], ue[:, :, TB:TB + 3]), reads=[ue], writes=[un])
                    fw.dma(pool, YI_d[:, :, n * TB:(n + 1) * TB], yb[:], yb, reads=[yb], writes=[YI_t[n]])
                fw.barrier()
            esL.close()
            fw.es = esM

            with ExitStack() as es2:
                fw.es = es2
                w_mb = fw.sb("w_mb", [128, 8, D], BF16); w_ob = fw.sb("w_ob", [128, 8, D], BF16)
                load_w(w_mb, win_d[:, 6 * D:7 * D], D); load_w(w_ob, woutb_d, D)
                hb = HxT(TB, "b2")
                pu = [fw.ps(f"pu{i}", [128, TB], F32) for i in range(2)]
                pyb = [fw.ps(f"pyb{i}", [128, TB], F32) for i in range(2)]
                ybin = [fw.sb(f"ybin{i}", [128, 8, TB], BF16) for i in range(2)]
                gyb = [fw.sb(f"gyb{i}", [128, 8, TB], BF16) for i in range(2)]
                sg_ = [fw.sb(f"gsg{i}", [128, TB], F32) for i in range(2)]
                for n, hxT, gnx in pipelined(hb, list(range(8)), TB):
                    next(pcg, None); next(pcg, None)
                    yb = ybin[n % 2]
                    fw.dma(sp, yb[:], YI_d[:, :, n * TB:(n + 1) * TB], yb, reads=[YI_t[n]], writes=[yb])
                    gy = gyb[n % 2]
                    for mo in range(8):
                        i = mo % 2
                        py = pyb[i]
                        for kc in range(8):
                            fw.op(pe, lambda: PE.matmul(py[:], w_ob[:, kc, mo * 128:(mo + 1) * 128], yb[:, kc, :],
                                                        start=(kc == 0), stop=(kc == 7)), reads=[w_ob, yb], writes=[py], inc=(kc == 7))
                        p = pu[i]
                        proj(p[:], p, w_mb, mo * 128, hxT, 0, TB)
                        sg = sg_[i]
                        fw.op(act, lambda: S.activation(sg[:], p[:], AF.Tanh, bias=sc[:, S_HBM + 8 + mo:S_HBM + 9 + mo], scale=0.5),
                              reads=[p, sc], writes=[sg])
                        fw.op(dve, lambda: V.scalar_tensor_tensor(gy[:, mo, :], sg[:], 1.0, py[:], ALU.add, ALU.mult),
                              reads=[sg, py], writes=[gy])
                        next(gnx, None)
                    fw.dma(pool, YB_d[:, :, n * TB:(n + 1) * TB], gy[:], gy, reads=[gy], writes=[YB_t[n]])
                fw.barrier()
            fw.es = esM


            with ExitStack() as es3:
                fw.es = es3
                w_gb = fw.sb("w_gb", [128, 8, D], BF16); w_gc = fw.sb("w_gc", [128, 8, D], BF16)
                w_xc = fw.sb("w_xc", [128, 8, D], BF16); w_oa = fw.sb("w_oa", [128, 8, D], BF16)
                load_w(w_gc, win_d[:, 1 * D:2 * D], D); load_w(w_xc, win_d[:, 2 * D:3 * D], D)
                load_w(w_gb, win_d[:, 0:D], D); load_w(w_oa, wouta_d, D)
                hb = HxT(TB + 64, "c")
                pext = [fw.sb(f"pext{i}", [128, 8, TB + 128], BF16) for i in range(2)]
                for u in pext:
                    fw.op(pool, lambda: G.memset(u[:], 0.0), writes=[u])
                pg = [fw.ps(f"pg{i}", [128, TB], F32) for i in range(2)]
                px = [fw.ps(f"px{i}", [128, TB], F32) for i in range(2)]
                ph = [fw.ps(f"ph{i}", [128, 128], F32) for i in range(2)]
                mk = lambda nm, dt, w=TB: [fw.sb(f"{nm}{i}", [128, w], dt) for i in range(2)]
                gcs, gch, q_ = mk("gcs", F32), mk("gch", F32, 64), mk("cq", F32)
                yain = [fw.sb(f"yain{i}", [128, 8, TB], BF16) for i in range(2)]
                yat = [fw.sb(f"yat{i}", [128, 8, TB], BF16) for i in range(2)]
                for n, hxT, gnx in pipelined(hb, list(range(8)), TB + 64):
                    next(pcg, None); next(pcg, None)
                    pe_t = pext[n % 2]; pprev = pext[(n + 1) % 2]
                    for kc in range(8):
                        i = kc % 2
                        proj(pg[i][:], pg[i], w_gc, kc * 128, hxT, 0, TB)
                        proj(px[i][:], px[i], w_xc, kc * 128, hxT, 0, TB)
                        fw.op(act, lambda: S.copy(gcs[i][:], pg[i][:]), reads=[pg[i]], writes=[gcs[i]])
                        fw.op(dve, lambda: V.tensor_tensor(pe_t[:, kc, 64:64 + TB], gcs[i][:], px[i][:], ALU.mult),
                              reads=[gcs[i], px[i]], writes=[pe_t])
                        if kc >= 4:
                            proj(ph[i][:, 0:64], ph[i], w_gc, kc * 128, hxT, TB, 64)
                            proj(ph[i][:, 64:128], ph[i], w_xc, kc * 128, hxT, TB, 64)
                            fw.op(act, lambda: S.copy(gch[i][:], ph[i][:, 0:64]), reads=[ph[i]], writes=[gch[i]])
                            fw.op(dve, lambda: V.tensor_tensor(pe_t[:, kc, 64 + TB:128 + TB], gch[i][:], ph[i][:, 64:128], ALU.mult),
                                  reads=[gch[i], ph[i]], writes=[pe_t])
                    if n > 0:
                        fw.op(pool, lambda: G.tensor_copy(pe_t[:, 4:8, 0:64], pprev[:, 4:8, TB:TB + 64]), reads=[pprev], writes=[pe_t])
                    else:
                        fw.op(pool, lambda: G.memset(pe_t[:, 4:8, 0:64], 0.0), writes=[pe_t])
                    ya = yain[n % 2]
                    for kc in range(8):
                        i = kc % 2
                        q = q_[i]
                        w0 = vT1[:, C_CAW + kc:C_CAW + kc + 1]; w1_ = vT1[:, C_CAW + 8 + kc:C_CAW + 9 + kc]
                        w2_ = vT1[:, C_CAW + 16 + kc:C_CAW + 17 + kc]
                        fw.op(dve, lambda: V.tensor_scalar(q[:], pe_t[:, kc, 64:64 + TB], w1_, None, ALU.mult), reads=[pe_t, vT1], writes=[q])
                        if kc < 4:
                            pb = pe_t.t[:, kc, 64:64 + TB].rearrange("p (r c) -> p r c", c=64)
                            qv = q.t[:].rearrange("p (r c) -> p r c", c=64)
                            fw.op(dve, lambda: V.scalar_tensor_tensor(qv[:, :, 1:64], pb[:, :, 0:63], w0, qv[:, :, 1:64], ALU.mult, ALU.add),
                                  reads=[pe_t, q, vT1], writes=[q])
                            fw.op(dve, lambda: V.scalar_tensor_tensor(qv[:, :, 0:63], pb[:, :, 1:64], w2_, qv[:, :, 0:63], ALU.mult, ALU.add),
                                  reads=[pe_t, q, vT1], writes=[q])
                        else:
                            fw.op(dve, lambda: V.scalar_tensor_tensor(q[:], pe_t[:, kc, 0:TB], w0, q[:], ALU.mult, ALU.add),
                                  reads=[pe_t, q, vT1], writes=[q])
                            fw.op(dve, lambda: V.scalar_tensor_tensor(q[:], pe_t[:, kc, 128:128 + TB], w2_, q[:], ALU.mult, ALU.add),
                                  reads=[pe_t, q, vT1], writes=[q])
                        proj(pg[i][:], pg[i], w_gb, kc * 128, hxT, 0, TB)
                        fw.op(dve, lambda: V.tensor_tensor(ya[:, kc, :], q[:], pg[i][:], ALU.mult), reads=[q, pg[i]], writes=[ya])
                        next(gnx, None)
                    yt = yat[n % 2]
                    for mo in range(8):
                        i = mo % 2
                        for kc in range(8):
                            fw.op(pe, lambda: PE.matmul(px[i][:], w_oa[:, kc, mo * 128:(mo + 1) * 128], ya[:, kc, :],
                                                        start=(kc == 0), stop=(kc == 7)), reads=[w_oa, ya], writes=[px[i]], inc=(kc == 7))
                        fw.op(act, lambda: S.copy(yt[:, mo, :], px[i][:]), reads=[px[i]], writes=[yt])
                    fw.dma(pool, YA_d[:, :, n * TB:(n + 1) * TB], yt[:], yt, reads=[yt], writes=[YA_t[n]])
                fw.barrier()
            fw.es = esM

            with ExitStack() as es4:
                fw.es = es4
                w_ma = fw.sb("w_ma", [128, 8, D], BF16); w_oo = fw.sb("w_oo", [128, 8, D], BF16)
                load_w(w_ma, win_d[:, 5 * D:6 * D], D); load_w(w_oo, wo_d, D)
                hb = HxT(TB, "d")
                pm = [fw.ps(f"pm{i}", [128, TB], F32) for i in range(2)]
                pmx = [fw.ps(f"pmx{i}", [128, TB], F32) for i in range(2)]
                yab = [fw.sb(f"yab{i}", [128, 8, TB], BF16) for i in range(2)]
                ybb = [fw.sb(f"ybb{i}", [128, 8, TB], BF16) for i in range(2)]
                mg = [fw.sb(f"mg{i}", [128, 8, TB], BF16) for i in range(2)]
                mk = lambda nm, dt, w=TB: [fw.sb(f"{nm}{i}", [128, w], dt) for i in range(2)]
                sga, tt_ = mk("sga", F32), mk("mtt", F32)
                xr = [fw.sb(f"xr{i}", [128, D], F32) for i in range(2)]
                x1o = [fw.sb(f"x1o{i}", [128, D], F32) for i in range(2)]
                for n, hxT, gnx in pipelined(hb, list(range(8)), TB):
                    next(pcg, None); next(pcg, None)
                    ya = yab[n % 2]; yb = ybb[n % 2]; m = mg[n % 2]
                    fw.dma(sp, ya[:], YA_d[:, :, n * TB:(n + 1) * TB], ya, reads=[YA_t[n]], writes=[ya])
                    fw.dma(sp, yb[:], YB_d[:, :, n * TB:(n + 1) * TB], yb, reads=[YB_t[n]], writes=[yb])
                    for mo in range(8):
                        i = mo % 2
                        proj(pm[i][:], pm[i], w_ma, mo * 128, hxT, 0, TB)
                        fw.op(act, lambda: S.activation(sga[i][:], pm[i][:], AF.Tanh, bias=sc[:, S_HBM + mo:S_HBM + mo + 1], scale=0.5),
                              reads=[pm[i], sc], writes=[sga[i]])
                        fw.op(dve, lambda: V.scalar_tensor_tensor(tt_[i][:], sga[i][:], 1.0, ya[:, mo, :], ALU.add, ALU.mult),
                              reads=[sga[i], ya], writes=[tt_[i]])
                        fw.op(dve, lambda: V.tensor_tensor(m[:, mo, :], tt_[i][:], yb[:, mo, :], ALU.add), reads=[tt_[i], yb], writes=[m])
                        next(gnx, None)
                    for tt in range(4):
                        tile_i = n * 4 + tt
                        xt = xr[tt % 2]; xo = x1o[tt % 2]
                        fw.dma(sp, xt[:], x_d[tile_i * 128:(tile_i + 1) * 128, :], xt, writes=[xt])
                        for hh in range(2):
                            p = pmx[hh]
                            for kc in range(8):
                                fw.op(pe, lambda: PE.matmul(p[:], m[:, kc, tt * 128:(tt + 1) * 128], w_oo[:, kc, hh * 512:(hh + 1) * 512],
                                                            start=(kc == 0), stop=(kc == 7)), reads=[m, w_oo], writes=[p], inc=(kc == 7))
                            cs = slice(hh * 512, hh * 512 + 512)
                            fw.op(dve, lambda: V.tensor_tensor(xo[:, cs], p[:], HG1[:, cs], ALU.mult), reads=[p, HG1], writes=[xo])
                            fw.op(pool, lambda: G.tensor_tensor(xo[:, cs], xo[:, cs], xt[:, cs], ALU.add), reads=[xo, xt], writes=[xo])
                        fw.dma(pool, X1_d[tile_i * 128:(tile_i + 1) * 128, :], xo[:], xo, reads=[xo], writes=[X1_t[tile_i]])
                fw.barrier()
            fw.es = esM
        fw.es = es0

        with ExitStack() as esE:
          if STAGE == "full":
            fw.es = esE
            NS = 64
            I32 = mybir.dt.int32
            for _ in pcg:
                pass
            XS_t = T(None, "XSall"); FXS_t = [T(None, f"FXS{n}") for n in range(32)]
            YS_t = [T(None, f"YS{n}") for n in range(NS)]
            rw = fw.sb("rw", [128, 8, NEXP], F32)
            rbb = fw.sb("rbb", [128, NEXP], F32)
            b2s = fw.sb("b2s", [NEXP, D], F32)
            LG = fw.sb("LG", [128, 32, NEXP], F32); RANK = fw.sb("RANK", [128, 32, NEXP], F32)
            GD = fw.sb("GD", [128, 32, NEXP], F32); MX8 = fw.sb("MX8", [128, 32, 8], F32)
            G4h = fw.sb("G4h", [128, 32, 4], F32); POS4f = fw.sb("POS4f", [128, 32, 4], F32)
            POS4 = fw.sb("POS4", [128, 32, 4], I32)
            cnt = fw.sb("cnt", [128, NEXP], F32); pcn = fw.sb("pcn", [128, NEXP], F32)
            pend = fw.sb("pend", [128, NEXP], F32); pstart = fw.sb("pstart", [128, NEXP], F32)
            esl = fw.sb("esl", [128, NS], F32); wfl = fw.sb("wfl", [128, NS], F32)
            widx = fw.sb("widx", [128, NS], I32); widx2 = fw.sb("widx2", [128, NS], I32); eidx = fw.sb("eidx", [128, NS], I32)
            Ustr = fw.sb("Ustr", [128, 128], F32); iop = fw.sb("iop", [128, 1], F32)
            onesb = fw.sb("onesb", [1, TB], BF16)
            j32 = fw.sb("j32", [128, NEXP], F32)
            fw.dma(sp, rw[:], rw_d.rearrange("(kc p) e -> p kc e", p=128), rw, writes=[rw])
            fw.dma(sp, rbb[:], rb_d[0:1, :].partition_broadcast(128), rbb, writes=[rbb])
            fw.dma(sp, b2s[:], b2_d, b2s, writes=[b2s])
            B1T_t = T(None, "B1Tt")
            with ExitStack() as esb:
                fw.es = esb
                b1r = fw.sb("b1r", [NEXP, 2 * D], F32)
                pb1 = fw.ps("pb1", [128, 16 * NEXP], F32)
                b1Te = fw.sb("b1Te", [128, NEXP, 16], F32)
                fw.dma(sp, b1r[:], b1_d, b1r, writes=[b1r])
                b1v_ = b1r.t[:].rearrange("e (j p two) -> e j two p", p=128, two=2)
                for j in range(8):
                    for two in range(2):
                        s_ = j * 2 + two
                        fw.op(pe, lambda: PE.transpose(pb1[:, s_ * NEXP:(s_ + 1) * NEXP], b1v_[:, j, two, :], identf[0:NEXP, 0:NEXP]),
                              reads=[b1r, identf], writes=[pb1], inc=(s_ == 15))
                fw.op(dve, lambda: V.tensor_copy(b1Te.t[:].rearrange("p e s -> p s e"), pb1.t[:].rearrange("p (s e) -> p s e", e=NEXP)),
                      reads=[pb1], writes=[b1Te])
                fw.dma(sp, B1T_d.rearrange("(e p) s -> p e s", p=128), b1Te[:], b1Te, reads=[b1Te], writes=[B1T_t])
                fw.barrier()
            fw.es = esE
            fw.op(pool, lambda: G.memset(Ustr[:], 1.0), writes=[Ustr])
            fw.op(pool, lambda: G.affine_select(Ustr[:], Ustr[:], [[1, 128]], ALU.is_gt, 0.0, base=0, channel_multiplier=-1),
                  reads=[Ustr], writes=[Ustr])
            fw.op(pool, lambda: G.iota(iop[:], [[0, 1]], base=0, channel_multiplier=1, allow_small_or_imprecise_dtypes=True), writes=[iop])
            fw.op(pool, lambda: G.memset(onesb[:], 1.0), writes=[onesb])
            fw.op(pool, lambda: G.memset(cnt[:], 0.0), writes=[cnt])

            def idma(out, in_, semt, in_off=None, out_off=None, eoff=0, reads=(), writes=()):
                pool.wait(fw._deps(reads, writes))
                if semt.dsem is None:
                    semt.dsem = fw.newsem("d_" + semt.name)
                    fw.dtiles.append(semt)
                inst = G.indirect_dma_start(out=out, out_offset=out_off, in_=in_, in_offset=in_off, element_offset=eoff)
                semt.dcnt += 16
                inst.then_inc(semt.dsem.h, 16)
                d = (semt.dsem, semt.dcnt)
                for t in reads:
                    t.r[semt.dsem.id] = d
                for t in writes:
                    t.w = d; t.r = {}

            with ExitStack() as esR:
                fw.es = esR
                G2 = fw.sb("G2", [128, D], F32); SH2 = fw.sb("SH2", [128, D], F32)
                for i_, t_ in enumerate((G2, SH2)):
                    fw.dma(sp, t_[:], MB_d[i_], t_, reads=[MB_t], writes=[t_])
                zt = fw.sb("zt", [128, 8192], BF16)
                fw.op(pool, lambda: G.memset(zt[:], 0.0), writes=[zt])
                XSv = XS_d.rearrange("(a p r) n -> a p (r n)", p=128, r=8)
                for a_ in range(NS * TB // 1024):
                    fw.dma(act, XSv[a_], zt[:], zt, reads=[zt])
                XS_t.w = (zt.dsem, zt.dcnt)
                nbf = NormBufs(F32, "f")
                ptf = fw.ps("ptf", [128, D], F32)
                fxf = [fw.sb(f"fxf{i}", [128, 8, 128], F32) for i in range(2)]
                fxb = [fw.sb(f"fxb{i}", [128, D], BF16) for i in range(2)]
                plg = fw.ps("plg", [128, 128], F32); prk = fw.ps("prk", [128, 128], F32); pcn_ = fw.ps("pcnp", [128, 128], F32)
                nmx_ = fw.sb("nmx_", [128, 1], F32); msk = fw.sb("msk", [128, NEXP], F32); ex = fw.sb("ex", [128, NEXP], F32)
                den = fw.sb("den", [128, 1], F32); e4 = fw.sb("e4", [128, 4], F32); den4 = fw.sb("den4", [128, 1], F32)
                fx_next, _ = norm_tile(nbf, X1_d[0:128, :], 128, G2, SH2, src_dep=[X1_t[0]])
                for ti in range(32):
                    fx = fx_next
                    if ti + 1 < 32:
                        fx_next, _ = norm_tile(nbf, X1_d[(ti + 1) * 128:(ti + 2) * 128, :], 128, G2, SH2, src_dep=[X1_t[ti + 1]])
                    fb = fxb[ti % 2]
                    fw.op(pool, lambda: G.tensor_copy(fb[:], fx[:]), reads=[fx], writes=[fb])
                    fw.dma(sp, FXS_d[ti * 128:(ti + 1) * 128, :], fb[:], fb, reads=[fb], writes=[FXS_t[ti]])
                    for kc in range(8):
                        fw.op(pe, lambda: PE.transpose(ptf[:, kc * 128:(kc + 1) * 128], fx[:, kc * 128:(kc + 1) * 128], identf[:]),
                              reads=[fx, identf], writes=[ptf], inc=(kc == 7))
                    ff = fxf[ti % 2]
                    fw.op(act, lambda: S.copy(ff.t[:].rearrange("p k n -> p (k n)"), ptf[:]), reads=[ptf], writes=[ff])
                    for kc in range(8):
                        fw.op(pe, lambda: PE.matmul(plg[:, 0:NEXP], ff[:, kc, :], rw[:, kc, :], start=(kc == 0), stop=(kc == 7)),
                              reads=[ff, rw], writes=[plg], inc=(kc == 7))
                    lg = LG[:, ti, :]; mx8 = MX8[:, ti, :]
                    fw.op(dve, lambda: V.tensor_tensor(lg, plg[:, 0:NEXP], rbb[:], ALU.add), reads=[plg, rbb], writes=[LG])
                    fw.op(dve, lambda: V.max(mx8, lg), reads=[LG], writes=[MX8])
                    fw.op(dve, lambda: V.tensor_scalar(msk[:], lg, MX8[:, ti, 3:4], None, ALU.is_ge), reads=[LG, MX8], writes=[msk])
                    fw.op(dve, lambda: V.tensor_scalar(nmx_[:], MX8[:, ti, 0:1], -1.0, None, ALU.mult), reads=[MX8], writes=[nmx_])
                    fw.op(act, lambda: S.activation(ex[:], lg, AF.Exp, bias=nmx_[:, 0:1], scale=1.0), reads=[LG, nmx_], writes=[ex])
                    fw.op(act, lambda: S.activation(e4[:], MX8[:, ti, 0:4], AF.Exp, bias=nmx_[:, 0:1], scale=1.0, accum_out=den4[:]),
                          reads=[MX8, nmx_], writes=[e4, den4])
                    fw.op(dve, lambda: V.reciprocal(den[:], den4[:]), reads=[den4], writes=[den])
                    fw.op(dve, lambda: V.tensor_scalar(G4h[:, ti, :], e4[:], den[:, 0:1], 0.5, ALU.mult, ALU.mult), reads=[e4, den], writes=[G4h])
                    fw.op(dve, lambda: V.scalar_tensor_tensor(GD[:, ti, :], ex[:], den[:, 0:1], msk[:], ALU.mult, ALU.mult),
                          reads=[ex, den, msk], writes=[GD])
                    fw.op(pe, lambda: PE.matmul(prk[:, 0:NEXP], Ustr[:], msk[:], start=True, stop=True), reads=[Ustr, msk], writes=[prk])
                    fw.op(pe, lambda: PE.matmul(pcn_[:, 0:NEXP], ones[:], msk[:], start=True, stop=True), reads=[ones, msk], writes=[pcn_])
                    fw.op(dve, lambda: V.tensor_tensor(RANK[:, ti, :], prk[:, 0:NEXP], cnt[:], ALU.add), reads=[prk, cnt], writes=[RANK])
                    fw.op(dve, lambda: V.tensor_tensor(cnt[:], pcn_[:, 0:NEXP], cnt[:], ALU.add), reads=[pcn_, cnt], writes=[cnt])
                fw.op(dve, lambda: V.tensor_scalar(pcn[:], cnt[:], 0.0, None, ALU.is_gt), reads=[cnt], writes=[pcn])
                for j in range(1, 8):
                    fw.op(dve, lambda: V.scalar_tensor_tensor(pcn[:], cnt[:], 512.0 * j, pcn[:], ALU.is_gt, ALU.add), reads=[cnt, pcn], writes=[pcn])
                fw.op(dve, lambda: V.tensor_scalar(pcn[:], pcn[:], 512.0, None, ALU.mult), reads=[pcn], writes=[pcn])
                fw.op(dve, lambda: V.tensor_tensor_scan(pend[:], ones[:, 0:NEXP], pcn[:], 0.0, ALU.mult, ALU.add), reads=[ones, pcn], writes=[pend])
                fw.op(dve, lambda: V.tensor_tensor(pstart[:], pend[:], pcn[:], ALU.subtract), reads=[pend, pcn], writes=[pstart])
                for s_ in range(NS):
                    fw.op(dve, lambda: V.tensor_scalar(j32[:], pend[:], 512.0 * s_, 0.0, ALU.is_le, ALU.add, accum_out=esl[:, s_:s_ + 1]),
                          reads=[pend], writes=[j32, esl])
                fw.op(dve, lambda: V.tensor_scalar(esl[:], esl[:], float(NEXP - 1), None, ALU.min), reads=[esl], writes=[esl])
                fw.op(dve, lambda: V.tensor_copy(eidx[:], esl[:]), reads=[esl], writes=[eidx])
                fw.op(dve, lambda: V.tensor_scalar(wfl[:], esl[:], 128.0, iop[:, 0:1], ALU.mult, ALU.add), reads=[esl, iop], writes=[wfl])
                fw.op(dve, lambda: V.tensor_copy(widx[:], wfl[:]), reads=[wfl], writes=[widx])
                posf = fw.sb("posf", [128, NEXP], F32)
                fbsc = [T(None, f"fbsc{i}") for i in range(2)]
                for ti in range(32):
                    fw.op(dve, lambda: V.tensor_tensor(posf[:], RANK[:, ti, :], pstart[:], ALU.add), reads=[RANK, pstart], writes=[posf])
                    for k in range(4):
                        fw.op(dve, lambda: V.scalar_tensor_tensor(j32[:], LG[:, ti, :], MX8[:, ti, k:k + 1], posf[:], ALU.is_equal, ALU.mult,
                                                                  accum_out=POS4f[:, ti, k:k + 1]), reads=[LG, MX8, posf], writes=[j32, POS4f])
                fw.op(dve, lambda: V.tensor_copy(POS4[:], POS4f[:]), reads=[POS4f], writes=[POS4])
                for ti in range(32):
                    fb = fxb[ti % 2]
                    fw.dma(sp, fb[:], FXS_d[ti * 128:(ti + 1) * 128, :], fb, reads=[FXS_t[ti]], writes=[fb])
                    for k in range(4):
                        idma(XS_d, fb[:], fbsc[ti % 2], out_off=bass.IndirectOffsetOnAxis(ap=POS4[:, ti, k:k + 1], axis=0), reads=[fb, POS4, XS_t])
                fw.barrier()
            fw.es = esE

            with ExitStack() as esS:
                fw.es = esS
                w1s = [fw.sb(f"w1s{i}", [128, 8, 2 * D], BF16) for i in range(2)]
                w2s = [fw.sb(f"w2s{i}", [128, 8, D], BF16) for i in range(2)]
                b1s = [fw.sb(f"b1s{i}", [128, 16], F32) for i in range(2)]
                xr = [fw.sb(f"xr{i}", [128, 4, D], BF16) for i in range(2)]
                xT = [fw.sb(f"xT{i}", [128, 8, TB], BF16) for i in range(2)]
                ptr = [fw.ps(f"ptrE{i}", [128, D], BF16) for i in range(2)]
                pgl = [fw.ps(f"pgl{i}", [128, TB], F32) for i in range(4)]
                pyy = [fw.ps(f"pyy{i}", [128, TB], F32) for i in range(2)]
                mk = lambda nm, dt: [fw.sb(f"{nm}{i}", [128, TB], dt) for i in range(2)]
                gl_, th_, l1_ = mk("egl", F32), mk("eth", F32), mk("el1", F32)
                actT = [fw.sb(f"actT{i}", [128, 8, TB], BF16) for i in range(2)]
                ysb = [fw.sb(f"ysb{i}", [128, D], F32) for i in range(2)]
                w1f = WB1_d.rearrange("e p n -> (e p) n"); w2f = WB2_d.rearrange("e p n -> (e p) n")
                pool.wait([(wbsem, wbcnt[0])])

                def load_slot(s_):
                    ws = s_ % 2
                    off = bass.IndirectOffsetOnAxis(ap=widx[:, s_:s_ + 1], axis=0)
                    idma(b1s[ws][:], B1T_d, b1s[ws], in_off=off, reads=[widx, B1T_t], writes=[b1s[ws]])
                    idma(w1s[ws].t[:].rearrange("p k n -> p (k n)"), w1f, w1s[ws], in_off=off, reads=[widx], writes=[w1s[ws]])
                    idma(w2s[ws].t[:].rearrange("p k n -> p (k n)"), w2f, w2s[ws], in_off=off, reads=[widx], writes=[w2s[ws]])
                    fw.dma(sp, xr[ws][:], XS_d[s_ * TB:(s_ + 1) * TB, :].rearrange("(t p) n -> p t n", p=128), xr[ws], writes=[xr[ws]])

                def slot_transposes(sx):
                    xtx = xT[sx % 2]; xrx = xr[sx % 2]
                    for t in range(4):
                        p = ptr[t % 2]
                        for kc in range(8):
                            fw.op(pe, lambda: PE.transpose(p[:, kc * 128:(kc + 1) * 128], xrx[:, t, kc * 128:(kc + 1) * 128], identb[:]),
                                  reads=[xrx, identb], writes=[p], inc=(kc == 7))
                        fw.op(act, lambda: S.copy(xtx[:, :, t * 128:(t + 1) * 128], p.t[:].rearrange("p (k n) -> p k n", k=8)),
                              reads=[p], writes=[xtx])

                load_slot(0)
                slot_transposes(0)
                for s_ in range(NS):
                    ws = s_ % 2
                    if s_ + 1 < NS:
                        load_slot(s_ + 1)
                    xt_ = xT[ws]; xr_ = xr[ws]
                    at = actT[ws]
                    w1v = w1s[ws].t[:].rearrange("p k (m two) -> p k m two", two=2)
                    for j in range(8):
                        i = j % 2
                        for two in range(2):
                            p = pgl[i * 2 + two]
                            for kc in range(8):
                                fw.op(pe, lambda: PE.matmul(p[:], w1v[:, kc, j * 128:(j + 1) * 128, two], xt_[:, kc, :],
                                                            start=(kc == 0), stop=(kc == 7)), reads=[w1s[ws], xt_], writes=[p], inc=(kc == 7))
                        gl, th, l1 = gl_[i], th_[i], l1_[i]
                        fw.op(dve, lambda: V.tensor_scalar(gl[:], pgl[i * 2][:], b1s[ws][:, 2 * j:2 * j + 1], 7.0, ALU.add, ALU.min),
                              reads=[pgl[i * 2], b1s[ws]], writes=[gl])
                        fw.op(dve, lambda: V.tensor_scalar(l1[:], pgl[i * 2 + 1][:], b1s[ws][:, 2 * j + 1:2 * j + 2], 7.0, ALU.add, ALU.min),
                              reads=[pgl[i * 2 + 1], b1s[ws]], writes=[l1])
                        fw.op(act, lambda: S.activation(th[:], gl[:], AF.Tanh, scale=0.851), reads=[gl], writes=[th])
                        fw.op(act, lambda: S.activation(l1[:], l1[:], AF.Relu, bias=7.0, scale=1.0), reads=[l1], writes=[l1])
                        fw.op(dve, lambda: V.scalar_tensor_tensor(th[:], th[:], 1.0, gl[:], ALU.add, ALU.mult), reads=[th, gl], writes=[th])
                        fw.op(dve, lambda: V.scalar_tensor_tensor(at[:, j, :], l1[:], -6.0, th[:], ALU.add, ALU.mult),
                              reads=[l1, th], writes=[at])
                    if s_ + 1 < NS:
                        slot_transposes(s_ + 1)
                    for tt in range(4):
                        yb_ = ysb[tt % 2]
                        for hh in range(2):
                            py = pyy[hh]
                            for kc in range(8):
                                fw.op(pe, lambda: PE.matmul(py[:], at[:, kc, tt * 128:(tt + 1) * 128], w2s[ws][:, kc, hh * 512:(hh + 1) * 512],
                                                            start=(kc == 0), stop=(kc == 7)), reads=[at, w2s[ws]], writes=[py], inc=(kc == 7))
                            fw.op(act, lambda: S.copy(yb_[:, hh * 512:(hh + 1) * 512], py[:]), reads=[py], writes=[yb_])
                        r0 = s_ * TB + tt * 128
                        fw.dma(sp, YS_d[r0:r0 + 128, :], yb_[:], yb_, reads=[yb_], writes=[YS_t[s_]])
                fw.barrier()
            fw.es = esE

            with ExitStack() as esC2:
                fw.es = esC2
                GATE2 = fw.sb("GATE2", [128, D], F32); NF = fw.sb("NF", [128, D], F32)
                fw.dma(sp, GATE2[:], MB_d[2], GATE2, reads=[MB_t], writes=[GATE2])
                fw.dma(sp, NF[:], nfin_d[0:1, :].partition_broadcast(128), NF, writes=[NF])
                plg = fw.ps("plgC", [128, 128], F32)
                pyy = [fw.ps(f"pyyC{i}", [128, TB], F32) for i in range(2)]
                ghT = [fw.sb(f"ghT{i}", [NEXP, 128], F32) for i in range(2)]
                yk = [fw.sb(f"yk{i}", [128, D], F32) for i in range(8)]
                accs = [fw.sb(f"accs{i}", [128, D], F32) for i in range(2)]
                x1r = [fw.sb(f"x1r{i}", [128, D], F32) for i in range(2)]
                junk = fw.sb("junkC", [128, D], BF16)
                ssC = [fw.sb(f"ssC{i}", [128, 1], F32) for i in range(2)]
                rsC = [fw.sb(f"rsC{i}", [128, 1], F32) for i in range(2)]
                def cgather(ti):
                    for k in range(4):
                        y_ = yk[(ti % 2) * 4 + k]
                        idma(y_[:], YS_d, y_, in_off=bass.IndirectOffsetOnAxis(ap=POS4[:, ti, k:k + 1], axis=0), reads=[POS4] + YS_t, writes=[y_])
                cgather(0)
                for ti in range(32):
                    i = ti % 2
                    if ti + 1 < 32:
                        cgather(ti + 1)
                    ac = accs[i]; xo = x1r[i]; ss = ssC[i]; rs = rsC[i]; gT = ghT[i]
                    fw.op(pe, lambda: PE.transpose(plg[0:NEXP, :], GD[:, ti, :], identf[:]), reads=[GD, identf], writes=[plg])
                    fw.op(act, lambda: S.copy(gT[:], plg[0:NEXP, :]), reads=[plg], writes=[gT])
                    for hh in range(2):
                        py = pyy[hh]
                        fw.op(pe, lambda: PE.matmul(py[:], gT[:], b2s[:, hh * 512:(hh + 1) * 512], start=True, stop=True),
                              reads=[gT, b2s], writes=[py])
                        fw.op(act, lambda: S.copy(ac[:, hh * 512:(hh + 1) * 512], py[:]), reads=[py], writes=[ac])
                    fw.dma(sp, xo[:], X1_d[ti * 128:(ti + 1) * 128, :], xo, reads=[X1_t[ti]], writes=[xo])
                    for k in range(4):
                        y_ = yk[(ti % 2) * 4 + k]
                        fw.op(dve, lambda: V.scalar_tensor_tensor(ac[:], y_[:], G4h[:, ti, k:k + 1], ac[:], ALU.mult, ALU.add),
                              reads=[y_, G4h, ac], writes=[ac])
                    fw.op(dve, lambda: V.tensor_tensor(ac[:], ac[:], GATE2[:], ALU.mult), reads=[ac, GATE2], writes=[ac])
                    fw.op(pool, lambda: G.tensor_tensor(ac[:], ac[:], xo[:], ALU.add), reads=[ac, xo], writes=[ac])
                    fw.op(act, lambda: S.activation(junk[:], ac[:], AF.Square, scale=1.0 / 32.0, accum_out=ss[:]), reads=[ac], writes=[junk, ss])
                    fw.op(pool, lambda: G.tensor_scalar(rs[:], ss[:], EPS, None, ALU.add), reads=[ss], writes=[rs])
                    fw.op(pool, lambda: G.tensor_tensor(rs[:], rs[:], mhalf[:], ALU.pow), reads=[rs, mhalf], writes=[rs])
                    fw.op(dve, lambda: V.scalar_tensor_tensor(xo[:], ac[:], rs[:, 0:1], NF[:], ALU.mult, ALU.mult), reads=[ac, rs, NF], writes=[xo])
                    fw.dma(sp, out_d[ti * 128:(ti + 1) * 128, :], xo[:], xo, reads=[xo])
                fw.barrier()
            fw.es = esE
            fw.barrier()
        fw.es = es0

    return nc


_NC = None


def kernel(**inp):
    global _NC
    f = lambda a: np.ascontiguousarray(np.asarray(a, dtype=np.float32))
    def wl2(a):
        a = np.asarray(a, dtype=np.float32)
        E_ = a.shape[0]
        return np.ascontiguousarray(a.reshape(E_, 4, 2, 128, D).transpose(0, 1, 3, 2, 4)).reshape(E_, 512, 2 * D)
    x = f(inp["x"]); ctx = f(inp["ctx"]); c = f(inp["c"]); c_ctx = f(inp["c_ctx"])
    if _NC is None:
        _NC = build_nc()
    nc = _NC
    shared = {
        "w_ada": f(inp["w_ada"][0]), "b_ada": f(inp["b_ada"][0]).reshape(1, -1),
        "norm_mix": f(inp["norm_mix"][0]).reshape(1, -1), "w_in": f(inp["w_in"][0]),
        "w_out_a": f(inp["w_out_a"][0]), "w_out_b": f(inp["w_out_b"][0]), "b_merge": f(inp["b_merge"][0]),
        "w_o": f(inp["w_o"][0]), "norm_ffn": f(inp["norm_ffn"][0]).reshape(1, -1),
        "router_w": f(inp["router_w"][0]), "router_b": f(inp["router_b"][0]).reshape(1, -1),
        "w1": f(inp["w1"][0] if STAGE == "full" else inp["w1"][0][:1]), "b1": f(inp["b1"][0]),
        "w2": f(inp["w2"][0] if STAGE == "full" else inp["w2"][0][:1]), "b2": f(inp["b2"][0]),
        "norm_final": f(inp["norm_final"]).reshape(1, -1),
    }
    lru_keys = ["lru_conv_w", "lru_conv_b", "lru_wa", "lru_ba", "lru_wx", "lru_bx", "lru_lambda"]
    in_maps = []
    for k in CORES:
        b, half = k // 2, k % 2
        m = dict(shared)
        if half == 0:
            m["x"] = x[b]; m["ctx"] = ctx[b]
            m["conv_a_w"] = f(inp["conv_a_w"][0])
            for key in lru_keys:
                m[key] = f(inp[key][0])
        else:
            m["x"] = np.ascontiguousarray(x[b][::-1]); m["ctx"] = np.ascontiguousarray(ctx[b][::-1])
            m["conv_a_w"] = np.ascontiguousarray(f(inp["conv_a_w"][0])[::-1])
            for key in lru_keys:
                m[key] = np.ascontiguousarray(f(inp[key][0])[::-1])
        m["cvec"] = np.ascontiguousarray(np.stack([c[b], c_ctx], axis=0))
        in_maps.append(m)
    res = run_bass_kernel_spmd(nc, in_maps, core_ids=list(range(len(CORES))))
    out = np.zeros((4, NTOK, D), np.float32)
    for i_, k in enumerate(CORES):
        b, half = k // 2, k % 2
        o = np.asarray(res.results[i_]["out"], dtype=np.float32)
        if half == 0:
            out[b, :OWN] = o
        else:
            out[b, OWN:] = o[::-1]
    if DEBUG:
        kernel.debug = res.results
    return out
```
